# Optimizing a Trainium2 kernel written in Bass

```python
import math
import jax
import jax.numpy as jnp
from jax import lax
import numpy as np

D_MODEL = 1024
BATCH = 2
SEQ = 8192
DEPTH = 4

GRID_W = 64
CTX_LEN = 256
HEAD_DIM = 64
N_HEADS = D_MODEL // 128
N_KV_HEADS = N_HEADS // 4
GQA_GROUP = N_HEADS // N_KV_HEADS
ATTN_WIDTH = N_HEADS * HEAD_DIM
KV_WIDTH = N_KV_HEADS * HEAD_DIM
WINDOW = 128
BLOCK = 128
ROPE_THETA = 10000.0
HYENA_ORDER = 2
HYENA_WIDTH = D_MODEL // 4
SHORT_CONV = 3
FILTER_BANDS = 16
FILTER_EMB = 2 * FILTER_BANDS + 1
FILTER_HIDDEN = 64
DECAY_TARGET = 1e-2
FAST_DECAY_PCT = 0.3
SLOW_DECAY_PCT = 1.5
FNET_WIDTH = D_MODEL // 4
FNET_GROUPS = 4
FNET_GROUP_DIM = FNET_WIDTH // FNET_GROUPS
N_BRANCHES = 3
Q_END = ATTN_WIDTH
K_END = Q_END + KV_WIDTH
V_END = K_END + KV_WIDTH
HY_END = V_END + (HYENA_ORDER + 1) * HYENA_WIDTH
FN_END = HY_END + FNET_WIDTH
IN_WIDTH = FN_END + N_BRANCHES * D_MODEL
D_FF = 256 * ((8 * D_MODEL // 3 + 255) // 256)
N_EXPERTS = 8
TOP_K = 2
EPS = 1e-6

kernel_name = "hybrid_dit_swa_hyena_fnet_moe"


def rms_norm(x, g):
    xf = x.astype(jnp.float32)
    y = xf * lax.rsqrt(jnp.mean(jnp.square(xf), axis=-1, keepdims=True) + EPS)
    return (y * g.astype(jnp.float32)).astype(x.dtype)


def modulate(x, g, shift, scale):
    return rms_norm(x, g) * (1 + scale[:, None, :]) + shift[:, None, :]


def adaln(cond, w, b, i):
    lo, hi = 3 * i * D_MODEL, 3 * (i + 1) * D_MODEL
    mod = jax.nn.silu(cond) @ w[:, lo:hi] + b[lo:hi]
    return jnp.split(mod, 3, axis=-1)


def axial_rope_tables(L):
    rows = L // GRID_W
    r, col = jnp.meshgrid(jnp.arange(rows), jnp.arange(GRID_W), indexing="ij")
    pos = jnp.stack([r.reshape(-1), col.reshape(-1)], axis=-1).astype(jnp.float32)
    n_freq = HEAD_DIM // 4
    inv = ROPE_THETA ** (-jnp.arange(n_freq, dtype=jnp.float32) / n_freq)
    ang = pos[:, :, None] * inv
    return jnp.cos(ang), jnp.sin(ang)


def apply_axial_rope(x, cos, sin):
    B, L, H, _ = x.shape
    xf = x.astype(jnp.float32).reshape(B, L, H, 2, 2, HEAD_DIM // 4)
    x1, x2 = xf[..., 0, :], xf[..., 1, :]
    c, s = cos[None, :, None], sin[None, :, None]
    out = jnp.stack([x1 * c - x2 * s, x1 * s + x2 * c], axis=-2)
    return out.reshape(B, L, H, HEAD_DIM).astype(x.dtype)


def q_heads(pr, q_gain):
    B, L, _ = pr.shape
    return rms_norm(pr[..., :Q_END].reshape(B, L, N_HEADS, HEAD_DIM), q_gain)


def kv_heads(pr_kv, k_gain):
    B, L, _ = pr_kv.shape
    k = rms_norm(pr_kv[..., :KV_WIDTH].reshape(B, L, N_KV_HEADS, HEAD_DIM), k_gain)
    v = pr_kv[..., KV_WIDTH:].reshape(B, L, N_KV_HEADS, HEAD_DIM)
    return k, v


def softmax_with_sink(s, sink):
    m = jnp.maximum(jnp.max(s, axis=-1, keepdims=True), sink)
    p = jnp.exp(s - m)
    return p / (jnp.sum(p, axis=-1, keepdims=True) + jnp.exp(sink - m))


def latent_window_attention(q, k, v, k_ctx, v_ctx, sink):
    B, L, _, _ = q.shape
    nb = L // BLOCK
    qb = q.reshape(B, nb, BLOCK, N_KV_HEADS, GQA_GROUP, HEAD_DIM)

    def band(t):
        tp = jnp.pad(t, ((0, 0), (BLOCK, BLOCK), (0, 0), (0, 0)))
        tp = tp.reshape(B, nb + 2, BLOCK, N_KV_HEADS, HEAD_DIM)
        return jnp.concatenate([tp[:, :-2], tp[:, 1:-1], tp[:, 2:]], axis=2)

    kb, vb = band(k), band(v)
    scale = HEAD_DIM ** -0.5
    s_loc = jnp.einsum("bnqhgd,bnjhd->bnhgqj", qb, kb,
                       preferred_element_type=jnp.float32) * scale
    qpos = jnp.arange(nb)[:, None, None] * BLOCK + jnp.arange(BLOCK)[None, :, None]
    kpos = (jnp.arange(nb)[:, None, None] - 1) * BLOCK + jnp.arange(3 * BLOCK)[None, None, :]
    mask = (jnp.abs(qpos - kpos) <= WINDOW) & (kpos >= 0) & (kpos < L)
    s_loc = jnp.where(mask[None, :, None, None], s_loc, -jnp.inf)
    s_ctx = jnp.einsum("bnqhgd,bjhd->bnhgqj", qb, k_ctx,
                       preferred_element_type=jnp.float32) * scale
    s = jnp.concatenate([s_loc, s_ctx], axis=-1)
    sk = sink.astype(jnp.float32).reshape(N_KV_HEADS, GQA_GROUP)[None, None, :, :, None, None]
    p = softmax_with_sink(s, sk).astype(v.dtype)
    n_loc = 3 * BLOCK
    o = (jnp.einsum("bnhgqj,bnjhd->bnqhgd", p[..., :n_loc], vb)
         + jnp.einsum("bnhgqj,bjhd->bnqhgd", p[..., n_loc:], v_ctx))
    return o.reshape(B, L, ATTN_WIDTH)


def context_attention(q, k, v, sink):
    B, C, _, _ = q.shape
    qg = q.reshape(B, C, N_KV_HEADS, GQA_GROUP, HEAD_DIM)
    s = jnp.einsum("bqhgd,bjhd->bhgqj", qg, k,
                   preferred_element_type=jnp.float32) * HEAD_DIM ** -0.5
    sk = sink.astype(jnp.float32).reshape(N_KV_HEADS, GQA_GROUP)[None, :, :, None, None]
    p = softmax_with_sink(s, sk).astype(v.dtype)
    o = jnp.einsum("bhgqj,bjhd->bqhgd", p, v)
    return o.reshape(B, C, ATTN_WIDTH)


def hyena_filter_spectrum(L, w1, b1, fr1, w2, b2, fr2, w3):
    f32 = jnp.float32
    t = jnp.linspace(0.0, 1.0, L, dtype=f32)[:, None]
    w = 2.0 * math.pi * jnp.arange(L, dtype=f32)[:, None] / L
    fb = jnp.linspace(1e-4, FILTER_BANDS - 1, FILTER_BANDS, dtype=f32)
    z = jnp.concatenate([t, jnp.cos(fb * w), -jnp.sin(fb * w)], axis=-1)
    h = jnp.sin(fr1.astype(f32) * (z @ w1.astype(f32) + b1.astype(f32)))
    h = jnp.sin(fr2.astype(f32) * (h @ w2.astype(f32) + b2.astype(f32)))
    h = (h @ w3.astype(f32)).reshape(L, 2, HYENA_ORDER, HYENA_WIDTH)
    deltas = jnp.abs(jnp.linspace(math.log(DECAY_TARGET) / SLOW_DECAY_PCT,
                                  math.log(DECAY_TARGET) / FAST_DECAY_PCT,
                                  HYENA_WIDTH, dtype=f32))
    h = h * jnp.exp(-t * deltas)[:, None, None, :]
    k = jnp.concatenate([h[:, 0], jnp.zeros_like(h[:1, 0]), h[:0:-1, 1]], axis=0)
    k = k / jnp.sum(jnp.abs(k), axis=0, keepdims=True)
    return jnp.fft.rfft(k, axis=0)


def hyena_mixer(u, conv_w, conv_b, k_spec, bias):
    L = u.shape[1]
    uc = lax.conv_general_dilated(u, conv_w, window_strides=(1,),
                                  padding=((SHORT_CONV // 2, SHORT_CONV // 2),),
                                  dimension_numbers=("NWC", "WIO", "NWC"),
                                  feature_group_count=u.shape[-1]) + conv_b
    *gates, z = jnp.split(uc.astype(jnp.float32), HYENA_ORDER + 1, axis=-1)
    for o, gate in enumerate(gates):
        zc = jnp.fft.irfft(jnp.fft.rfft(z, n=2 * L, axis=1) * k_spec[None, :, o],
                           n=2 * L, axis=1)[:, :L]
        z = gate * (zc + bias[o].astype(jnp.float32) * z)
    return z.astype(u.dtype)


def fnet_mixer(u):
    B, L, _ = u.shape
    ug = u.astype(jnp.float32).reshape(B, L, FNET_GROUPS, FNET_GROUP_DIM).transpose(0, 2, 1, 3)
    y = jnp.fft.fft2(ug, norm="ortho").real
    return y.transpose(0, 2, 1, 3).reshape(B, L, FNET_WIDTH).astype(u.dtype)


def merge_branches(pr, attn_o, hy_o, fn_o, w_pa, w_ph, w_pf, w_o):
    B, L, _ = pr.shape
    g = jax.nn.sigmoid(pr[..., FN_END:].reshape(B, L, N_BRANCHES, D_MODEL))
    m = (g[:, :, 0] * (attn_o @ w_pa) + g[:, :, 1] * (hy_o @ w_ph) + g[:, :, 2] * (fn_o @ w_pf))
    return m @ w_o


def swiglu(h, wg, wu, wd):
    return (jax.nn.silu(h @ wg) * (h @ wu)) @ wd


def moe_swiglu(h, w_router, wg, wu, wd):
    logits = jnp.einsum("bld,de->ble", h, w_router).astype(jnp.float32)
    top_val, top_idx = lax.top_k(logits, TOP_K)
    top_w = jax.nn.softmax(top_val, axis=-1)
    combine = jnp.einsum("blk,blke->ble", top_w,
                         jax.nn.one_hot(top_idx, N_EXPERTS, dtype=jnp.float32)).astype(h.dtype)
    out = jnp.zeros_like(h)
    for e in range(N_EXPERTS):
        out = out + combine[..., e:e + 1] * swiglu(h, wg[e], wu[e], wd[e])
    return out


def setup_inputs(seed: int = 0) -> dict:
    key = jax.random.key(seed)
    ks = iter(jax.random.split(key, 40))

    def nrm(shape, scale=1.0):
        return jax.random.normal(next(ks), shape, jnp.float32) * scale

    D, W, E, F = D_MODEL, HYENA_WIDTH, N_EXPERTS, D_FF
    n_dense, n_moe = (DEPTH + 1) // 2, DEPTH // 2
    return {
        "x": nrm((BATCH, SEQ, D)),
        "c": nrm((BATCH, D)),
        "ctx": nrm((BATCH, CTX_LEN, D)),
        "c_ctx": nrm((D,)),
        "w_ada": nrm((DEPTH, D, 6 * D), 0.5 * D ** -0.5),
        "b_ada": nrm((DEPTH, 6 * D), 0.01),
        "norm1_g": 1.0 + nrm((DEPTH, D), 0.02),
        "norm2_g": 1.0 + nrm((DEPTH, D), 0.02),
        "w_in": nrm((DEPTH, D, IN_WIDTH), D ** -0.5),
        "q_norm_g": 1.0 + nrm((DEPTH, HEAD_DIM), 0.02),
        "k_norm_g": 1.0 + nrm((DEPTH, HEAD_DIM), 0.02),
        "attn_sink": nrm((DEPTH, N_HEADS), 0.5),
        "hy_conv_w": nrm((DEPTH, SHORT_CONV, 1, (HYENA_ORDER + 1) * W), SHORT_CONV ** -0.5),
        "hy_conv_b": nrm((DEPTH, (HYENA_ORDER + 1) * W), 0.02),
        "hy_filt_w1": nrm((DEPTH, FILTER_EMB, FILTER_HIDDEN), FILTER_EMB ** -0.5),
        "hy_filt_b1": nrm((DEPTH, FILTER_HIDDEN), 0.1),
        "hy_filt_freq1": 1.0 + nrm((DEPTH, FILTER_HIDDEN), 0.02),
        "hy_filt_w2": nrm((DEPTH, FILTER_HIDDEN, FILTER_HIDDEN), FILTER_HIDDEN ** -0.5),
        "hy_filt_b2": nrm((DEPTH, FILTER_HIDDEN), 0.1),
        "hy_filt_freq2": 1.0 + nrm((DEPTH, FILTER_HIDDEN), 0.02),
        "hy_filt_w3": nrm((DEPTH, FILTER_HIDDEN, 2 * HYENA_ORDER * W), FILTER_HIDDEN ** -0.5),
        "hy_bias": nrm((DEPTH, HYENA_ORDER, W)),
        "w_proj_attn": nrm((DEPTH, ATTN_WIDTH, D), ATTN_WIDTH ** -0.5),
        "w_proj_hyena": nrm((DEPTH, W, D), W ** -0.5),
        "w_proj_fnet": nrm((DEPTH, FNET_WIDTH, D), FNET_WIDTH ** -0.5),
        "w_out": nrm((DEPTH, D, D), D ** -0.5),
        "ffn_w_gate": nrm((n_dense, D, F), D ** -0.5),
        "ffn_w_up": nrm((n_dense, D, F), D ** -0.5),
        "ffn_w_down": nrm((n_dense, F, D), F ** -0.5),
        "moe_router": nrm((n_moe, D, E), D ** -0.5),
        "moe_w_gate": nrm((n_moe, E, D, F), D ** -0.5),
        "moe_w_up": nrm((n_moe, E, D, F), D ** -0.5),
        "moe_w_down": nrm((n_moe, E, F, D), F ** -0.5),
    }


def reference(x, c, ctx, c_ctx, w_ada, b_ada, norm1_g, norm2_g, w_in, q_norm_g, k_norm_g,
              attn_sink, hy_conv_w, hy_conv_b, hy_filt_w1, hy_filt_b1, hy_filt_freq1,
              hy_filt_w2, hy_filt_b2, hy_filt_freq2, hy_filt_w3, hy_bias, w_proj_attn,
              w_proj_hyena, w_proj_fnet, w_out, ffn_w_gate, ffn_w_up, ffn_w_down,
              moe_router, moe_w_gate, moe_w_up, moe_w_down):
    L = x.shape[1]
    C = ctx.shape[1]
    cos, sin = axial_rope_tables(L)
    cond_ctx = c_ctx[None, :]
    for l in range(DEPTH):
        last = l == DEPTH - 1
        filt = (hy_filt_w1[l], hy_filt_b1[l], hy_filt_freq1[l], hy_filt_w2[l], hy_filt_b2[l],
                hy_filt_freq2[l], hy_filt_w3[l])
        proj = (w_proj_attn[l], w_proj_hyena[l], w_proj_fnet[l], w_out[l])

        sh, sc, gt = adaln(c, w_ada[l], b_ada[l], 0)
        sh_c, sc_c, gt_c = adaln(cond_ctx, w_ada[l], b_ada[l], 0)
        h = modulate(x, norm1_g[l], sh, sc)
        h_c = modulate(ctx, norm1_g[l], sh_c, sc_c)

        pr = h @ w_in[l]
        q = apply_axial_rope(q_heads(pr, q_norm_g[l]), cos, sin)
        k, v = kv_heads(pr[..., Q_END:V_END], k_norm_g[l])
        k = apply_axial_rope(k, cos, sin)
        if last:
            k_c, v_c = kv_heads(h_c @ w_in[l][:, Q_END:V_END], k_norm_g[l])
        else:
            pr_c = h_c @ w_in[l]
            q_c = q_heads(pr_c, q_norm_g[l])
            k_c, v_c = kv_heads(pr_c[..., Q_END:V_END], k_norm_g[l])

        attn_o = latent_window_attention(q, k, v, k_c, v_c, attn_sink[l])
        hy_o = hyena_mixer(pr[..., V_END:HY_END], hy_conv_w[l], hy_conv_b[l],
                           hyena_filter_spectrum(L, *filt), hy_bias[l])
        fn_o = fnet_mixer(pr[..., HY_END:FN_END])
        y = merge_branches(pr, attn_o, hy_o, fn_o, *proj)
        x = x + gt[:, None, :] * y

        if not last:
            attn_c = context_attention(q_c, k_c, v_c, attn_sink[l])
            hy_c = hyena_mixer(pr_c[..., V_END:HY_END], hy_conv_w[l], hy_conv_b[l],
                               hyena_filter_spectrum(C, *filt), hy_bias[l])
            fn_c = fnet_mixer(pr_c[..., HY_END:FN_END])
            ctx = ctx + gt_c[:, None, :] * merge_branches(pr_c, attn_c, hy_c, fn_c, *proj)

        sh2, sc2, gt2 = adaln(c, w_ada[l], b_ada[l], 1)
        if l % 2 == 0:
            i = l // 2
            ffn = functools_partial_dense = (lambda t, i=i: swiglu(t, ffn_w_gate[i], ffn_w_up[i], ffn_w_down[i]))
        else:
            i = l // 2
            ffn = (lambda t, i=i: moe_swiglu(t, moe_router[i], moe_w_gate[i], moe_w_up[i],
                                            moe_w_down[i]))
        x = x + gt2[:, None, :] * ffn(modulate(x, norm2_g[l], sh2, sc2))
        if not last:
            sh2_c, sc2_c, gt2_c = adaln(cond_ctx, w_ada[l], b_ada[l], 1)
            ctx = ctx + gt2_c[:, None, :] * ffn(modulate(ctx, norm2_g[l], sh2_c, sc2_c))
    return x
```

```python
import contextlib
import numpy as np
import concourse.bass as bass
import concourse.mybir as mybir
from concourse.bass_utils import run_bass_kernel_spmd

F32 = mybir.dt.float32
BF16 = mybir.dt.bfloat16
ALU = mybir.AluOpType
ACT = mybir.ActivationFunctionType
AX = mybir.AxisListType
NPOOL = 12


class Buf:
    def __init__(self, t):
        self.t = t
        self.w = None
        self.r = {}

    def __getitem__(self, k):
        return self.t[k]


class MK:
    def __init__(self, nc):
        self.nc = nc
        self.es = contextlib.ExitStack()
        self.sems = {}
        self.engs = {}
        for name in ["tensor", "vector", "scalar", "gpsimd", "sync"]:
            sem = self.es.enter_context(nc.semaphore("s_" + name))
            self.sems[name] = sem
            self.engs[name] = dict(key=name, cnt=0, seen={}, ops=[])
        self.pool = {}
        self.rr = {}
        for q in ["sync", "gpsimd", "scalar"]:
            self.pool[q] = []
            for i in range(NPOOL):
                key = "d_%s%d" % (q, i)
                self.sems[key] = self.es.enter_context(nc.semaphore(key))
                self.pool[q].append(dict(key=key, val=0))
            self.rr[q] = 0
        self.nbuf = 0

    def sb(self, shape, dtype, name=None):
        self.nbuf += 1
        t = self.es.enter_context(self.nc.sbuf_tensor(name or ("sb%d" % self.nbuf), list(shape), dtype))
        return Buf(t)

    def ps(self, shape, dtype, name=None):
        self.nbuf += 1
        t = self.es.enter_context(self.nc.psum_tensor(name or ("ps%d" % self.nbuf), list(shape), dtype))
        return Buf(t)

    def dram_in(self, name, shape, dtype=F32):
        return Buf(self.nc.dram_tensor(name, list(shape), dtype, kind="ExternalInput").ap())

    def dram_out(self, name, shape, dtype=F32):
        return Buf(self.nc.dram_tensor(name, list(shape), dtype, kind="ExternalOutput").ap())

    def dram_tmp(self, name, shape, dtype=F32):
        return Buf(self.nc.dram_tensor(name, list(shape), dtype, kind="Internal").ap())

    def _deps(self, E, reads, writes, skip_self):
        deps = {}

        def need(ev):
            if ev is None:
                return
            k, v = ev
            if deps.get(k, 0) < v:
                deps[k] = v

        for b in reads:
            need(b.w)
        for b in writes:
            need(b.w)
            for k, v in b.r.items():
                need((k, v))
        waits = []
        for k, v in deps.items():
            if skip_self and k == E["key"]:
                continue
            if E["seen"].get(k, 0) >= v:
                continue
            E["seen"][k] = v
            waits.append((k, v))
        return waits

    def _mark(self, ev, reads, writes):
        k, v = ev
        for b in reads:
            if b.r.get(k, 0) < v:
                b.r[k] = v
        for b in writes:
            b.w = ev
            b.r = {}

    def op(self, en, fn, reads=(), writes=()):
        E = self.engs[en]
        waits = self._deps(E, reads, writes, skip_self=(en == "tensor"))
        E["cnt"] += 1
        ev = (E["key"], E["cnt"])
        if isinstance(fn, tuple):
            nm, kw = fn
            fn = (lambda e, nm=nm, kw=kw: getattr(e, nm)(**kw))
        E["ops"].append((waits, fn, (E["key"], 1)))
        self._mark(ev, reads, writes)

    def dma(self, q, out, in_, reads=(), writes=(), **kw):
        E = self.engs[q]
        p = self.pool[q][self.rr[q] % NPOOL]
        self.rr[q] += 1
        waits = self._deps(E, reads, writes, skip_self=False)
        if p["val"] > 0 and E["seen"].get(p["key"], 0) < p["val"]:
            E["seen"][p["key"]] = p["val"]
            waits.append((p["key"], p["val"]))
        p["val"] += 16
        ev = (p["key"], p["val"])
        E["ops"].append((waits, lambda e, out=out, in_=in_, kw=kw: e.dma_start(out=out, in_=in_, **kw), (p["key"], 16)))
        self._mark(ev, reads, writes)

    def finish(self):
        E = self.engs["sync"]
        waits = []
        for q in self.pool:
            for p in self.pool[q]:
                if p["val"] > 0:
                    waits.append((p["key"], p["val"]))
        for name in ["tensor", "vector", "scalar", "gpsimd"]:
            c = self.engs[name]["cnt"]
            if c > 0:
                waits.append((name, c))
        E["ops"].append((waits, None, None))

    def emit(self):
        nc = self.nc
        sems = self.sems
        engs = self.engs

        def run(e, name):
            for waits, fn, inc in engs[name]["ops"]:
                for k, v in waits:
                    e.wait_ge(sems[k], v)
                if fn is None:
                    continue
                ins = fn(e)
                ins.then_inc(sems[inc[0]], inc[1])

        with nc.Block() as block:
            @block.sync
            def _(e):
                run(e, "sync")

            @block.tensor
            def _(e):
                run(e, "tensor")

            @block.vector
            def _(e):
                run(e, "vector")

            @block.scalar
            def _(e):
                run(e, "scalar")

            @block.gpsimd
            def _(e):
                run(e, "gpsimd")
        self.es.close()

    def mm(self, out, lhsT, rhs, start, stop, reads, writes):
        self.op("tensor", lambda e, out=out, lhsT=lhsT, rhs=rhs, start=start, stop=stop: e.matmul(out, lhsT, rhs, start=start, stop=stop),
                reads=reads, writes=writes)

    def tr(self, out, in_, ident, reads, writes):
        self.op("tensor", lambda e, out=out, in_=in_, ident=ident: e.transpose(out, in_, ident), reads=reads, writes=writes)
import math

D = 1024
NLAT = 16
NT = 18
ROWS = NT * 128
EPS = 1e-6


def emit_adaln(mk, condT_d, w_ada_d, b_ada_d, c0, c1, psb, wa_views=None):
    n = c1 - c0
    ct = mk.sb([128, 16], F32)
    mk.dma("sync", ct[:], condT_d[:], reads=[condT_d], writes=[ct])
    sc = mk.sb([128, 16], F32)
    mk.op("scalar", ("activation", dict(out=sc[:], in_=ct[:], func=ACT.Silu)), reads=[ct], writes=[sc])
    LB = mk.sb([128, 16, 128], BF16)
    mk.op("vector", ("tensor_copy", dict(out=LB[:], in_=sc[:, :].unsqueeze(2).broadcast_to([128, 16, 128]))),
          reads=[sc], writes=[LB])
    bbs = [mk.sb([128, 512], F32), mk.sb([128, 512], F32)]
    mods = [mk.sb([128, n], F32), mk.sb([128, n], F32)]
    if wa_views is None:
        w0, w1 = mk.sb([128, 8, 512], BF16), mk.sb([128, 8, 512], BF16)
        wa_views = [(w0, w0[:, :, :]), (w1, w1[:, :, :])]
    wv = w_ada_d.t.rearrange("(c p) n -> p c n", p=128)
    for nb in range(n // 512):
        wa, wap = wa_views[nb % 2]
        bb = bbs[nb % 2]
        mk.dma("sync", bb[:], b_ada_d[c0 + nb * 512:c0 + (nb + 1) * 512].partition_broadcast(128), reads=[b_ada_d], writes=[bb])
        mk.dma("gpsimd", wap, wv[:, :, c0 + nb * 512:c0 + (nb + 1) * 512], reads=[w_ada_d], writes=[wa])
        for r in range(2):
            for c in range(8):
                mk.mm(psb[:], LB[:, r * 8 + c, :], wap[:, c, :], c == 0, c == 7, reads=[LB, wa], writes=[psb])
            mk.op("vector", ("tensor_tensor", dict(out=mods[r][:, nb * 512:(nb + 1) * 512], in0=psb[:], in1=bb[:], op=ALU.add)),
                  reads=[psb, bb], writes=[mods[r]])
    return mods


def emit_modulate_setup(mk, mods, g_d, sh_off, sc_off):
    gb = mk.sb([128, D], F32)
    mk.dma("sync", gb[:], g_d[:].partition_broadcast(128), reads=[g_d], writes=[gb])
    for r in range(2):
        m = mods[r]
        mk.op("vector", ("scalar_tensor_tensor", dict(out=m[:, sc_off:sc_off + D], in0=m[:, sc_off:sc_off + D], scalar=1.0,
                                                               in1=gb[:], op0=ALU.add, op1=ALU.mult)),
              reads=[m, gb], writes=[m])


def emit_norm_mod_T(mk, xt, m, sh_off, sc_off, ss, rs, col, epsb, hf, hb, PT, hT, identb, hT_ap=None):
    mk.op("scalar", ("activation", dict(out=hf[:], in_=xt[:], func=ACT.Square, accum_out=ss[:, col:col + 1])),
          reads=[xt], writes=[hf, ss])
    mk.op("scalar", ("activation", dict(out=rs[:, col:col + 1], in_=ss[:, col:col + 1], func=ACT.Sqrt, scale=1.0 / D, bias=epsb[:, 0:1])),
          reads=[ss, epsb], writes=[rs])
    mk.op("vector", ("reciprocal", dict(out=rs[:, col:col + 1], in_=rs[:, col:col + 1])), reads=[rs], writes=[rs])
    mk.op("vector", ("scalar_tensor_tensor", dict(out=hf[:], in0=xt[:], scalar=rs[:, col:col + 1], in1=m[:, sc_off:sc_off + D],
                                                     op0=ALU.mult, op1=ALU.mult)), reads=[xt, rs, m], writes=[hf])
    mk.op("vector", ("tensor_tensor", dict(out=hb[:], in0=hf[:], in1=m[:, sh_off:sh_off + D], op=ALU.add)),
          reads=[hf, m], writes=[hb])
    for c in range(8):
        mk.tr(PT[:, c, :], hb[:, c * 128:(c + 1) * 128], identb[:], reads=[hb, identb], writes=[PT])
    mk.op("scalar", ("copy", dict(out=(hT[:] if hT_ap is None else hT_ap), in_=PT[:])), reads=[PT], writes=[hT])


def emit_headnorm_rope(mk, src_ap, src_buf, nh, gain_b, cs_c, cs_s, tabs, sqb, qs, qn, qo, epsb):
    w = nh * 64
    mk.op("scalar", ("activation", dict(out=sqb[:, 0:w], in_=src_ap, func=ACT.Square)), reads=[src_buf], writes=[sqb])
    mk.op("vector", ("tensor_reduce", dict(out=qs[:, 0:nh], in_=sqb[:, 0:w].rearrange("p (h d) -> p h d", h=nh), axis=AX.X, op=ALU.add)),
          reads=[sqb], writes=[qs])
    mk.op("scalar", ("activation", dict(out=qs[:, 0:nh], in_=qs[:, 0:nh], func=ACT.Sqrt, scale=1.0 / 64, bias=epsb[:, 0:1])),
          reads=[qs, epsb], writes=[qs])
    mk.op("vector", ("reciprocal", dict(out=qs[:, 0:nh], in_=qs[:, 0:nh])), reads=[qs], writes=[qs])
    qn3 = qn[:, 0:w].rearrange("p (h d) -> p h d", h=nh)
    mk.op("vector", ("tensor_tensor", dict(out=qn3, in0=src_ap.rearrange("p (h d) -> p h d", h=nh),
                                              in1=qs[:, 0:nh].unsqueeze(2).broadcast_to([128, nh, 64]), op=ALU.mult)),
          reads=[src_buf, qs], writes=[qn])
    mk.op("vector", ("tensor_tensor", dict(out=qn3, in0=qn3, in1=gain_b[:, :].unsqueeze(1).broadcast_to([128, nh, 64]), op=ALU.mult)),
          reads=[qn, gain_b], writes=[qn])
    v5 = qn[:, 0:w].rearrange("p (h a j f) -> p h a j f", h=nh, a=2, j=2)
    o5 = qo[:, 0:w].rearrange("p (h a j f) -> p h a j f", h=nh, a=2, j=2)
    t5 = sqb[:, 0:w].rearrange("p (h a j f) -> p h a j f", h=nh, a=2, j=2)
    x1, x2 = v5[:, :, :, 0, :], v5[:, :, :, 1, :]
    cb = cs_c.unsqueeze(1).broadcast_to([128, nh, 2, 16])
    sb_ = cs_s.unsqueeze(1).broadcast_to([128, nh, 2, 16])
    rd = [qn]
    mk.op("vector", ("tensor_tensor", dict(out=t5[:, :, :, 0, :], in0=x1, in1=cb, op=ALU.mult)), reads=rd + tabs, writes=[sqb])
    mk.op("vector", ("tensor_tensor", dict(out=t5[:, :, :, 1, :], in0=x2, in1=sb_, op=ALU.mult)), reads=rd + tabs, writes=[sqb])
    mk.op("vector", ("tensor_tensor", dict(out=o5[:, :, :, 0, :], in0=t5[:, :, :, 0, :], in1=t5[:, :, :, 1, :], op=ALU.subtract)),
          reads=[sqb], writes=[qo])
    mk.op("vector", ("tensor_tensor", dict(out=t5[:, :, :, 0, :], in0=x1, in1=sb_, op=ALU.mult)), reads=rd + tabs, writes=[sqb])
    mk.op("vector", ("tensor_tensor", dict(out=t5[:, :, :, 1, :], in0=x2, in1=cb, op=ALU.mult)), reads=rd + tabs, writes=[sqb])
    mk.op("vector", ("tensor_tensor", dict(out=o5[:, :, :, 1, :], in0=t5[:, :, :, 0, :], in1=t5[:, :, :, 1, :], op=ALU.add)),
          reads=[sqb], writes=[qo])


def build_p1(NT=NT, NLAT=NLAT):
    ROWS = NT * 128
    nc = bass.Bass("TRN2", target_bir_lowering=False)
    mk = MK(nc)
    xs = mk.dram_in("xs", [ROWS, D])
    condT = mk.dram_in("condT", [128, 16])
    w_ada = mk.dram_in("w_ada", [D, 6 * D])
    b_ada = mk.dram_in("b_ada", [6 * D])
    n1g = mk.dram_in("n1g", [D])
    w_in = mk.dram_in("w_in", [D, 4864])
    qg = mk.dram_in("qg", [64])
    kg = mk.dram_in("kg", [64])
    ropec = mk.dram_in("ropec", [ROWS, 32])
    ropes = mk.dram_in("ropes", [ROWS, 32])
    f64 = mk.dram_in("f64", [128, 256])
    ident = mk.dram_in("ident", [128, 128])
    q_o = mk.dram_out("q_o", [ROWS, 512])
    kv_o = mk.dram_out("kv_o", [ROWS, 256])
    hy_o = mk.dram_out("hy_o", [ROWS, 768])
    fnv_o = mk.dram_out("fnv_o", [ROWS, 512])

    psA = mk.ps([128, 512], F32)
    psB = mk.ps([128, 512], F32)
    psC = mk.ps([128, 512], F32)
    psF = mk.ps([128, 2, 128], F32)
    psV = mk.ps([128, 512], F32)
    PT = mk.ps([128, 8, 128], BF16)

    identb = mk.sb([128, 128], BF16)
    mk.dma("gpsimd", identb[:], ident[:], reads=[ident], writes=[identb])
    f64b = mk.sb([128, 256], BF16)
    mk.dma("gpsimd", f64b[:], f64[:], reads=[f64], writes=[f64b])
    epsb = mk.sb([128, 1], F32)
    mk.op("vector", ("memset", dict(ap=epsb[:], constant=EPS)), writes=[epsb])
    ss = mk.sb([128, NT], F32)
    rs = mk.sb([128, NT], F32)
    mk.op("vector", ("memset", dict(ap=ss[:], constant=0.0)), writes=[ss])
    qgb = mk.sb([128, 64], F32)
    kgb = mk.sb([128, 64], F32)
    mk.dma("sync", qgb[:], qg[:].partition_broadcast(128), reads=[qg], writes=[qgb])
    mk.dma("sync", kgb[:], kg[:].partition_broadcast(128), reads=[kg], writes=[kgb])
    rc = mk.sb([128, NT, 32], F32)
    rsn = mk.sb([128, NT, 32], F32)
    mk.dma("sync", rc[:], ropec.t.rearrange("(t p) f -> p t f", p=128), reads=[ropec], writes=[rc])
    mk.dma("sync", rsn[:], ropes.t.rearrange("(t p) f -> p t f", p=128), reads=[ropes], writes=[rsn])

    mods = emit_adaln(mk, condT, w_ada, b_ada, 0, 2048, psA)
    emit_modulate_setup(mk, mods, n1g, 0, 1024)

    WB = mk.sb([128, 8, 1792], BF16)
    wv = w_in.t.rearrange("(c p) n -> p c n", p=128)
    for c in range(8):
        mk.dma("gpsimd", WB[:, c, :], wv[:, c, 0:1792], reads=[w_in], writes=[WB])

    xts = [mk.sb([128, D], F32), mk.sb([128, D], F32)]
    hf = mk.sb([128, D], F32)
    hb = mk.sb([128, D], BF16)
    hTs = [mk.sb([128, 8, 128], BF16), mk.sb([128, 8, 128], BF16)]
    sqb = mk.sb([128, 512], F32)
    qs = mk.sb([128, 8], F32)
    qn = mk.sb([128, 512], F32)
    qos = [mk.sb([128, 512], F32), mk.sb([128, 512], F32)]
    kvos = [mk.sb([128, 256], F32), mk.sb([128, 256], F32)]
    hyos = [mk.sb([128, 768], F32), mk.sb([128, 768], F32)]
    fnT = mk.sb([128, 2, 128], BF16)
    fvs = [mk.sb([128, 512], F32), mk.sb([128, 512], F32)]

    for t in range(NT):
        r = 0 if t < NLAT else 1
        xt = xts[t % 2]
        hT = hTs[t % 2]
        qo, kvo, hyo, fv = qos[t % 2], kvos[t % 2], hyos[t % 2], fvs[t % 2]
        rows = slice(t * 128, (t + 1) * 128)
        mk.dma("sync", xt[:], xs[rows, :], reads=[xs], writes=[xt])
        emit_norm_mod_T(mk, xt, mods[r], 0, 1024, ss, rs, t, epsb, hf, hb, PT, hT, identb)
        for (pb, c0) in ((psA, 0), (psB, 512), (psC, 1024)):
            for c in range(8):
                mk.mm(pb[:], hT[:, c, :], WB[:, c, c0:c0 + 512], c == 0, c == 7, reads=[hT, WB], writes=[pb])
        for blk in range(2):
            for c in range(8):
                mk.mm(psF[:, blk, :], WB[:, c, 1536 + blk * 128:1536 + (blk + 1) * 128], hT[:, c, :], c == 0, c == 7,
                      reads=[hT, WB], writes=[psF])
        mk.op("scalar", ("copy", dict(out=fnT[:], in_=psF[:])), reads=[psF], writes=[fnT])
        for blk in range(2):
            mk.mm(psV[:, blk * 256:(blk + 1) * 256], fnT[:, blk, :], f64b[:, :], True, True, reads=[fnT, f64b], writes=[psV])
        mk.op("scalar", ("copy", dict(out=fv[:], in_=psV[:])), reads=[psV], writes=[fv])
        mk.dma("sync", fnv_o[rows, :], fv[:], reads=[fv], writes=[fnv_o])
        emit_headnorm_rope(mk, psA[:, :], psA, 8, qgb, rc[:, t, :].rearrange("p (a f) -> p a f", a=2),
                           rsn[:, t, :].rearrange("p (a f) -> p a f", a=2), [rc, rsn], sqb, qs, qn, qo, epsb)
        mk.dma("sync", q_o[rows, :], qo[:], reads=[qo], writes=[q_o])
        emit_headnorm_rope(mk, psB[:, 0:128], psB, 2, kgb, rc[:, t, :].rearrange("p (a f) -> p a f", a=2),
                           rsn[:, t, :].rearrange("p (a f) -> p a f", a=2), [rc, rsn], sqb, qs, qn, kvo, epsb)
        mk.op("scalar", ("copy", dict(out=kvo[:, 128:256], in_=psB[:, 128:256])), reads=[psB], writes=[kvo])
        mk.dma("sync", kv_o[rows, :], kvo[:], reads=[kvo], writes=[kv_o])
        mk.op("scalar", ("copy", dict(out=hyo[:, 0:256], in_=psB[:, 256:512])), reads=[psB], writes=[hyo])
        mk.op("scalar", ("copy", dict(out=hyo[:, 256:768], in_=psC[:, :])), reads=[psC], writes=[hyo])
        mk.dma("sync", hy_o[rows, :], hyo[:], reads=[hyo], writes=[hy_o])
    mk.finish()
    mk.emit()
    return nc

TWO_PI = 2.0 * math.pi


def _p2_bufs(mk):
    B = {}
    B["uh"] = [mk.sb([128, 130, 32], F32) for _ in range(2)]
    B["uc"] = [mk.sb([128, 128, 32], F32) for _ in range(3)]
    B["tmp"] = mk.sb([128, 128 * 32], F32)
    B["zb"] = mk.sb([128, 128, 32], BF16)
    B["cr"] = mk.sb([128, 4096], BF16)
    B["ci"] = mk.sb([128, 4096], BF16)
    B["bb"] = mk.sb([128, 2, 4096], BF16)
    B["h2t"] = mk.sb([64, 16384], BF16)
    B["ta"] = mk.sb([128, 512], F32)
    B["tb"] = mk.sb([128, 512], F32)
    B["zt"] = [mk.sb([33, 512], F32), mk.sb([33, 512], F32)]
    B["tiq"] = mk.sb([64, 512], mybir.dt.int32)
    B["h1"] = mk.sb([64, 512], F32)
    B["dec"] = [mk.sb([128, 8, 32], F32), mk.sb([128, 8, 32], F32)]
    B["deci"] = 0
    B["asum"] = mk.sb([128, 32], F32)
    B["rn"] = mk.sb([128, 32], F32)
    B["ps"] = [mk.ps([128, 512], F32) for _ in range(6)]
    B["psi"] = 0
    return B


def _nps(B):
    p = B["ps"][B["psi"] % len(B["ps"])]
    B["psi"] += 1
    return p


def _cmul_psum(mk, B, pr_ap, pi_ap, pbuf, tr_ap, ti_ap, tbufs, outr_ap, outi_ap, outr_buf, outi_buf, shape_view, conj=False):
    ta, tb = B["ta"], B["tb"]
    va, vb = shape_view(ta), shape_view(tb)
    mk.op("vector", ("tensor_tensor", dict(out=va, in0=pr_ap, in1=tr_ap, op=ALU.mult)), reads=[pbuf] + tbufs, writes=[ta])
    mk.op("vector", ("tensor_tensor", dict(out=vb, in0=pi_ap, in1=ti_ap, op=ALU.mult)), reads=[pbuf] + tbufs, writes=[tb])
    mk.op("vector", ("tensor_tensor", dict(out=outr_ap, in0=va, in1=vb, op=(ALU.add if conj else ALU.subtract))),
          reads=[ta, tb], writes=[outr_buf])
    mk.op("vector", ("tensor_tensor", dict(out=va, in0=pr_ap, in1=ti_ap, op=ALU.mult)), reads=[pbuf] + tbufs, writes=[ta])
    mk.op("vector", ("tensor_tensor", dict(out=vb, in0=pi_ap, in1=tr_ap, op=ALU.mult)), reads=[pbuf] + tbufs, writes=[tb])
    if conj:
        mk.op("vector", ("tensor_tensor", dict(out=outi_ap, in0=vb, in1=va, op=ALU.subtract)), reads=[ta, tb], writes=[outi_buf])
    else:
        mk.op("vector", ("tensor_tensor", dict(out=outi_ap, in0=va, in1=vb, op=ALU.add)), reads=[ta, tb], writes=[outi_buf])


def emit_fft_fwd(mk, B, M, src, nch, RAb, TW, WC, w):
    cr, ci = B["cr"], B["ci"]
    per = 512 // (2 * w)
    for c0 in range(0, nch, per):
        pa = _nps(B)
        for j in range(per):
            mk.mm(pa[0:M, j * 2 * w:(j + 1) * 2 * w], src[:, 0:M, c0 + j], RAb[:, :], True, True, reads=[src, RAb], writes=[pa])
        pv = pa[0:M, :].rearrange("p (j r k) -> p j r k", j=per, r=2)
        sv = lambda t: t[0:M, 0:per * w].rearrange("p (j k) -> p j k", j=per)
        trb = TW[0:M, 0, :].unsqueeze(1).broadcast_to([M, per, w])
        tib = TW[0:M, 1, :].unsqueeze(1).broadcast_to([M, per, w])
        crv = cr[0:M, c0 * w:(c0 + per) * w].rearrange("p (j k) -> p j k", j=per)
        civ = ci[0:M, c0 * w:(c0 + per) * w].rearrange("p (j k) -> p j k", j=per)
        _cmul_psum(mk, B, pv[:, :, 0, :], pv[:, :, 1, :], pa, trb, tib, [TW], crv, civ, cr, ci, sv)


def emit_hyena(mk, B, M, tag, d, C):
    L = 64 * M
    NCIRC = 128 * M
    uh, uc, tmp, zb, cr, ci, bb, h2t = B["uh"], B["uc"], B["tmp"], B["zb"], B["cr"], B["ci"], B["bb"], B["h2t"]
    hyu = d["hyu" + tag]
    cw, cb = C["cw"], C["cb"]
    tv = tmp[:, 0:M * 32].rearrange("p (m c) -> p m c", c=32)
    for comp in range(3):
        cs = slice(comp * 32, (comp + 1) * 32)
        uhc = uh[comp % 2]
        for b in range(2):
            ps_ = slice(b * 64, (b + 1) * 64)
            mk.dma("sync", uhc[ps_, 1:M + 1, :], hyu.t[b, 1:L + 1, cs].rearrange("(n m) c -> n m c", m=M), reads=[hyu], writes=[uhc])
            mk.dma("sync", uhc[ps_, 0, :], hyu.t[b, 0:L, cs].rearrange("(n m) c -> n m c", m=M)[:, 0, :], reads=[hyu], writes=[uhc])
            mk.dma("sync", uhc[ps_, M + 1, :], hyu.t[b, 2:L + 2, cs].rearrange("(n m) c -> n m c", m=M)[:, M - 1, :], reads=[hyu], writes=[uhc])
        o = uc[comp][:, 0:M, :]
        wb = lambda k, cs=cs: cw[:, k, cs].unsqueeze(1).broadcast_to([128, M, 32])
        mk.op("vector", ("tensor_tensor", dict(out=o, in0=uhc[:, 0:M, :], in1=wb(0), op=ALU.mult)), reads=[uhc, cw], writes=[uc[comp]])
        mk.op("gpsimd", ("tensor_tensor", dict(out=tv, in0=uhc[:, 1:M + 1, :], in1=wb(1), op=ALU.mult)), reads=[uhc, cw], writes=[tmp])
        mk.op("vector", ("tensor_tensor", dict(out=o, in0=o, in1=tv, op=ALU.add)), reads=[uc[comp], tmp], writes=[uc[comp]])
        mk.op("gpsimd", ("tensor_tensor", dict(out=tv, in0=uhc[:, 2:M + 2, :], in1=wb(2), op=ALU.mult)), reads=[uhc, cw], writes=[tmp])
        mk.op("vector", ("tensor_tensor", dict(out=o, in0=o, in1=tv, op=ALU.add)), reads=[uc[comp], tmp], writes=[uc[comp]])
        mk.op("vector", ("tensor_tensor", dict(out=o, in0=o, in1=cb[:, cs].unsqueeze(1).broadcast_to([128, M, 32]), op=ALU.add)),
              reads=[uc[comp], cb], writes=[uc[comp]])
    zf = d["zfeat" + tag]
    zt_s = B["zt"]
    tq, tiq, tfq = B["ta"], B["tiq"], B["tb"]
    h1 = B["h1"]
    nblk = max(1, NCIRC // 512)
    bw = min(512, NCIRC)
    for blk in range(nblk):
        zt = zt_s[blk % 2]
        mk.dma("sync", zt[:, 0:bw], zf[:, blk * bw:(blk + 1) * bw], reads=[zf], writes=[zt])
        src, srcb = zt, None
        for layer in range(2):
            wl = C["w1"] if layer == 0 else C["w2"]
            kk = 33 if layer == 0 else 64
            p = _nps(B)
            rhs = zt[0:33, 0:bw] if layer == 0 else h1[:, 0:bw]
            rb = zt if layer == 0 else h1
            mk.mm(p[0:64, 0:bw], wl[0:kk, :], rhs, True, True, reads=[wl, rb], writes=[p])
            a_, c_ = C["fa"][:, layer:layer + 1], C["fc"][:, layer:layer + 1]
            mk.op("vector", ("tensor_scalar", dict(out=tq[0:64, 0:bw], in0=p[0:64, 0:bw], scalar1=a_, scalar2=c_, op0=ALU.mult, op1=ALU.add)),
                  reads=[p, C["fa"], C["fc"]], writes=[tq])
            mk.op("vector", ("tensor_copy", dict(out=tiq[:, 0:bw], in_=tq[0:64, 0:bw])), reads=[tq], writes=[tiq])
            mk.op("vector", ("tensor_copy", dict(out=tfq[0:64, 0:bw], in_=tiq[:, 0:bw])), reads=[tiq], writes=[tfq])
            mk.op("vector", ("tensor_tensor", dict(out=tq[0:64, 0:bw], in0=tq[0:64, 0:bw], in1=tfq[0:64, 0:bw], op=ALU.subtract)), reads=[tq, tfq], writes=[tq])
            if layer == 0:
                mk.op("scalar", ("activation", dict(out=h1[:, 0:bw], in_=tq[0:64, 0:bw], func=ACT.Sin, scale=TWO_PI)), reads=[tq], writes=[h1])
            else:
                mk.op("scalar", ("activation", dict(out=h2t[:, blk * bw:(blk + 1) * bw], in_=tq[0:64, 0:bw], func=ACT.Sin, scale=TWO_PI)),
                      reads=[tq], writes=[h2t])
    hr, hi = uh[0], uh[1]
    taps = mk_view_taps = tmp
    hrv = lambda: hr[:, :, :].rearrange("p a b -> p (a b)")[0:M, 0:4096]
    hiv = lambda: hi[:, :, :].rearrange("p a b -> p (a b)")[0:M, 0:4096]
    taps3 = tmp[:, 0:M * 32].rearrange("p (m c) -> p m c", c=32)
    tapv = taps3
    tapsb = zb
    dec_d = d["decay" + tag]
    TW, TWT = C["tw" + tag], C["twt" + tag]
    WCm, ICm = C["wc" + tag], C["ic" + tag]
    h2v = h2t[:, 0:NCIRC].rearrange("p (n m) -> p n m", m=M)
    zsrc = uc[2]
    for o in range(2):
        G = min(8, M)
        for g0 in range(0, M, G):
            p = _nps(B)
            for j in range(G):
                mk.mm(p[:, j * 64:(j + 1) * 64], h2v[:, :, g0 + j], C["w3"][:, o, :], True, True, reads=[h2t, C["w3"]], writes=[p])
            pv = p[:, 0:G * 64].rearrange("p (j r c) -> p j r c", j=G, r=2)
            dec = B["dec"][B["deci"] % 2]
            B["deci"] += 1
            mk.dma("sync", dec[:, 0:G, :], dec_d[:, g0:g0 + G, :], reads=[dec_d], writes=[dec])
            mk.op("vector", ("tensor_tensor", dict(out=taps3[0:64, g0:g0 + G, :], in0=pv[0:64, :, 0, :], in1=dec[0:64, 0:G, :], op=ALU.mult)),
                  reads=[p, dec], writes=[taps])
            mk.op("vector", ("tensor_tensor", dict(out=taps3[64:128, g0:g0 + G, :], in0=pv[64:128, :, 1, :], in1=dec[64:128, 0:G, :], op=ALU.mult)),
                  reads=[p, dec], writes=[taps])
        asum = B["asum"]
        mk.op("vector", ("tensor_reduce", dict(out=asum[:], in_=tapv.rearrange("p m c -> p c m"), axis=AX.X, op=ALU.add, apply_absolute_value=True)),
              reads=[taps], writes=[asum])
        p = _nps(B)
        mk.mm(p[:, 0:32], C["ones"][:, :], asum[:, :], True, True, reads=[C["ones"], asum], writes=[p])
        rn = B["rn"]
        mk.op("vector", ("reciprocal", dict(out=rn[:], in_=p[:, 0:32])), reads=[p], writes=[rn])
        mk.op("vector", ("tensor_copy", dict(out=tapsb[:, 0:M, :], in_=tapv)), reads=[taps], writes=[tapsb])
        emit_fft_fwd(mk, B, M, tapsb, 32, C["rf"], TW, WCm, 128)
        for blk in range(8):
            cs = slice(blk * 512, (blk + 1) * 512)
            pr_, pi_ = _nps(B), _nps(B)
            mk.mm(pr_[0:M, :], WCm[0:M, 0, :], cr[0:M, cs], True, False, reads=[WCm, cr], writes=[pr_])
            mk.mm(pr_[0:M, :], WCm[0:M, 2, :], ci[0:M, cs], False, True, reads=[WCm, ci], writes=[pr_])
            mk.mm(pi_[0:M, :], WCm[0:M, 1, :], cr[0:M, cs], True, False, reads=[WCm, cr], writes=[pi_])
            mk.mm(pi_[0:M, :], WCm[0:M, 0, :], ci[0:M, cs], False, True, reads=[WCm, ci], writes=[pi_])
            rnb = rn[0:M, blk * 4:(blk + 1) * 4].unsqueeze(2).broadcast_to([M, 4, 128])
            mk.op("vector", ("tensor_tensor", dict(out=hrv()[:, cs].rearrange("p (j k) -> p j k", j=4),
                                                                              in0=pr_[0:M, :].rearrange("p (j k) -> p j k", j=4), in1=rnb, op=ALU.mult)),
                  reads=[pr_, rn], writes=[hr])
            mk.op("vector", ("tensor_tensor", dict(out=hiv()[:, cs].rearrange("p (j k) -> p j k", j=4),
                                                                              in0=pi_[0:M, :].rearrange("p (j k) -> p j k", j=4), in1=rnb, op=ALU.mult)),
                  reads=[pi_, rn], writes=[hi])
        zsv = zsrc[:, 0:M, :]
        mk.op("vector", ("tensor_copy", dict(out=zb[:, 0:M, :], in_=zsv)), reads=[zsrc], writes=[zb])
        emit_fft_fwd(mk, B, M, zb, 32, C["ra"], TW, WCm, 128)
        for blk in range(8):
            cs = slice(blk * 512, (blk + 1) * 512)
            pr_, pi_ = _nps(B), _nps(B)
            mk.mm(pr_[0:M, :], WCm[0:M, 0, :], cr[0:M, cs], True, False, reads=[WCm, cr], writes=[pr_])
            mk.mm(pr_[0:M, :], WCm[0:M, 2, :], ci[0:M, cs], False, True, reads=[WCm, ci], writes=[pr_])
            mk.mm(pi_[0:M, :], WCm[0:M, 1, :], cr[0:M, cs], True, False, reads=[WCm, cr], writes=[pi_])
            mk.mm(pi_[0:M, :], WCm[0:M, 0, :], ci[0:M, cs], False, True, reads=[WCm, ci], writes=[pi_])
            ta, tb = B["ta"], B["tb"]
            mk.op("vector", ("tensor_tensor", dict(out=ta[0:M, :], in0=pr_[0:M, :], in1=hrv()[:, cs], op=ALU.mult)), reads=[pr_, hr], writes=[ta])
            mk.op("vector", ("tensor_tensor", dict(out=tb[0:M, :], in0=pi_[0:M, :], in1=hiv()[:, cs], op=ALU.mult)), reads=[pi_, hi], writes=[tb])
            mk.op("vector", ("tensor_tensor", dict(out=cr[0:M, cs], in0=ta[0:M, :], in1=tb[0:M, :], op=ALU.subtract)), reads=[ta, tb], writes=[cr])
            mk.op("vector", ("tensor_tensor", dict(out=ta[0:M, :], in0=pr_[0:M, :], in1=hiv()[:, cs], op=ALU.mult)), reads=[pr_, hi], writes=[ta])
            mk.op("vector", ("tensor_tensor", dict(out=tb[0:M, :], in0=pi_[0:M, :], in1=hrv()[:, cs], op=ALU.mult)), reads=[pi_, hr], writes=[tb])
            mk.op("vector", ("tensor_tensor", dict(out=ci[0:M, cs], in0=ta[0:M, :], in1=tb[0:M, :], op=ALU.add)), reads=[ta, tb], writes=[ci])
        per = min(32, 512 // (2 * M))
        for c0 in range(0, 32, per):
            pb_ = _nps(B)
            for j in range(per):
                ch = c0 + j
                osl = pb_[:, j * 2 * M:(j + 1) * 2 * M]
                mk.mm(osl, cr[0:M, ch * 128:(ch + 1) * 128], ICm[0:M, 0, :], True, False, reads=[cr, ICm], writes=[pb_])
                mk.mm(osl, ci[0:M, ch * 128:(ch + 1) * 128], ICm[0:M, 1, :], False, True, reads=[ci, ICm], writes=[pb_])
            pv = pb_[:, 0:per * 2 * M].rearrange("p (j r k) -> p j r k", j=per, r=2)
            sv = lambda t: t[:, 0:per * M].rearrange("p (j k) -> p j k", j=per)
            trb = TWT[:, 0, :].unsqueeze(1).broadcast_to([128, per, M])
            tib = TWT[:, 1, :].unsqueeze(1).broadcast_to([128, per, M])
            brv = bb[:, 0, c0 * M:(c0 + per) * M].rearrange("p (j k) -> p j k", j=per)
            biv = bb[:, 1, c0 * M:(c0 + per) * M].rearrange("p (j k) -> p j k", j=per)
            _cmul_psum(mk, B, pv[:, :, 0, :], pv[:, :, 1, :], pb_, trb, tib, [TWT], brv, biv, bb, bb, sv, conj=True)
        tot = 32 * M
        bwid = min(512, tot)
        cpb = bwid // M
        for blk in range(tot // bwid):
            po = _nps(B)
            cs = slice(blk * bwid, (blk + 1) * bwid)
            mk.mm(po[:, 0:bwid], C["l1"][:, :], bb[:, 0, cs], True, False, reads=[C["l1"], bb], writes=[po])
            mk.mm(po[:, 0:bwid], C["l2"][:, :], bb[:, 1, cs], False, True, reads=[C["l2"], bb], writes=[po])
            ov = tmp[:, 0:M * 32].rearrange("p (m c) -> p m c", c=32)[:, :, blk * cpb:(blk + 1) * cpb].rearrange("p m c -> p c m")
            mk.op("scalar", ("activation", dict(out=ov, in_=po[:, 0:bwid].rearrange("p (c m) -> p c m", m=M), func=ACT.Copy, scale=1.0 / NCIRC)),
                  reads=[po], writes=[tmp])
        zn = zsv
        hbv = C["hb"][:, o, :].unsqueeze(1).broadcast_to([128, M, 32])
        mk.op("vector", ("tensor_tensor", dict(out=zn, in0=zn, in1=hbv, op=ALU.mult)), reads=[zsrc, C["hb"]], writes=[zsrc])
        mk.op("vector", ("tensor_tensor", dict(out=zn, in0=zn, in1=tv, op=ALU.add)), reads=[zsrc, tmp], writes=[zsrc])
        mk.op("vector", ("tensor_tensor", dict(out=zn, in0=zn, in1=uc[o][:, 0:M, :], op=ALU.mult)), reads=[zsrc, uc[o]], writes=[zsrc])
    hyo = d["hyo" + tag]
    for b in range(2):
        mk.dma("sync", hyo.t[b].rearrange("(n m) c -> n m c", m=M), uc[2][b * 64:(b + 1) * 64, 0:M, :], reads=[uc[2]], writes=[hyo])


def emit_fnet(mk, B, M, tag, d, C):
    L = 64 * M
    cr, ci, bb, tmp = B["cr"], B["ci"], B["bb"], B["tmp"]
    fnv = d["fnv" + tag]
    V = bb
    Vv = bb[:, :, :].rearrange("p a b -> p (a b)")[:, 0:M * 64].rearrange("p (m c) -> p m c", c=64)
    mk.dma("gpsimd", Vv[0:64], fnv.t[:, 0:64].rearrange("(n m) c -> n m c", m=M), reads=[fnv], writes=[bb])
    mk.dma("gpsimd", Vv[64:128], fnv.t[:, 64:128].rearrange("(n m) c -> n m c", m=M), reads=[fnv], writes=[bb])
    TW, WCm = C["twf" + tag], C["wc" + tag]

    per = 4
    for c0 in range(0, 64, per):
        pa = _nps(B)
        for j in range(per):
            mk.mm(pa[0:M, j * 128:(j + 1) * 128], Vv[:, :, c0 + j], C["ra64"][:, :], True, True, reads=[bb, C["ra64"]], writes=[pa])
        pv = pa[0:M, :].rearrange("p (j r k) -> p j r k", j=per, r=2)
        sv = lambda t: t[0:M, 0:per * 64].rearrange("p (j k) -> p j k", j=per)
        trb = TW[0:M, 0, :].unsqueeze(1).broadcast_to([M, per, 64])
        tib = TW[0:M, 1, :].unsqueeze(1).broadcast_to([M, per, 64])
        crv = cr[0:M, c0 * 64:(c0 + per) * 64].rearrange("p (j k) -> p j k", j=per)
        civ = ci[0:M, c0 * 64:(c0 + per) * 64].rearrange("p (j k) -> p j k", j=per)
        _cmul_psum(mk, B, pv[:, :, 0, :], pv[:, :, 1, :], pa, trb, tib, [TW], crv, civ, cr, ci, sv)
    scale = 1.0 / math.sqrt(L * 64.0)
    fo = tmp[:, 0:4096].rearrange("p (k c) -> p k c", c=64)
    for blk in range(8):
        cs = slice(blk * 512, (blk + 1) * 512)
        p = _nps(B)
        mk.mm(p[0:M, :], WCm[0:M, 0, :], cr[0:M, cs], True, False, reads=[WCm, cr], writes=[p])
        mk.mm(p[0:M, :], WCm[0:M, 2, :], ci[0:M, cs], False, True, reads=[WCm, ci], writes=[p])
        ov = fo[0:M, :, blk * 8:(blk + 1) * 8].rearrange("p k c -> p c k")
        mk.op("scalar", ("activation", dict(out=ov, in_=p[0:M, :].rearrange("p (c k) -> p c k", k=64), func=ACT.Copy, scale=scale)),
              reads=[p], writes=[tmp])
    fno = d["fno" + tag]
    mk.dma("sync", fno.t.rearrange("(a k) c -> a (k c)", k=64), tmp[0:M, 0:4096], reads=[tmp], writes=[fno])


def build_p2(seqs=(("m", 128), ("c", 4))):
    nc = bass.Bass("TRN2", target_bir_lowering=False)
    mk = MK(nc)
    d = {}
    for tag, M in seqs:
        L = 64 * M
        d["hyu" + tag] = mk.dram_in("hyu" + tag, [2, L + 2, 96])
        d["fnv" + tag] = mk.dram_in("fnv" + tag, [L, 128])
        d["zfeat" + tag] = mk.dram_in("zfeat" + tag, [33, 128 * M])
        d["hyo" + tag] = mk.dram_out("hyo" + tag, [2, L, 32])
        d["fno" + tag] = mk.dram_out("fno" + tag, [L, 64])
    C = {}

    def cload(name, shape, dtype, q=None):
        dd = mk.dram_in(name, shape)
        t = mk.sb(shape, dtype)
        mk.dma(q or ("gpsimd" if dtype == BF16 else "sync"), t[:], dd[:], reads=[dd], writes=[t])
        C[name] = t
        return t
    cload("ra", [128, 256], BF16)
    cload("rf", [128, 256], BF16)
    cload("ra64", [128, 128], BF16)
    cload("l1", [128, 128], BF16)
    cload("l2", [128, 128], BF16)
    cload("ones", [128, 128], F32)
    cload("w1", [33, 64], F32)
    cload("w2", [64, 64], F32)
    cload("fa", [64, 2], F32)
    cload("fc", [64, 2], F32)
    cload("w3", [64, 2, 64], BF16)
    cload("hb", [128, 2, 32], F32)
    cload("cw", [128, 3, 96], F32)
    cload("cb", [128, 96], F32)
    for tag, M in seqs:
        d["decay" + tag] = mk.dram_in("decay" + tag, [128, M, 32])
        cload("tw" + tag, [M, 2, 128], F32)
        cload("twt" + tag, [128, 2, M], F32)
        cload("twf" + tag, [M, 2, 64], F32)
        cload("wc" + tag, [M, 3, M], BF16)
        cload("ic" + tag, [M, 2, 2 * M], BF16)
    mk.op("vector", ("tensor_tensor", dict(out=C["fc"][:], in0=C["fc"][:], in1=C["fa"][:], op=ALU.mult)), reads=[C["fa"], C["fc"]], writes=[C["fc"]])
    mk.op("vector", ("tensor_scalar_mul", dict(out=C["fc"][:], in0=C["fc"][:], scalar1=1.0 / TWO_PI)), reads=[C["fc"]], writes=[C["fc"]])
    mk.op("vector", ("tensor_scalar_mul", dict(out=C["fa"][:], in0=C["fa"][:], scalar1=1.0 / TWO_PI)), reads=[C["fa"]], writes=[C["fa"]])
    B = _p2_bufs(mk)
    for tag, M in seqs:
        emit_hyena(mk, B, M, tag, d, C)
        emit_fnet(mk, B, M, tag, d, C)
    mk.finish()
    mk.emit()
    return nc

import os as _os
MS_ENG = _os.environ.get('MS_ENG', 'vector')
NKT = 20


def build_p3a(NT=NT, NLAT=NLAT, stop=99):
    ROWS = NT * 128
    NKT_ = NLAT + 4
    nc = bass.Bass("TRN2", target_bir_lowering=False)
    mk = MK(nc)
    xs = mk.dram_in("xs", [ROWS, D])
    condT = mk.dram_in("condT", [128, 16])
    w_ada = mk.dram_in("w_ada", [D, 6 * D])
    b_ada = mk.dram_in("b_ada", [6 * D])
    n1g = mk.dram_in("n1g", [D])
    w_in = mk.dram_in("w_in", [D, 4864])
    q_d = mk.dram_in("q", [ROWS, 512])
    kvx = mk.dram_in("kvx", [NKT_ * 128, 256])
    hy_d = mk.dram_in("hy", [ROWS, 256])
    fn_d = mk.dram_in("fn", [ROWS, 256])
    w_pa = mk.dram_in("w_pa", [512, D])
    w_ph = mk.dram_in("w_ph", [256, D])
    w_pf = mk.dram_in("w_pf", [256, D])
    w_o = mk.dram_in("w_o", [D, D])
    sink = mk.dram_in("sink", [8])
    maskb = mk.dram_in("maskb", [4, 128, 512])
    ident = mk.dram_in("ident", [128, 128])
    x1_o = mk.dram_out("x1", [ROWS, D])

    psS = [mk.ps([128, 512], F32) for _ in range(3)]
    psO = mk.ps([128, 4, 65], F32)
    PT = mk.ps([128, 8, 128], BF16)
    psG = [mk.ps([128, 512], F32) for _ in range(3)]

    identb = mk.sb([128, 128], BF16)
    mk.dma("gpsimd", identb[:], ident[:], reads=[ident], writes=[identb])
    mb = mk.sb([128, 4, 512], BF16)
    mk.dma("gpsimd", mb[:], maskb.t.rearrange("i p n -> p i n"), reads=[maskb], writes=[mb])
    esink = mk.sb([128, 8], F32)
    mk.dma("sync", esink[:], sink[:].partition_broadcast(128), reads=[sink], writes=[esink])
    mk.op("scalar", ("activation", dict(out=esink[:], in_=esink[:], func=ACT.Exp)), reads=[esink], writes=[esink])
    epsb = mk.sb([128, 1], F32)
    mk.op("vector", ("memset", dict(ap=epsb[:], constant=EPS)), writes=[epsb])
    ss = mk.sb([128, NT], F32)
    rs = mk.sb([128, NT], F32)
    mk.op("vector", ("memset", dict(ap=ss[:], constant=0.0)), writes=[ss])

    WG = mk.sb([128, 8, 3072], BF16)
    mods = emit_adaln(mk, condT, w_ada, b_ada, 0, 3072, psG[0], wa_views=[(WG, WG[:, :, 0:512]), (WG, WG[:, :, 512:1024])])
    emit_modulate_setup(mk, mods, n1g, 0, 1024)

    if stop == 1:
        mk.finish(); mk.emit(); return nc
    wv = w_in.t.rearrange("(c p) n -> p c n", p=128)
    for c in range(8):
        mk.dma("gpsimd", WG[:, c, :], wv[:, c, 1792:4864], reads=[w_in], writes=[WG])
    WP = mk.sb([128, 8, D], BF16)
    mk.dma("gpsimd", WP[:, 0:4, :], w_pa.t.rearrange("(c p) n -> p c n", p=128), reads=[w_pa], writes=[WP])
    mk.dma("gpsimd", WP[:, 4:6, :], w_ph.t.rearrange("(c p) n -> p c n", p=128), reads=[w_ph], writes=[WP])
    mk.dma("gpsimd", WP[:, 6:8, :], w_pf.t.rearrange("(c p) n -> p c n", p=128), reads=[w_pf], writes=[WP])
    WO = mk.sb([128, 8, D], BF16)
    wov = w_o.t.rearrange("(c p) n -> p c n", p=128)
    for c in range(0, 8, 2):
        mk.dma("gpsimd", WO[:, c:c + 2, :], wov[:, c:c + 2, :], reads=[w_o], writes=[WO])

    if stop == 2:
        mk.finish(); mk.emit(); return nc
    KZ = [[mk.sb([128, NKT_, 128], BF16) for _ in range(2)] for _ in range(2)]
    for kv in range(2):
        for hf_ in range(2):
            mk.op(MS_ENG, ("memset", dict(ap=KZ[kv][hf_][:], constant=0.0)), writes=[KZ[kv][hf_]])
    VA = mk.sb([128, NKT_, 2, 65], BF16)
    mk.op(MS_ENG, ("memset", dict(ap=VA[:], constant=1.0)), writes=[VA])
    kvt = [mk.sb([128, 256], F32), mk.sb([128, 256], F32)]
    kb = mk.sb([128, 2, 128], BF16)
    PTk = PT
    for kt in range(NKT_):
        kf = kvt[kt % 2]
        mk.dma("sync", kf[:], kvx[kt * 128:(kt + 1) * 128, :], reads=[kvx], writes=[kf])
        mk.op("vector", ("tensor_copy", dict(out=kb[:, 0, :], in_=kf[:, 0:128])), reads=[kf], writes=[kb])
        mk.op("vector", ("tensor_copy", dict(out=kb[:, 1, 0:64], in_=kf[:, 64:128])), reads=[kf], writes=[kb])
        mk.op("vector", ("tensor_copy", dict(out=kb[:, 1, 64:128], in_=kf[:, 0:64])), reads=[kf], writes=[kb])
        mk.op("vector", ("tensor_copy", dict(out=VA[:, kt, :, 0:64], in_=kf[:, 128:256].rearrange("p (h d) -> p h d", h=2))), reads=[kf], writes=[VA])
        if stop == 31:
            mk.finish(); mk.emit(); return nc
        mk.tr(PTk[:, 0, :], kb[:, 0, :], identb[:], reads=[kb, identb], writes=[PTk])
        mk.tr(PTk[:, 1, :], kb[:, 1, :], identb[:], reads=[kb, identb], writes=[PTk])
        if stop == 32:
            mk.finish(); mk.emit(); return nc
        mk.op("scalar", ("copy", dict(out=KZ[0][0][0:64, kt, :], in_=PTk[0:64, 0, :])), reads=[PTk], writes=[KZ[0][0]])
        mk.op("scalar", ("copy", dict(out=KZ[1][1][64:128, kt, :], in_=PTk[64:128, 0, :])), reads=[PTk], writes=[KZ[1][1]])
        if stop == 33:
            mk.finish(); mk.emit(); return nc
        mk.op("scalar", ("copy", dict(out=KZ[1][0][0:64, kt, :], in_=PTk[0:64, 1, :])), reads=[PTk], writes=[KZ[1][0]])
        mk.op("scalar", ("copy", dict(out=KZ[0][1][64:128, kt, :], in_=PTk[64:128, 1, :])), reads=[PTk], writes=[KZ[0][1]])
        if stop == 34 + kt:
            mk.finish(); mk.emit(); return nc

    if stop == 3:
        mk.finish(); mk.emit(); return nc
    xts = [mk.sb([128, D], F32), mk.sb([128, D], F32)]
    hf = mk.sb([128, D], F32)
    hb = mk.sb([128, D], BF16)
    hT = mk.sb([128, 8, 128], BF16)
    qf1 = mk.sb([128, 512], F32)
    qf = [qf1, qf1]
    qb = mk.sb([128, 512], BF16)
    QT = mk.sb([128, 4, 128], BF16)
    Es = [mk.sb([128, 512], BF16) for _ in range(5)]
    den = mk.sb([128, 4], F32)
    attn = mk.sb([128, 512], BF16)
    hyf1 = mk.sb([128, 512], F32)
    hyf = [hyf1, hyf1]
    hfb = mk.sb([128, 512], BF16)
    BT = mk.sb([128, 8, 128], BF16)
    Gt = mk.sb([128, 3072], BF16)
    t1 = mk.sb([128, 512], F32)
    t2 = mk.sb([128, 512], F32)
    mbf = mk.sb([128, D], BF16)
    mT = mk.sb([128, 8, 128], BF16)
    xo1 = mk.sb([128, D], F32)
    xo = [xo1, xo1]
    si = 0
    for t in range(NT):
        r = 0 if t < NLAT else 1
        rows = slice(t * 128, (t + 1) * 128)
        if t < NLAT:
            keys = [(t, 0 if t == 0 else 1), (t + 1, None), (t + 2, 3 if t == NLAT - 1 else 2), (NLAT + 2, None), (NLAT + 3, None)]
        else:
            keys = [(NLAT + 2, None), (NLAT + 3, None)]
        q_ = qf[t % 2]
        mk.dma("sync", q_[:], q_d[rows, :], reads=[q_d], writes=[q_])
        mk.op("vector", ("tensor_copy", dict(out=qb[:], in_=q_[:])), reads=[q_], writes=[qb])
        for p_ in range(4):
            mk.tr(PT[:, p_, :], qb[:, p_ * 128:(p_ + 1) * 128], identb[:], reads=[qb, identb], writes=[PT])
        mk.op("scalar", ("copy", dict(out=QT[:], in_=PT[:, 0:4, :])), reads=[PT], writes=[QT])
        for kv in range(2):
            for ki, (kt, mi) in enumerate(keys):
                S = psS[si % 3]
                si += 1
                if mi is not None:
                    mk.mm(S[:], identb[:], mb[:, mi, :], True, False, reads=[identb, mb], writes=[S])
                for hh in range(4):
                    h = 4 * kv + hh
                    mk.mm(S[:, hh * 128:(hh + 1) * 128], KZ[kv][h % 2][:, kt, :], QT[:, h // 2, :],
                          (mi is None and hh == 0), hh == 3, reads=[KZ[kv][h % 2], QT], writes=[S])
                mk.op("scalar", ("activation", dict(out=Es[ki][:], in_=S[:], func=ACT.Exp, scale=0.125)), reads=[S], writes=[Es[ki]])
            for hh in range(4):
                for ki, (kt, mi) in enumerate(keys):
                    mk.mm(psO[:, hh, :], Es[ki][:, hh * 128:(hh + 1) * 128], VA[:, kt, kv, :], ki == 0, ki == len(keys) - 1,
                          reads=[Es[ki], VA], writes=[psO])
            mk.op("vector", ("tensor_tensor", dict(out=den[:], in0=psO[:, :, 64], in1=esink[:, 4 * kv:4 * kv + 4], op=ALU.add)), reads=[psO, esink], writes=[den])
            mk.op("vector", ("reciprocal", dict(out=den[:], in_=den[:])), reads=[den], writes=[den])
            mk.op("vector", ("tensor_tensor", dict(out=attn[:, kv * 256:(kv + 1) * 256].rearrange("p (h d) -> p h d", h=4), in0=psO[:, :, 0:64],
                                                    in1=den[:, :].unsqueeze(2).broadcast_to([128, 4, 64]), op=ALU.mult)), reads=[psO, den], writes=[attn])
        if stop == 4:
            mk.finish(); mk.emit(); return nc
        hy_ = hyf[t % 2]
        mk.dma("sync", hy_[:, 0:256], hy_d[rows, :], reads=[hy_d], writes=[hy_])
        mk.dma("sync", hy_[:, 256:512], fn_d[rows, :], reads=[fn_d], writes=[hy_])
        mk.op("vector", ("tensor_copy", dict(out=hfb[:], in_=hy_[:])), reads=[hy_], writes=[hfb])
        for c in range(4):
            mk.tr(PT[:, c, :], attn[:, c * 128:(c + 1) * 128], identb[:], reads=[attn, identb], writes=[PT])
        for c in range(4):
            mk.tr(PT[:, 4 + c, :], hfb[:, c * 128:(c + 1) * 128], identb[:], reads=[hfb, identb], writes=[PT])
        mk.op("scalar", ("copy", dict(out=BT[:], in_=PT[:])), reads=[PT], writes=[BT])
        if stop == 5:
            mk.finish(); mk.emit(); return nc
        xt = xts[t % 2]
        mk.dma("sync", xt[:], xs[rows, :], reads=[xs], writes=[xt])
        emit_norm_mod_T(mk, xt, mods[r], 0, 1024, ss, rs, t, epsb, hf, hb, PT, hT, identb)
        for nb in range(6):
            pg = psG[nb % 3]
            for c in range(8):
                mk.mm(pg[:], hT[:, c, :], WG[:, c, nb * 512:(nb + 1) * 512], c == 0, c == 7, reads=[hT, WG], writes=[pg])
            mk.op("scalar", ("activation", dict(out=Gt[:, nb * 512:(nb + 1) * 512], in_=pg[:], func=ACT.Sigmoid)), reads=[pg], writes=[Gt])
        if stop == 6:
            mk.finish(); mk.emit(); return nc
        for half in range(2):
            cs = slice(half * 512, (half + 1) * 512)
            for bi, (c0, c1) in enumerate(((0, 4), (4, 6), (6, 8))):
                for c in range(c0, c1):
                    mk.mm(psG[bi][:], BT[:, c, :], WP[:, c, cs], c == c0, c == c1 - 1, reads=[BT, WP], writes=[psG[bi]])
            mk.op("vector", ("tensor_tensor", dict(out=t1[:], in0=psG[0][:], in1=Gt[:, half * 512:(half + 1) * 512], op=ALU.mult)), reads=[psG[0], Gt], writes=[t1])
            mk.op("vector", ("tensor_tensor", dict(out=t2[:], in0=psG[1][:], in1=Gt[:, 1024 + half * 512:1024 + (half + 1) * 512], op=ALU.mult)), reads=[psG[1], Gt], writes=[t2])
            mk.op("gpsimd", ("tensor_tensor", dict(out=t1[:], in0=t1[:], in1=t2[:], op=ALU.add)), reads=[t1, t2], writes=[t1])
            mk.op("vector", ("tensor_tensor", dict(out=t2[:], in0=psG[2][:], in1=Gt[:, 2048 + half * 512:2048 + (half + 1) * 512], op=ALU.mult)), reads=[psG[2], Gt], writes=[t2])
            mk.op("gpsimd", ("tensor_tensor", dict(out=mbf[:, cs], in0=t1[:], in1=t2[:], op=ALU.add)), reads=[t1, t2], writes=[mbf])
        for c in range(8):
            mk.tr(PT[:, c, :], mbf[:, c * 128:(c + 1) * 128], identb[:], reads=[mbf, identb], writes=[PT])
        mk.op("scalar", ("copy", dict(out=mT[:], in_=PT[:])), reads=[PT], writes=[mT])
        xo_ = xo[t % 2]
        for half in range(2):
            cs = slice(half * 512, (half + 1) * 512)
            pg = psG[half]
            for c in range(8):
                mk.mm(pg[:], mT[:, c, :], WO[:, c, cs], c == 0, c == 7, reads=[mT, WO], writes=[pg])
            mk.op("vector", ("tensor_tensor", dict(out=t1[:], in0=pg[:], in1=mods[r][:, 2048 + half * 512:2048 + (half + 1) * 512], op=ALU.mult)), reads=[pg, mods[r]], writes=[t1])
            mk.op("gpsimd", ("tensor_tensor", dict(out=xo_[:, cs], in0=t1[:], in1=xt[:, cs], op=ALU.add)), reads=[t1, xt], writes=[xo_])
        mk.dma("sync", x1_o[rows, :], xo_[:], reads=[xo_], writes=[x1_o])
    mk.finish()
    mk.emit()
    return nc


def build_p3b(NE, NT=NT, NLAT=NLAT):
    ROWS = NT * 128
    FF = 2816
    NFC = FF // 128
    GF = 2
    nc = bass.Bass("TRN2", target_bir_lowering=False)
    mk = MK(nc)
    xs = mk.dram_in("xs", [ROWS, D])
    condT = mk.dram_in("condT", [128, 16])
    w_ada = mk.dram_in("w_ada", [D, 6 * D])
    b_ada = mk.dram_in("b_ada", [6 * D])
    n2g = mk.dram_in("n2g", [D])
    wg_d = mk.dram_in("wg", [NE, D, FF])
    wu_d = mk.dram_in("wu", [NE, D, FF])
    wd_d = mk.dram_in("wd", [NE, FF, D])
    ident = mk.dram_in("ident", [128, 128])
    if NE > 1:
        wr_d = mk.dram_in("wr", [D, 8])
    x2_o = mk.dram_out("x2", [ROWS, D])

    psG = [mk.ps([128, 512], F32) for _ in range(2)]
    psU = [mk.ps([128, 512], F32) for _ in range(2)]
    psY = [mk.ps([128, 512], F32) for _ in range(2)]
    PT = mk.ps([128, 8, 128], BF16)
    psR = mk.ps([128, 512], F32)

    identb = mk.sb([128, 128], BF16)
    mk.dma("gpsimd", identb[:], ident[:], reads=[ident], writes=[identb])
    epsb = mk.sb([128, 1], F32)
    mk.op("vector", ("memset", dict(ap=epsb[:], constant=EPS)), writes=[epsb])
    ss = mk.sb([128, NT], F32)
    rs = mk.sb([128, NT], F32)
    mk.op("vector", ("memset", dict(ap=ss[:], constant=0.0)), writes=[ss])
    h2T = mk.sb([128, 8, ROWS], BF16)
    if ROWS >= 1024:
        wav = [(h2T, h2T[:, :, 0:512]), (h2T, h2T[:, :, 512:1024])]
    else:
        wav = None
    mods = emit_adaln(mk, condT, w_ada, b_ada, 3072, 6144, psR, wa_views=wav)
    emit_modulate_setup(mk, mods, n2g, 0, 1024)

    yacc = mk.sb([128, NT, D], F32)
    mk.op("gpsimd", ("memset", dict(ap=yacc[:], constant=0.0)), writes=[yacc])
    comb = mk.sb([128, NT, 8], F32)
    if NE > 1:
        wrb = mk.sb([128, 8, 8], BF16)
        mk.dma("gpsimd", wrb[:], wr_d.t.rearrange("(c p) n -> p c n", p=128), reads=[wr_d], writes=[wrb])
    xts = [mk.sb([128, D], F32), mk.sb([128, D], F32)]
    hf = mk.sb([128, D], F32)
    hb = mk.sb([128, D], BF16)
    hT = mk.sb([128, 8, 128], BF16)
    sm = mk.sb([128, 64], F32)
    for t in range(NT):
        r = 0 if t < NLAT else 1
        xt = xts[t % 2]
        mk.dma("sync", xt[:], xs[t * 128:(t + 1) * 128, :], reads=[xs], writes=[xt])
        emit_norm_mod_T(mk, xt, mods[r], 0, 1024, ss, rs, t, epsb, hf, hb, PT, h2T, identb, hT_ap=h2T[:, :, t * 128:(t + 1) * 128])
        if NE > 1:
            for c in range(8):
                mk.mm(psR[:, 0:8], h2T[:, c, t * 128:(t + 1) * 128], wrb[:, c, :], c == 0, c == 7, reads=[h2T, wrb], writes=[psR])
            lg, m1, eq1, lg2, m2, eq2, dl, w1, w2 = (sm[:, 0:8], sm[:, 8:9], sm[:, 16:24], sm[:, 24:32], sm[:, 9:10], sm[:, 32:40],
                                                    sm[:, 10:11], sm[:, 11:12], sm[:, 12:13])
            R, W = [sm], [sm]
            mk.op("vector", ("tensor_copy", dict(out=lg, in_=psR[:, 0:8])), reads=[psR], writes=W)
            mk.op("vector", ("tensor_reduce", dict(out=m1, in_=lg, axis=AX.X, op=ALU.max)), reads=R, writes=W)
            mk.op("vector", ("tensor_scalar", dict(out=eq1, in0=lg, scalar1=m1, scalar2=None, op0=ALU.is_equal)), reads=R, writes=W)
            mk.op("vector", ("scalar_tensor_tensor", dict(out=lg2, in0=eq1, scalar=-1e30, in1=lg, op0=ALU.mult, op1=ALU.add)), reads=R, writes=W)
            mk.op("vector", ("tensor_reduce", dict(out=m2, in_=lg2, axis=AX.X, op=ALU.max)), reads=R, writes=W)
            mk.op("vector", ("tensor_scalar", dict(out=eq2, in0=lg2, scalar1=m2, scalar2=None, op0=ALU.is_equal)), reads=R, writes=W)
            mk.op("vector", ("tensor_tensor", dict(out=dl, in0=m1, in1=m2, op=ALU.subtract)), reads=R, writes=W)
            mk.op("scalar", ("activation", dict(out=w1, in_=dl, func=ACT.Sigmoid)), reads=R, writes=W)
            mk.op("scalar", ("activation", dict(out=w2, in_=dl, func=ACT.Sigmoid, scale=-1.0)), reads=R, writes=W)
            mk.op("vector", ("tensor_scalar", dict(out=eq1, in0=eq1, scalar1=w1, scalar2=None, op0=ALU.mult)), reads=R, writes=W)
            mk.op("vector", ("scalar_tensor_tensor", dict(out=comb[:, t, :], in0=eq2, scalar=w2, in1=eq1, op0=ALU.mult, op1=ALU.add)), reads=R, writes=[comb])
    wgs = [mk.sb([128, 8, GF * 128], BF16) for _ in range(2)]
    wus = [mk.sb([128, 8, GF * 128], BF16) for _ in range(2)]
    wds = [mk.sb([128, GF, D], BF16) for _ in range(2)]
    aT = mk.sb([128, GF, ROWS], BF16)
    sg = [mk.sb([128, 512], F32), mk.sb([128, 512], F32)]
    tblocks = [(a, min(512, ROWS - a)) for a in range(0, ROWS, 512)]
    gi = 0
    pi = 0
    for e in range(NE):
        wgv = wg_d.t[e].rearrange("(c p) n -> p c n", p=128)
        wuv = wu_d.t[e].rearrange("(c p) n -> p c n", p=128)
        wdv = wd_d.t[e].rearrange("(c p) n -> p c n", p=128)
        for g0 in range(0, NFC, GF):
            wg_, wu_, wd_ = wgs[gi % 2], wus[gi % 2], wds[gi % 2]
            gi += 1
            mk.dma("gpsimd", wg_[:], wgv[:, :, g0 * 128:(g0 + GF) * 128], reads=[wg_d], writes=[wg_])
            mk.dma("gpsimd", wu_[:], wuv[:, :, g0 * 128:(g0 + GF) * 128], reads=[wu_d], writes=[wu_])
            mk.dma("gpsimd", wd_[:], wdv[:, g0:g0 + GF, :], reads=[wd_d], writes=[wd_])
            for j in range(GF):
                for (a, w) in tblocks:
                    pg, pu = psG[pi % 2], psU[pi % 2]
                    s_ = sg[pi % 2]
                    pi += 1
                    for c in range(8):
                        mk.mm(pg[:, 0:w], wg_[:, c, j * 128:(j + 1) * 128], h2T[:, c, a:a + w], c == 0, c == 7, reads=[wg_, h2T], writes=[pg])
                    for c in range(8):
                        mk.mm(pu[:, 0:w], wu_[:, c, j * 128:(j + 1) * 128], h2T[:, c, a:a + w], c == 0, c == 7, reads=[wu_, h2T], writes=[pu])
                    mk.op("scalar", ("activation", dict(out=s_[:, 0:w], in_=pg[:, 0:w], func=ACT.Silu)), reads=[pg], writes=[s_])
                    mk.op("vector", ("tensor_tensor", dict(out=aT[:, j, a:a + w], in0=s_[:, 0:w], in1=pu[:, 0:w], op=ALU.mult)), reads=[s_, pu], writes=[aT])
            for t in range(NT):
                for half in range(2):
                    py = psY[(2 * t + half) % 2]
                    cs = slice(half * 512, (half + 1) * 512)
                    for j in range(GF):
                        mk.mm(py[:], aT[:, j, t * 128:(t + 1) * 128], wd_[:, j, cs], j == 0, j == GF - 1, reads=[aT, wd_], writes=[py])
                    sc = comb[:, t, e:e + 1] if NE > 1 else 1.0
                    rd = [py, yacc] + ([comb] if NE > 1 else [])
                    eng = "vector" if half == 0 else "gpsimd"
                    if eng == "gpsimd":
                        s_ = sg[(2 * t + half) % 2]
                        mk.op("scalar", ("activation", dict(out=s_[:], in_=py[:], func=ACT.Copy, scale=sc)), reads=[py] + ([comb] if NE > 1 else []), writes=[s_])
                        mk.op("gpsimd", ("tensor_tensor", dict(out=yacc[:, t, cs], in0=s_[:], in1=yacc[:, t, cs], op=ALU.add)),
                              reads=[s_, yacc], writes=[yacc])
                    else:
                        mk.op("vector", ("scalar_tensor_tensor", dict(out=yacc[:, t, cs], in0=py[:], scalar=sc, in1=yacc[:, t, cs], op0=ALU.mult, op1=ALU.add)),
                              reads=rd, writes=[yacc])
    xo = [mk.sb([128, D], F32), mk.sb([128, D], F32)]
    for t in range(NT):
        r = 0 if t < NLAT else 1
        xt = xts[t % 2]
        mk.dma("sync", xt[:], xs[t * 128:(t + 1) * 128, :], reads=[xs], writes=[xt])
        xo_ = xo[t % 2]
        mk.op("vector", ("tensor_tensor", dict(out=xo_[:], in0=yacc[:, t, :], in1=mods[r][:, 2048:3072], op=ALU.mult)), reads=[yacc, mods[r]], writes=[xo_])
        mk.op("gpsimd", ("tensor_tensor", dict(out=xo_[:], in0=xo_[:], in1=xt[:], op=ALU.add)), reads=[xo_, xt], writes=[xo_])
        mk.dma("sync", x2_o[t * 128:(t + 1) * 128, :], xo_[:], reads=[xo_], writes=[x2_o])
    mk.finish()
    mk.emit()
    return nc

def _rope_tables_core(qtr):
    t = np.arange(2048, dtype=np.int64) + qtr * 2048
    pos = np.stack([t // 64, t % 64], axis=-1).astype(np.float32)
    inv = (np.float32(10000.0) ** (-np.arange(16, dtype=np.float32) / np.float32(16))).astype(np.float32)
    ang = (pos[:, :, None] * inv).astype(np.float32)
    c = np.cos(ang).astype(np.float32).reshape(2048, 32)
    s = np.sin(ang).astype(np.float32).reshape(2048, 32)
    c = np.concatenate([c, np.ones((256, 32), np.float32)], 0)
    s = np.concatenate([s, np.zeros((256, 32), np.float32)], 0)
    return np.ascontiguousarray(c), np.ascontiguousarray(s)


def _dft(n):
    k = np.arange(n)
    a = 2.0 * np.pi * np.outer(k, k) / n
    return np.cos(a), np.sin(a)


def _condT(cb, c_ctx):
    cond = np.stack([cb, c_ctx], 0).astype(np.float32)
    return np.ascontiguousarray(cond.reshape(2, 8, 128).transpose(2, 0, 1).reshape(128, 16))


_CACHE = {}


def _get(name, fn):
    if name not in _CACHE:
        _CACHE[name] = fn()
    return _CACHE[name]


def p1_inmaps(inp, l, xcur, ctxcur):
    c64, s64 = _dft(64)
    f64 = np.concatenate([c64, -s64], 1).astype(np.float32)
    bd = np.zeros((128, 256), np.float32)
    bd[:64, :128] = f64
    bd[64:, 128:] = f64
    f64 = bd
    ident = np.eye(128, dtype=np.float32)
    maps = []
    for i in range(8):
        b, qtr = i // 4, i % 4
        rc, rs = _rope_tables_core(qtr)
        maps.append(dict(
            xs=np.ascontiguousarray(np.concatenate([xcur[b, qtr * 2048:(qtr + 1) * 2048], ctxcur[b]], 0)),
            condT=_condT(inp["c"][b], inp["c_ctx"]),
            w_ada=inp["w_ada"][l], b_ada=inp["b_ada"][l], n1g=inp["norm1_g"][l], w_in=inp["w_in"][l],
            qg=inp["q_norm_g"][l], kg=inp["k_norm_g"][l], ropec=rc, ropes=rs, f64=f64, ident=ident))
    return maps


def _zfeat_circ(L):
    f32 = np.float32
    t = np.linspace(0.0, 1.0, L, dtype=f32)[:, None]
    w = (f32(2.0 * math.pi) * np.arange(L, dtype=f32)[:, None] / f32(L)).astype(f32)
    fb = np.linspace(1e-4, 15, 16, dtype=f32)
    z = np.concatenate([t, np.cos(fb * w), -np.sin(fb * w)], axis=-1).astype(f32)
    zc = np.zeros((2 * L, 33), f32)
    zc[:L] = z
    zc[L + 1:] = z[1:][::-1]
    return np.ascontiguousarray(zc.T), t[:, 0]


def _decay_circ(L, M, chs):
    f32 = np.float32
    t = np.linspace(0.0, 1.0, L, dtype=f32)
    deltas = np.abs(np.linspace(math.log(1e-2) / 1.5, math.log(1e-2) / 0.3, 256, dtype=f32)).astype(f32)
    dec = np.exp(-(t[:, None] * deltas[None, chs])).astype(f32)
    dc = np.zeros((2 * L, len(chs)), f32)
    dc[:L] = dec
    dc[L + 1:] = dec[1:][::-1]
    return np.ascontiguousarray(dc.reshape(128, M, len(chs)))


def p2_consts(M):
    N = 128 * M
    L = 64 * M
    wr, ws = _dft(128)
    wi = -ws
    out = {}
    out["ra"] = np.concatenate([np.concatenate([wr[:64], wi[:64]], 1), np.concatenate([-wi[:64], wr[:64]], 1)], 0)
    out["rf"] = np.concatenate([wr, wi], 1)
    c64, s64 = _dft(64)
    w64r, w64i = c64, -s64
    out["ra64"] = np.concatenate([np.concatenate([w64r, w64i], 1), np.concatenate([-w64i, w64r], 1)], 0)
    out["l1"] = np.concatenate([wr[:, :64], -wi[:, :64]], 1)
    out["l2"] = np.concatenate([wi[:, :64], wr[:, :64]], 1)
    n2 = np.arange(M)[:, None]
    k1 = np.arange(128)[None, :]
    a = 2.0 * np.pi * n2 * k1 / N
    tw = np.stack([np.cos(a), -np.sin(a)], 1)
    out["tw"] = tw
    out["twt"] = np.ascontiguousarray(tw.transpose(2, 1, 0))
    a = 2.0 * np.pi * n2 * np.arange(64)[None, :] / L
    out["twf"] = np.stack([np.cos(a), -np.sin(a)], 1)
    mr, ms = _dft(M)
    mi = -ms
    out["wc"] = np.stack([mr, mi, -mi], 1)
    out["ic"] = np.stack([np.concatenate([mr, -mi], 1), np.concatenate([mi, mr], 1)], 1)
    return {k: np.ascontiguousarray(v.astype(np.float32)) for k, v in out.items()}


P2_SEQS = (("m", 128), ("c", 4))


def p2_inmaps(inp, l, hy_full, fnv_full, hy_ctx, fnv_ctx):
    maps = []
    cm = {tag: _get("p2c%d" % M, lambda M=M: p2_consts(M)) for tag, M in P2_SEQS}
    zf = {tag: _get("zf%d" % M, lambda M=M: _zfeat_circ(64 * M)[0]) for tag, M in P2_SEQS}
    for i in range(8):
        chs = np.arange(i * 32, (i + 1) * 32)
        b, g = i // 4, i % 4
        m = {}
        for (tag, M), hy, fv in ((P2_SEQS[0], hy_full, fnv_full), (P2_SEQS[1], hy_ctx, fnv_ctx)):
            L = 64 * M
            cols = np.concatenate([chs, 256 + chs, 512 + chs])
            hp = np.zeros((2, L + 2, 96), np.float32)
            hp[:, 1:L + 1] = hy[:, :, cols]
            m["hyu" + tag] = hp
            m["fnv" + tag] = np.ascontiguousarray(fv[b, :, g * 128:(g + 1) * 128])
            m["zfeat" + tag] = zf[tag]
            m["decay" + tag] = _get("dec%d_%d" % (M, i), lambda M=M, chs=chs: _decay_circ(64 * M, M, chs))
            for k in ("tw", "twt", "twf", "wc", "ic"):
                m[k + tag] = cm[tag][k]
        for k in ("ra", "rf", "ra64", "l1", "l2"):
            m[k] = cm["m"][k]
        m["ones"] = np.ones((128, 128), np.float32)
        m["w1"] = inp["hy_filt_w1"][l]
        m["w2"] = inp["hy_filt_w2"][l]
        m["fa"] = np.ascontiguousarray(np.stack([inp["hy_filt_freq1"][l], inp["hy_filt_freq2"][l]], 1))
        m["fc"] = np.ascontiguousarray(np.stack([inp["hy_filt_b1"][l], inp["hy_filt_b2"][l]], 1))
        w3 = inp["hy_filt_w3"][l].reshape(64, 2, 2, 256)[:, :, :, chs]
        m["w3"] = np.ascontiguousarray(w3.transpose(0, 2, 1, 3).reshape(64, 2, 64))
        m["hb"] = np.ascontiguousarray(np.broadcast_to(inp["hy_bias"][l][:, chs][None], (128, 2, 32)))
        cw = inp["hy_conv_w"][l][:, 0, :]
        cols = np.concatenate([chs, 256 + chs, 512 + chs])
        m["cw"] = np.ascontiguousarray(np.broadcast_to(cw[:, cols][None], (128, 3, 96)))
        m["cb"] = np.ascontiguousarray(np.broadcast_to(inp["hy_conv_b"][l][cols][None], (128, 96)))
        maps.append(m)
    return maps


def _masks(qtr):
    j = np.arange(128)[:, None]
    q = np.arange(128)[None, :]
    NEG = np.float32(-30000.0)
    prev = np.where(j >= q, np.float32(0), NEG).astype(np.float32)
    nxt = np.where(j <= q, np.float32(0), NEG).astype(np.float32)
    allm = np.full((128, 128), NEG, np.float32)
    ms = [allm if qtr == 0 else prev, prev, nxt, allm if qtr == 3 else nxt]
    return np.ascontiguousarray(np.stack([np.tile(m, (1, 4)) for m in ms], 0))


def p3a_inmaps(inp, l, xcur, ctxcur, q_all, kv_all, kv_ctx, hyo, fno, hyo_c, fno_c, q_ctx):
    ident = np.eye(128, dtype=np.float32)
    maps = []
    z = np.zeros((128, 256), np.float32)
    for i in range(8):
        b, qtr = i // 4, i % 4
        sl = slice(qtr * 2048, (qtr + 1) * 2048)
        prev = kv_all[b, qtr * 2048 - 128:qtr * 2048] if qtr > 0 else z
        nxt = kv_all[b, (qtr + 1) * 2048:(qtr + 1) * 2048 + 128] if qtr < 3 else z
        maps.append(dict(
            xs=np.ascontiguousarray(np.concatenate([xcur[b, sl], ctxcur[b]], 0)),
            condT=_condT(inp["c"][b], inp["c_ctx"]),
            w_ada=inp["w_ada"][l], b_ada=inp["b_ada"][l], n1g=inp["norm1_g"][l], w_in=inp["w_in"][l],
            q=np.ascontiguousarray(np.concatenate([q_all[b, sl], q_ctx[b]], 0)),
            kvx=np.ascontiguousarray(np.concatenate([prev, kv_all[b, sl], nxt, kv_ctx[b]], 0)),
            hy=np.ascontiguousarray(np.concatenate([hyo[b, sl], hyo_c[b]], 0)),
            fn=np.ascontiguousarray(np.concatenate([fno[b, sl], fno_c[b]], 0)),
            w_pa=inp["w_proj_attn"][l], w_ph=inp["w_proj_hyena"][l], w_pf=inp["w_proj_fnet"][l], w_o=inp["w_out"][l],
            sink=inp["attn_sink"][l], maskb=_masks(qtr), ident=ident))
    return maps


def p3b_inmaps(inp, l, x1, ctx1):
    ident = np.eye(128, dtype=np.float32)
    i_ = l // 2
    maps = []
    for i in range(8):
        b, qtr = i // 4, i % 4
        sl = slice(qtr * 2048, (qtr + 1) * 2048)
        m = dict(xs=np.ascontiguousarray(np.concatenate([x1[b, sl], ctx1[b]], 0)),
                 condT=_condT(inp["c"][b], inp["c_ctx"]), w_ada=inp["w_ada"][l], b_ada=inp["b_ada"][l],
                 n2g=inp["norm2_g"][l], ident=ident)
        if l % 2 == 0:
            m.update(wg=inp["ffn_w_gate"][i_][None], wu=inp["ffn_w_up"][i_][None], wd=inp["ffn_w_down"][i_][None])
        else:
            m.update(wg=inp["moe_w_gate"][i_], wu=inp["moe_w_up"][i_], wd=inp["moe_w_down"][i_], wr=inp["moe_router"][i_])
        maps.append(m)
    return maps


_NC = {}


def _prog(name, fn):
    if name not in _NC:
        _NC[name] = fn()
    return _NC[name]


def _run(nc, maps):
    res = run_bass_kernel_spmd(nc, maps, core_ids=list(range(8)))
    return res.results


def _gather_tok(results, key, width):
    lat = np.empty((2, 8192, width), np.float32)
    cx = np.empty((2, 256, width), np.float32)
    for i in range(8):
        b, qtr = i // 4, i % 4
        lat[b, qtr * 2048:(qtr + 1) * 2048] = results[i][key][:2048]
        if qtr == 0:
            cx[b] = results[i][key][2048:]
    return lat, cx


def kernel(**inputs):
    inp = {k: np.ascontiguousarray(np.asarray(v, dtype=np.float32)) for k, v in inputs.items()}
    x = inp["x"]
    ctx = inp["ctx"]
    for l in range(4):
        r1 = _run(_prog("p1", build_p1), p1_inmaps(inp, l, x, ctx))
        q_all, q_ctx = _gather_tok(r1, "q_o", 512)
        kv_all, kv_ctx = _gather_tok(r1, "kv_o", 256)
        hy_all, hy_ctx = _gather_tok(r1, "hy_o", 768)
        fv_all, fv_ctx = _gather_tok(r1, "fnv_o", 512)
        del r1
        r2 = _run(_prog("p2", build_p2), p2_inmaps(inp, l, hy_all, fv_all, hy_ctx, fv_ctx))
        hyo = np.concatenate([r2[i]["hyom"] for i in range(8)], -1)
        hyo_c = np.concatenate([r2[i]["hyoc"] for i in range(8)], -1)
        fno = np.stack([np.concatenate([r2[b * 4 + g]["fnom"] for g in range(4)], -1) for b in range(2)], 0)
        fno_c = np.stack([np.concatenate([r2[b * 4 + g]["fnoc"] for g in range(4)], -1) for b in range(2)], 0)
        del r2
        r3 = _run(_prog("p3a", build_p3a), p3a_inmaps(inp, l, x, ctx, q_all, kv_all, kv_ctx, hyo, fno, hyo_c, fno_c, q_ctx))
        x1, ctx1 = _gather_tok(r3, "x1", 1024)
        del r3
        ne = 1 if l % 2 == 0 else 8
        r4 = _run(_prog("p3b%d" % ne, lambda: build_p3b(ne)), p3b_inmaps(inp, l, x1, ctx1))
        x, ctx = _gather_tok(r4, "x2", 1024)
        del r4
    return x
```

```python
import contextlib
import numpy as np
import concourse.bass as bass
import concourse.mybir as mybir
from concourse.bass_utils import run_bass_kernel_spmd

F32 = mybir.dt.float32
BF16 = mybir.dt.bfloat16
ALU = mybir.AluOpType
ACT = mybir.ActivationFunctionType
AX = mybir.AxisListType
NPOOL = 12


class Buf:
    def __init__(self, t):
        self.t = t
        self.w = None
        self.r = {}

    def __getitem__(self, k):
        return self.t[k]


class MK:
    def __init__(self, nc):
        self.nc = nc
        self.es = contextlib.ExitStack()
        self.sems = {}
        self.engs = {}
        for name in ["tensor", "vector", "scalar", "gpsimd", "sync"]:
            sem = self.es.enter_context(nc.semaphore("s_" + name))
            self.sems[name] = sem
            self.engs[name] = dict(key=name, cnt=0, seen={}, ops=[])
        self.pool = {}
        self.rr = {}
        for q in ["sync", "gpsimd", "scalar"]:
            self.pool[q] = []
            for i in range(NPOOL):
                key = "d_%s%d" % (q, i)
                self.sems[key] = self.es.enter_context(nc.semaphore(key))
                self.pool[q].append(dict(key=key, val=0))
            self.rr[q] = 0
        self.nbuf = 0

    def phase_begin(self):
        self.pes = contextlib.ExitStack()

    def phase_end(self):
        self.barrier()
        self.pes.close()
        self.pes = None

    def sb(self, shape, dtype, name=None):
        self.nbuf += 1
        st = self.pes if getattr(self, "pes", None) is not None else self.es
        t = st.enter_context(self.nc.sbuf_tensor(name or ("sb%d" % self.nbuf), list(shape), dtype))
        return Buf(t)

    def ps(self, shape, dtype, name=None):
        self.nbuf += 1
        st = self.pes if getattr(self, "pes", None) is not None else self.es
        t = st.enter_context(self.nc.psum_tensor(name or ("ps%d" % self.nbuf), list(shape), dtype))
        return Buf(t)

    def dram_in(self, name, shape, dtype=F32):
        return Buf(self.nc.dram_tensor(name, list(shape), dtype, kind="ExternalInput").ap())

    def dram_out(self, name, shape, dtype=F32):
        return Buf(self.nc.dram_tensor(name, list(shape), dtype, kind="ExternalOutput").ap())

    def dram_tmp(self, name, shape, dtype=F32):
        return Buf(self.nc.dram_tensor(name, list(shape), dtype, kind="Internal").ap())

    def _deps(self, E, reads, writes, skip_self):
        deps = {}

        def need(ev):
            if ev is None:
                return
            k, v = ev
            if deps.get(k, 0) < v:
                deps[k] = v

        for b in reads:
            need(b.w)
        for b in writes:
            need(b.w)
            for k, v in b.r.items():
                need((k, v))
        waits = []
        for k, v in deps.items():
            if skip_self and k == E["key"]:
                continue
            if E["seen"].get(k, 0) >= v:
                continue
            E["seen"][k] = v
            waits.append((k, v))
        return waits

    def _mark(self, ev, reads, writes):
        k, v = ev
        for b in reads:
            if b.r.get(k, 0) < v:
                b.r[k] = v
        for b in writes:
            b.w = ev
            b.r = {}

    def op(self, en, fn, reads=(), writes=()):
        E = self.engs[en]
        waits = self._deps(E, reads, writes, skip_self=(en == "tensor"))
        E["cnt"] += 1
        ev = (E["key"], E["cnt"])
        if isinstance(fn, tuple):
            nm, kw = fn
            fn = (lambda e, nm=nm, kw=kw: getattr(e, nm)(**kw))
        E["ops"].append((waits, fn, (E["key"], 1)))
        self._mark(ev, reads, writes)

    def dma(self, q, out, in_, reads=(), writes=(), fn=None, **kw):
        E = self.engs[q]
        p = self.pool[q][self.rr[q] % NPOOL]
        self.rr[q] += 1
        waits = self._deps(E, reads, writes, skip_self=False)
        if p["val"] > 0 and E["seen"].get(p["key"], 0) < p["val"]:
            E["seen"][p["key"]] = p["val"]
            waits.append((p["key"], p["val"]))
        p["val"] += 16
        ev = (p["key"], p["val"])
        if fn is None:
            fn = (lambda e, out=out, in_=in_, kw=kw: e.dma_start(out=out, in_=in_, **kw))
        E["ops"].append((waits, fn, (p["key"], 16)))
        self._mark(ev, reads, writes)

    def barrier(self):
        allw = []
        for q in self.pool:
            for p in self.pool[q]:
                if p["val"] > 0:
                    allw.append((p["key"], p["val"]))
        for name in self.engs:
            c = self.engs[name]["cnt"]
            if c > 0:
                allw.append((name, c))
        for name, E in self.engs.items():
            waits = []
            for k, v in allw:
                if k == name:
                    continue
                if E["seen"].get(k, 0) >= v:
                    continue
                E["seen"][k] = v
                waits.append((k, v))
            E["ops"].append((waits, None, None))

    def finish(self):
        E = self.engs["sync"]
        waits = []
        for q in self.pool:
            for p in self.pool[q]:
                if p["val"] > 0:
                    waits.append((p["key"], p["val"]))
        for name in ["tensor", "vector", "scalar", "gpsimd"]:
            c = self.engs[name]["cnt"]
            if c > 0:
                waits.append((name, c))
        E["ops"].append((waits, None, None))

    def emit(self):
        nc = self.nc
        sems = self.sems
        engs = self.engs

        def run(e, name):
            for waits, fn, inc in engs[name]["ops"]:
                for k, v in waits:
                    e.wait_ge(sems[k], v)
                if fn is None:
                    continue
                ins = fn(e)
                ins.then_inc(sems[inc[0]], inc[1])

        with nc.Block() as block:
            @block.sync
            def _(e):
                run(e, "sync")

            @block.tensor
            def _(e):
                run(e, "tensor")

            @block.vector
            def _(e):
                run(e, "vector")

            @block.scalar
            def _(e):
                run(e, "scalar")

            @block.gpsimd
            def _(e):
                run(e, "gpsimd")
        self.es.close()

    def mm(self, out, lhsT, rhs, start, stop, reads, writes):
        self.op("tensor", lambda e, out=out, lhsT=lhsT, rhs=rhs, start=start, stop=stop: e.matmul(out, lhsT, rhs, start=start, stop=stop),
                reads=reads, writes=writes)

    def tr(self, out, in_, ident, reads, writes):
        self.op("tensor", lambda e, out=out, in_=in_, ident=ident: e.transpose(out, in_, ident), reads=reads, writes=writes)
import math

D = 1024
NLAT = 16
NT = 18
ROWS = NT * 128
EPS = 1e-6


def emit_adaln(mk, condT_d, w_ada_d, b_ada_d, c0, c1, psb, wa_views=None):
    n = c1 - c0
    ct = mk.sb([128, 16], F32)
    mk.dma("sync", ct[:], condT_d[:], reads=[condT_d], writes=[ct])
    sc = mk.sb([128, 16], F32)
    mk.op("scalar", ("activation", dict(out=sc[:], in_=ct[:], func=ACT.Silu)), reads=[ct], writes=[sc])
    LB = mk.sb([128, 16, 128], BF16)
    mk.op("vector", ("tensor_copy", dict(out=LB[:], in_=sc[:, :].unsqueeze(2).broadcast_to([128, 16, 128]))),
          reads=[sc], writes=[LB])
    bbs = [mk.sb([128, 512], F32), mk.sb([128, 512], F32)]
    mods = [mk.sb([128, n], F32), mk.sb([128, n], F32)]
    if wa_views is None:
        w0, w1 = mk.sb([128, 8, 512], BF16), mk.sb([128, 8, 512], BF16)
        wa_views = [(w0, w0[:, :, :]), (w1, w1[:, :, :])]
    wv = w_ada_d.t.rearrange("(c p) n -> p c n", p=128)
    for nb in range(n // 512):
        wa, wap = wa_views[nb % 2]
        bb = bbs[nb % 2]
        mk.dma("sync", bb[:], b_ada_d[c0 + nb * 512:c0 + (nb + 1) * 512].partition_broadcast(128), reads=[b_ada_d], writes=[bb])
        mk.dma("gpsimd", wap, wv[:, :, c0 + nb * 512:c0 + (nb + 1) * 512], reads=[w_ada_d], writes=[wa])
        for r in range(2):
            for c in range(8):
                mk.mm(psb[:], LB[:, r * 8 + c, :], wap[:, c, :], c == 0, c == 7, reads=[LB, wa], writes=[psb])
            mk.op("vector", ("tensor_tensor", dict(out=mods[r][:, nb * 512:(nb + 1) * 512], in0=psb[:], in1=bb[:], op=ALU.add)),
                  reads=[psb, bb], writes=[mods[r]])
    return mods


def emit_modulate_setup(mk, mods, g_d, sh_off, sc_off):
    gb = mk.sb([128, D], F32)
    mk.dma("sync", gb[:], g_d[:].partition_broadcast(128), reads=[g_d], writes=[gb])
    for r in range(2):
        m = mods[r]
        mk.op("vector", ("scalar_tensor_tensor", dict(out=m[:, sc_off:sc_off + D], in0=m[:, sc_off:sc_off + D], scalar=1.0,
                                                               in1=gb[:], op0=ALU.add, op1=ALU.mult)),
              reads=[m, gb], writes=[m])


def emit_norm_mod_T(mk, xt, m, sh_off, sc_off, ss, rs, col, epsb, hf, hb, PT, hT, identb, hT_ap=None):
    mk.op("scalar", ("activation", dict(out=hf[:], in_=xt[:], func=ACT.Square, accum_out=ss[:, col:col + 1])),
          reads=[xt], writes=[hf, ss])
    mk.op("scalar", ("activation", dict(out=rs[:, col:col + 1], in_=ss[:, col:col + 1], func=ACT.Sqrt, scale=1.0 / D, bias=epsb[:, 0:1])),
          reads=[ss, epsb], writes=[rs])
    mk.op("vector", ("reciprocal", dict(out=rs[:, col:col + 1], in_=rs[:, col:col + 1])), reads=[rs], writes=[rs])
    mk.op("vector", ("scalar_tensor_tensor", dict(out=hf[:], in0=xt[:], scalar=rs[:, col:col + 1], in1=m[:, sc_off:sc_off + D],
                                                     op0=ALU.mult, op1=ALU.mult)), reads=[xt, rs, m], writes=[hf])
    mk.op("vector", ("tensor_tensor", dict(out=hb[:], in0=hf[:], in1=m[:, sh_off:sh_off + D], op=ALU.add)),
          reads=[hf, m], writes=[hb])
    for c in range(8):
        mk.tr(PT[:, c, :], hb[:, c * 128:(c + 1) * 128], identb[:], reads=[hb, identb], writes=[PT])
    mk.op("scalar", ("copy", dict(out=(hT[:] if hT_ap is None else hT_ap), in_=PT[:])), reads=[PT], writes=[hT])


def emit_headnorm_rope(mk, src_ap, src_buf, nh, gain_b, cs_c, cs_s, tabs, sqb, qs, qn, qo, epsb):
    w = nh * 64
    mk.op("scalar", ("activation", dict(out=sqb[:, 0:w], in_=src_ap, func=ACT.Square)), reads=[src_buf], writes=[sqb])
    mk.op("vector", ("tensor_reduce", dict(out=qs[:, 0:nh], in_=sqb[:, 0:w].rearrange("p (h d) -> p h d", h=nh), axis=AX.X, op=ALU.add)),
          reads=[sqb], writes=[qs])
    mk.op("scalar", ("activation", dict(out=qs[:, 0:nh], in_=qs[:, 0:nh], func=ACT.Sqrt, scale=1.0 / 64, bias=epsb[:, 0:1])),
          reads=[qs, epsb], writes=[qs])
    mk.op("vector", ("reciprocal", dict(out=qs[:, 0:nh], in_=qs[:, 0:nh])), reads=[qs], writes=[qs])
    qn3 = qn[:, 0:w].rearrange("p (h d) -> p h d", h=nh)
    mk.op("vector", ("tensor_tensor", dict(out=qn3, in0=src_ap.rearrange("p (h d) -> p h d", h=nh),
                                              in1=qs[:, 0:nh].unsqueeze(2).broadcast_to([128, nh, 64]), op=ALU.mult)),
          reads=[src_buf, qs], writes=[qn])
    mk.op("vector", ("tensor_tensor", dict(out=qn3, in0=qn3, in1=gain_b[:, :].unsqueeze(1).broadcast_to([128, nh, 64]), op=ALU.mult)),
          reads=[qn, gain_b], writes=[qn])
    v5 = qn[:, 0:w].rearrange("p (h a j f) -> p h a j f", h=nh, a=2, j=2)
    o5 = qo[:, 0:w].rearrange("p (h a j f) -> p h a j f", h=nh, a=2, j=2)
    t5 = sqb[:, 0:w].rearrange("p (h a j f) -> p h a j f", h=nh, a=2, j=2)
    x1, x2 = v5[:, :, :, 0, :], v5[:, :, :, 1, :]
    cb = cs_c.unsqueeze(1).broadcast_to([128, nh, 2, 16])
    sb_ = cs_s.unsqueeze(1).broadcast_to([128, nh, 2, 16])
    rd = [qn]
    mk.op("vector", ("tensor_tensor", dict(out=t5[:, :, :, 0, :], in0=x1, in1=cb, op=ALU.mult)), reads=rd + tabs, writes=[sqb])
    mk.op("vector", ("tensor_tensor", dict(out=t5[:, :, :, 1, :], in0=x2, in1=sb_, op=ALU.mult)), reads=rd + tabs, writes=[sqb])
    mk.op("vector", ("tensor_tensor", dict(out=o5[:, :, :, 0, :], in0=t5[:, :, :, 0, :], in1=t5[:, :, :, 1, :], op=ALU.subtract)),
          reads=[sqb], writes=[qo])
    mk.op("vector", ("tensor_tensor", dict(out=t5[:, :, :, 0, :], in0=x1, in1=sb_, op=ALU.mult)), reads=rd + tabs, writes=[sqb])
    mk.op("vector", ("tensor_tensor", dict(out=t5[:, :, :, 1, :], in0=x2, in1=cb, op=ALU.mult)), reads=rd + tabs, writes=[sqb])
    mk.op("vector", ("tensor_tensor", dict(out=o5[:, :, :, 1, :], in0=t5[:, :, :, 0, :], in1=t5[:, :, :, 1, :], op=ALU.add)),
          reads=[sqb], writes=[qo])


def build_p1(NT=NT, NLAT=NLAT):
    ROWS = NT * 128
    nc = bass.Bass("TRN2", target_bir_lowering=False)
    mk = MK(nc)
    xs = mk.dram_in("xs", [ROWS, D])
    condT = mk.dram_in("condT", [128, 16])
    w_ada = mk.dram_in("w_ada", [D, 6 * D])
    b_ada = mk.dram_in("b_ada", [6 * D])
    n1g = mk.dram_in("n1g", [D])
    w_in = mk.dram_in("w_in", [D, 4864])
    qg = mk.dram_in("qg", [64])
    kg = mk.dram_in("kg", [64])
    ropec = mk.dram_in("ropec", [ROWS, 32])
    ropes = mk.dram_in("ropes", [ROWS, 32])
    f64 = mk.dram_in("f64", [128, 256])
    ident = mk.dram_in("ident", [128, 128])
    q_o = mk.dram_out("q_o", [ROWS, 512])
    kv_o = mk.dram_out("kv_o", [ROWS, 256])
    hy_o = mk.dram_out("hy_o", [ROWS, 768])
    fnv_o = mk.dram_out("fnv_o", [ROWS, 512])

    psA = mk.ps([128, 512], F32)
    psB = mk.ps([128, 512], F32)
    psC = mk.ps([128, 512], F32)
    psF = mk.ps([128, 2, 128], F32)
    psV = mk.ps([128, 512], F32)
    PT = mk.ps([128, 8, 128], BF16)

    identb = mk.sb([128, 128], BF16)
    mk.dma("gpsimd", identb[:], ident[:], reads=[ident], writes=[identb])
    f64b = mk.sb([128, 256], BF16)
    mk.dma("gpsimd", f64b[:], f64[:], reads=[f64], writes=[f64b])
    epsb = mk.sb([128, 1], F32)
    mk.op("vector", ("memset", dict(ap=epsb[:], constant=EPS)), writes=[epsb])
    ss = mk.sb([128, NT], F32)
    rs = mk.sb([128, NT], F32)
    mk.op("vector", ("memset", dict(ap=ss[:], constant=0.0)), writes=[ss])
    qgb = mk.sb([128, 64], F32)
    kgb = mk.sb([128, 64], F32)
    mk.dma("sync", qgb[:], qg[:].partition_broadcast(128), reads=[qg], writes=[qgb])
    mk.dma("sync", kgb[:], kg[:].partition_broadcast(128), reads=[kg], writes=[kgb])
    rc = mk.sb([128, NT, 32], F32)
    rsn = mk.sb([128, NT, 32], F32)
    mk.dma("sync", rc[:], ropec.t.rearrange("(t p) f -> p t f", p=128), reads=[ropec], writes=[rc])
    mk.dma("sync", rsn[:], ropes.t.rearrange("(t p) f -> p t f", p=128), reads=[ropes], writes=[rsn])

    mods = emit_adaln(mk, condT, w_ada, b_ada, 0, 2048, psA)
    emit_modulate_setup(mk, mods, n1g, 0, 1024)

    WB = mk.sb([128, 8, 1792], BF16)
    wv = w_in.t.rearrange("(c p) n -> p c n", p=128)
    for c in range(8):
        mk.dma("gpsimd", WB[:, c, :], wv[:, c, 0:1792], reads=[w_in], writes=[WB])

    xts = [mk.sb([128, D], F32), mk.sb([128, D], F32)]
    hf = mk.sb([128, D], F32)
    hb = mk.sb([128, D], BF16)
    hTs = [mk.sb([128, 8, 128], BF16), mk.sb([128, 8, 128], BF16)]
    sqb = mk.sb([128, 512], F32)
    qs = mk.sb([128, 8], F32)
    qn = mk.sb([128, 512], F32)
    qos = [mk.sb([128, 512], F32), mk.sb([128, 512], F32)]
    kvos = [mk.sb([128, 256], F32), mk.sb([128, 256], F32)]
    hyos = [mk.sb([128, 768], F32), mk.sb([128, 768], F32)]
    fnT = mk.sb([128, 2, 128], BF16)
    fvs = [mk.sb([128, 512], F32), mk.sb([128, 512], F32)]

    for t in range(NT):
        r = 0 if t < NLAT else 1
        xt = xts[t % 2]
        hT = hTs[t % 2]
        qo, kvo, hyo, fv = qos[t % 2], kvos[t % 2], hyos[t % 2], fvs[t % 2]
        rows = slice(t * 128, (t + 1) * 128)
        mk.dma("sync", xt[:], xs[rows, :], reads=[xs], writes=[xt])
        emit_norm_mod_T(mk, xt, mods[r], 0, 1024, ss, rs, t, epsb, hf, hb, PT, hT, identb)
        for (pb, c0) in ((psA, 0), (psB, 512), (psC, 1024)):
            for c in range(8):
                mk.mm(pb[:], hT[:, c, :], WB[:, c, c0:c0 + 512], c == 0, c == 7, reads=[hT, WB], writes=[pb])
        for blk in range(2):
            for c in range(8):
                mk.mm(psF[:, blk, :], WB[:, c, 1536 + blk * 128:1536 + (blk + 1) * 128], hT[:, c, :], c == 0, c == 7,
                      reads=[hT, WB], writes=[psF])
        mk.op("scalar", ("copy", dict(out=fnT[:], in_=psF[:])), reads=[psF], writes=[fnT])
        for blk in range(2):
            mk.mm(psV[:, blk * 256:(blk + 1) * 256], fnT[:, blk, :], f64b[:, :], True, True, reads=[fnT, f64b], writes=[psV])
        mk.op("scalar", ("copy", dict(out=fv[:], in_=psV[:])), reads=[psV], writes=[fv])
        mk.dma("sync", fnv_o[rows, :], fv[:], reads=[fv], writes=[fnv_o])
        emit_headnorm_rope(mk, psA[:, :], psA, 8, qgb, rc[:, t, :].rearrange("p (a f) -> p a f", a=2),
                           rsn[:, t, :].rearrange("p (a f) -> p a f", a=2), [rc, rsn], sqb, qs, qn, qo, epsb)
        mk.dma("sync", q_o[rows, :], qo[:], reads=[qo], writes=[q_o])
        emit_headnorm_rope(mk, psB[:, 0:128], psB, 2, kgb, rc[:, t, :].rearrange("p (a f) -> p a f", a=2),
                           rsn[:, t, :].rearrange("p (a f) -> p a f", a=2), [rc, rsn], sqb, qs, qn, kvo, epsb)
        mk.op("scalar", ("copy", dict(out=kvo[:, 128:256], in_=psB[:, 128:256])), reads=[psB], writes=[kvo])
        mk.dma("sync", kv_o[rows, :], kvo[:], reads=[kvo], writes=[kv_o])
        mk.op("scalar", ("copy", dict(out=hyo[:, 0:256], in_=psB[:, 256:512])), reads=[psB], writes=[hyo])
        mk.op("scalar", ("copy", dict(out=hyo[:, 256:768], in_=psC[:, :])), reads=[psC], writes=[hyo])
        mk.dma("sync", hy_o[rows, :], hyo[:], reads=[hyo], writes=[hy_o])
    mk.finish()
    mk.emit()
    return nc

TWO_PI = 2.0 * math.pi


def _p2_bufs(mk):
    B = {}
    B["uh"] = [mk.sb([128, 130, 32], F32) for _ in range(2)]
    B["uc"] = [mk.sb([128, 128, 32], F32) for _ in range(3)]
    B["tmp"] = mk.sb([128, 128 * 32], F32)
    B["zb"] = mk.sb([128, 128, 32], BF16)
    B["cr"] = mk.sb([128, 4096], BF16)
    B["ci"] = mk.sb([128, 4096], BF16)
    B["bb"] = mk.sb([128, 2, 4096], BF16)
    B["h2t"] = mk.sb([64, 16384], BF16)
    B["ta"] = mk.sb([128, 512], F32)
    B["tb"] = mk.sb([128, 512], F32)
    B["zt"] = [mk.sb([33, 512], F32), mk.sb([33, 512], F32)]
    B["tiq"] = mk.sb([64, 512], mybir.dt.int32)
    B["h1"] = mk.sb([64, 512], F32)
    B["dec"] = [mk.sb([128, 8, 32], F32), mk.sb([128, 8, 32], F32)]
    B["deci"] = 0
    B["asum"] = mk.sb([128, 32], F32)
    B["rn"] = mk.sb([128, 32], F32)
    B["ps"] = [mk.ps([128, 512], F32) for _ in range(6)]
    B["psi"] = 0
    return B


def _nps(B):
    p = B["ps"][B["psi"] % len(B["ps"])]
    B["psi"] += 1
    return p


def _cmul_psum(mk, B, pr_ap, pi_ap, pbuf, tr_ap, ti_ap, tbufs, outr_ap, outi_ap, outr_buf, outi_buf, shape_view, conj=False):
    ta, tb = B["ta"], B["tb"]
    va, vb = shape_view(ta), shape_view(tb)
    mk.op("vector", ("tensor_tensor", dict(out=va, in0=pr_ap, in1=tr_ap, op=ALU.mult)), reads=[pbuf] + tbufs, writes=[ta])
    mk.op("vector", ("tensor_tensor", dict(out=vb, in0=pi_ap, in1=ti_ap, op=ALU.mult)), reads=[pbuf] + tbufs, writes=[tb])
    mk.op("vector", ("tensor_tensor", dict(out=outr_ap, in0=va, in1=vb, op=(ALU.add if conj else ALU.subtract))),
          reads=[ta, tb], writes=[outr_buf])
    mk.op("vector", ("tensor_tensor", dict(out=va, in0=pr_ap, in1=ti_ap, op=ALU.mult)), reads=[pbuf] + tbufs, writes=[ta])
    mk.op("vector", ("tensor_tensor", dict(out=vb, in0=pi_ap, in1=tr_ap, op=ALU.mult)), reads=[pbuf] + tbufs, writes=[tb])
    if conj:
        mk.op("vector", ("tensor_tensor", dict(out=outi_ap, in0=vb, in1=va, op=ALU.subtract)), reads=[ta, tb], writes=[outi_buf])
    else:
        mk.op("vector", ("tensor_tensor", dict(out=outi_ap, in0=va, in1=vb, op=ALU.add)), reads=[ta, tb], writes=[outi_buf])


def emit_fft_fwd(mk, B, M, src, nch, RAb, TW, WC, w):
    cr, ci = B["cr"], B["ci"]
    per = 512 // (2 * w)
    for c0 in range(0, nch, per):
        pa = _nps(B)
        for j in range(per):
            mk.mm(pa[0:M, j * 2 * w:(j + 1) * 2 * w], src[:, 0:M, c0 + j], RAb[:, :], True, True, reads=[src, RAb], writes=[pa])
        pv = pa[0:M, :].rearrange("p (j r k) -> p j r k", j=per, r=2)
        sv = lambda t: t[0:M, 0:per * w].rearrange("p (j k) -> p j k", j=per)
        trb = TW[0:M, 0, :].unsqueeze(1).broadcast_to([M, per, w])
        tib = TW[0:M, 1, :].unsqueeze(1).broadcast_to([M, per, w])
        crv = cr[0:M, c0 * w:(c0 + per) * w].rearrange("p (j k) -> p j k", j=per)
        civ = ci[0:M, c0 * w:(c0 + per) * w].rearrange("p (j k) -> p j k", j=per)
        _cmul_psum(mk, B, pv[:, :, 0, :], pv[:, :, 1, :], pa, trb, tib, [TW], crv, civ, cr, ci, sv)


def emit_hyena(mk, B, M, tag, d, C):
    L = 64 * M
    NCIRC = 128 * M
    uh, uc, tmp, zb, cr, ci, bb, h2t = B["uh"], B["uc"], B["tmp"], B["zb"], B["cr"], B["ci"], B["bb"], B["h2t"]
    hyu = d["hyu" + tag]
    cw, cb = C["cw"], C["cb"]
    tv = tmp[:, 0:M * 32].rearrange("p (m c) -> p m c", c=32)
    for comp in range(3):
        cs = slice(comp * 32, (comp + 1) * 32)
        uhc = uh[comp % 2]
        for b in range(2):
            ps_ = slice(b * 64, (b + 1) * 64)
            mk.dma("sync", uhc[ps_, 1:M + 1, :], hyu.t[b, 1:L + 1, cs].rearrange("(n m) c -> n m c", m=M), reads=[hyu], writes=[uhc])
            mk.dma("sync", uhc[ps_, 0, :], hyu.t[b, 0:L, cs].rearrange("(n m) c -> n m c", m=M)[:, 0, :], reads=[hyu], writes=[uhc])
            mk.dma("sync", uhc[ps_, M + 1, :], hyu.t[b, 2:L + 2, cs].rearrange("(n m) c -> n m c", m=M)[:, M - 1, :], reads=[hyu], writes=[uhc])
        o = uc[comp][:, 0:M, :]
        wb = lambda k, cs=cs: cw[:, k, cs].unsqueeze(1).broadcast_to([128, M, 32])
        mk.op("vector", ("tensor_tensor", dict(out=o, in0=uhc[:, 0:M, :], in1=wb(0), op=ALU.mult)), reads=[uhc, cw], writes=[uc[comp]])
        mk.op("gpsimd", ("tensor_tensor", dict(out=tv, in0=uhc[:, 1:M + 1, :], in1=wb(1), op=ALU.mult)), reads=[uhc, cw], writes=[tmp])
        mk.op("vector", ("tensor_tensor", dict(out=o, in0=o, in1=tv, op=ALU.add)), reads=[uc[comp], tmp], writes=[uc[comp]])
        mk.op("gpsimd", ("tensor_tensor", dict(out=tv, in0=uhc[:, 2:M + 2, :], in1=wb(2), op=ALU.mult)), reads=[uhc, cw], writes=[tmp])
        mk.op("vector", ("tensor_tensor", dict(out=o, in0=o, in1=tv, op=ALU.add)), reads=[uc[comp], tmp], writes=[uc[comp]])
        mk.op("vector", ("tensor_tensor", dict(out=o, in0=o, in1=cb[:, cs].unsqueeze(1).broadcast_to([128, M, 32]), op=ALU.add)),
              reads=[uc[comp], cb], writes=[uc[comp]])
    zf = d["zfeat" + tag]
    zt_s = B["zt"]
    tq, tiq, tfq = B["ta"], B["tiq"], B["tb"]
    h1 = B["h1"]
    nblk = max(1, NCIRC // 512)
    bw = min(512, NCIRC)
    for blk in range(nblk):
        zt = zt_s[blk % 2]
        mk.dma("sync", zt[:, 0:bw], zf[:, blk * bw:(blk + 1) * bw], reads=[zf], writes=[zt])
        src, srcb = zt, None
        for layer in range(2):
            wl = C["w1"] if layer == 0 else C["w2"]
            kk = 33 if layer == 0 else 64
            p = _nps(B)
            rhs = zt[0:33, 0:bw] if layer == 0 else h1[:, 0:bw]
            rb = zt if layer == 0 else h1
            mk.mm(p[0:64, 0:bw], wl[0:kk, :], rhs, True, True, reads=[wl, rb], writes=[p])
            a_, c_ = C["fa"][:, layer:layer + 1], C["fc"][:, layer:layer + 1]
            mk.op("vector", ("tensor_scalar", dict(out=tq[0:64, 0:bw], in0=p[0:64, 0:bw], scalar1=a_, scalar2=c_, op0=ALU.mult, op1=ALU.add)),
                  reads=[p, C["fa"], C["fc"]], writes=[tq])
            mk.op("vector", ("tensor_copy", dict(out=tiq[:, 0:bw], in_=tq[0:64, 0:bw])), reads=[tq], writes=[tiq])
            mk.op("vector", ("tensor_copy", dict(out=tfq[0:64, 0:bw], in_=tiq[:, 0:bw])), reads=[tiq], writes=[tfq])
            mk.op("vector", ("tensor_tensor", dict(out=tq[0:64, 0:bw], in0=tq[0:64, 0:bw], in1=tfq[0:64, 0:bw], op=ALU.subtract)), reads=[tq, tfq], writes=[tq])
            if layer == 0:
                mk.op("scalar", ("activation", dict(out=h1[:, 0:bw], in_=tq[0:64, 0:bw], func=ACT.Sin, scale=TWO_PI)), reads=[tq], writes=[h1])
            else:
                mk.op("scalar", ("activation", dict(out=h2t[:, blk * bw:(blk + 1) * bw], in_=tq[0:64, 0:bw], func=ACT.Sin, scale=TWO_PI)),
                      reads=[tq], writes=[h2t])
    hr, hi = uh[0], uh[1]
    taps = mk_view_taps = tmp
    hrv = lambda: hr[:, :, :].rearrange("p a b -> p (a b)")[0:M, 0:4096]
    hiv = lambda: hi[:, :, :].rearrange("p a b -> p (a b)")[0:M, 0:4096]
    taps3 = tmp[:, 0:M * 32].rearrange("p (m c) -> p m c", c=32)
    tapv = taps3
    tapsb = zb
    dec_d = d["decay" + tag]
    TW, TWT = C["tw" + tag], C["twt" + tag]
    WCm, ICm = C["wc" + tag], C["ic" + tag]
    h2v = h2t[:, 0:NCIRC].rearrange("p (n m) -> p n m", m=M)
    zsrc = uc[2]
    for o in range(2):
        G = min(8, M)
        for g0 in range(0, M, G):
            p = _nps(B)
            for j in range(G):
                mk.mm(p[:, j * 64:(j + 1) * 64], h2v[:, :, g0 + j], C["w3"][:, o, :], True, True, reads=[h2t, C["w3"]], writes=[p])
            pv = p[:, 0:G * 64].rearrange("p (j r c) -> p j r c", j=G, r=2)
            dec = B["dec"][B["deci"] % 2]
            B["deci"] += 1
            mk.dma("sync", dec[:, 0:G, :], dec_d[:, g0:g0 + G, :], reads=[dec_d], writes=[dec])
            mk.op("vector", ("tensor_tensor", dict(out=taps3[0:64, g0:g0 + G, :], in0=pv[0:64, :, 0, :], in1=dec[0:64, 0:G, :], op=ALU.mult)),
                  reads=[p, dec], writes=[taps])
            mk.op("vector", ("tensor_tensor", dict(out=taps3[64:128, g0:g0 + G, :], in0=pv[64:128, :, 1, :], in1=dec[64:128, 0:G, :], op=ALU.mult)),
                  reads=[p, dec], writes=[taps])
        asum = B["asum"]
        mk.op("vector", ("tensor_reduce", dict(out=asum[:], in_=tapv.rearrange("p m c -> p c m"), axis=AX.X, op=ALU.add, apply_absolute_value=True)),
              reads=[taps], writes=[asum])
        p = _nps(B)
        mk.mm(p[:, 0:32], C["ones"][:, :], asum[:, :], True, True, reads=[C["ones"], asum], writes=[p])
        rn = B["rn"]
        mk.op("vector", ("reciprocal", dict(out=rn[:], in_=p[:, 0:32])), reads=[p], writes=[rn])
        mk.op("vector", ("tensor_copy", dict(out=tapsb[:, 0:M, :], in_=tapv)), reads=[taps], writes=[tapsb])
        emit_fft_fwd(mk, B, M, tapsb, 32, C["rf"], TW, WCm, 128)
        for blk in range(8):
            cs = slice(blk * 512, (blk + 1) * 512)
            pr_, pi_ = _nps(B), _nps(B)
            mk.mm(pr_[0:M, :], WCm[0:M, 0, :], cr[0:M, cs], True, False, reads=[WCm, cr], writes=[pr_])
            mk.mm(pr_[0:M, :], WCm[0:M, 2, :], ci[0:M, cs], False, True, reads=[WCm, ci], writes=[pr_])
            mk.mm(pi_[0:M, :], WCm[0:M, 1, :], cr[0:M, cs], True, False, reads=[WCm, cr], writes=[pi_])
            mk.mm(pi_[0:M, :], WCm[0:M, 0, :], ci[0:M, cs], False, True, reads=[WCm, ci], writes=[pi_])
            rnb = rn[0:M, blk * 4:(blk + 1) * 4].unsqueeze(2).broadcast_to([M, 4, 128])
            mk.op("vector", ("tensor_tensor", dict(out=hrv()[:, cs].rearrange("p (j k) -> p j k", j=4),
                                                                              in0=pr_[0:M, :].rearrange("p (j k) -> p j k", j=4), in1=rnb, op=ALU.mult)),
                  reads=[pr_, rn], writes=[hr])
            mk.op("vector", ("tensor_tensor", dict(out=hiv()[:, cs].rearrange("p (j k) -> p j k", j=4),
                                                                              in0=pi_[0:M, :].rearrange("p (j k) -> p j k", j=4), in1=rnb, op=ALU.mult)),
                  reads=[pi_, rn], writes=[hi])
        zsv = zsrc[:, 0:M, :]
        mk.op("vector", ("tensor_copy", dict(out=zb[:, 0:M, :], in_=zsv)), reads=[zsrc], writes=[zb])
        emit_fft_fwd(mk, B, M, zb, 32, C["ra"], TW, WCm, 128)
        for blk in range(8):
            cs = slice(blk * 512, (blk + 1) * 512)
            pr_, pi_ = _nps(B), _nps(B)
            mk.mm(pr_[0:M, :], WCm[0:M, 0, :], cr[0:M, cs], True, False, reads=[WCm, cr], writes=[pr_])
            mk.mm(pr_[0:M, :], WCm[0:M, 2, :], ci[0:M, cs], False, True, reads=[WCm, ci], writes=[pr_])
            mk.mm(pi_[0:M, :], WCm[0:M, 1, :], cr[0:M, cs], True, False, reads=[WCm, cr], writes=[pi_])
            mk.mm(pi_[0:M, :], WCm[0:M, 0, :], ci[0:M, cs], False, True, reads=[WCm, ci], writes=[pi_])
            ta, tb = B["ta"], B["tb"]
            mk.op("vector", ("tensor_tensor", dict(out=ta[0:M, :], in0=pr_[0:M, :], in1=hrv()[:, cs], op=ALU.mult)), reads=[pr_, hr], writes=[ta])
            mk.op("vector", ("tensor_tensor", dict(out=tb[0:M, :], in0=pi_[0:M, :], in1=hiv()[:, cs], op=ALU.mult)), reads=[pi_, hi], writes=[tb])
            mk.op("vector", ("tensor_tensor", dict(out=cr[0:M, cs], in0=ta[0:M, :], in1=tb[0:M, :], op=ALU.subtract)), reads=[ta, tb], writes=[cr])
            mk.op("vector", ("tensor_tensor", dict(out=ta[0:M, :], in0=pr_[0:M, :], in1=hiv()[:, cs], op=ALU.mult)), reads=[pr_, hi], writes=[ta])
            mk.op("vector", ("tensor_tensor", dict(out=tb[0:M, :], in0=pi_[0:M, :], in1=hrv()[:, cs], op=ALU.mult)), reads=[pi_, hr], writes=[tb])
            mk.op("vector", ("tensor_tensor", dict(out=ci[0:M, cs], in0=ta[0:M, :], in1=tb[0:M, :], op=ALU.add)), reads=[ta, tb], writes=[ci])
        per = min(32, 512 // (2 * M))
        for c0 in range(0, 32, per):
            pb_ = _nps(B)
            for j in range(per):
                ch = c0 + j
                osl = pb_[:, j * 2 * M:(j + 1) * 2 * M]
                mk.mm(osl, cr[0:M, ch * 128:(ch + 1) * 128], ICm[0:M, 0, :], True, False, reads=[cr, ICm], writes=[pb_])
                mk.mm(osl, ci[0:M, ch * 128:(ch + 1) * 128], ICm[0:M, 1, :], False, True, reads=[ci, ICm], writes=[pb_])
            pv = pb_[:, 0:per * 2 * M].rearrange("p (j r k) -> p j r k", j=per, r=2)
            sv = lambda t: t[:, 0:per * M].rearrange("p (j k) -> p j k", j=per)
            trb = TWT[:, 0, :].unsqueeze(1).broadcast_to([128, per, M])
            tib = TWT[:, 1, :].unsqueeze(1).broadcast_to([128, per, M])
            brv = bb[:, 0, c0 * M:(c0 + per) * M].rearrange("p (j k) -> p j k", j=per)
            biv = bb[:, 1, c0 * M:(c0 + per) * M].rearrange("p (j k) -> p j k", j=per)
            _cmul_psum(mk, B, pv[:, :, 0, :], pv[:, :, 1, :], pb_, trb, tib, [TWT], brv, biv, bb, bb, sv, conj=True)
        tot = 32 * M
        bwid = min(512, tot)
        cpb = bwid // M
        for blk in range(tot // bwid):
            po = _nps(B)
            cs = slice(blk * bwid, (blk + 1) * bwid)
            mk.mm(po[:, 0:bwid], C["l1"][:, :], bb[:, 0, cs], True, False, reads=[C["l1"], bb], writes=[po])
            mk.mm(po[:, 0:bwid], C["l2"][:, :], bb[:, 1, cs], False, True, reads=[C["l2"], bb], writes=[po])
            ov = tmp[:, 0:M * 32].rearrange("p (m c) -> p m c", c=32)[:, :, blk * cpb:(blk + 1) * cpb].rearrange("p m c -> p c m")
            mk.op("scalar", ("activation", dict(out=ov, in_=po[:, 0:bwid].rearrange("p (c m) -> p c m", m=M), func=ACT.Copy, scale=1.0 / NCIRC)),
                  reads=[po], writes=[tmp])
        zn = zsv
        hbv = C["hb"][:, o, :].unsqueeze(1).broadcast_to([128, M, 32])
        mk.op("vector", ("tensor_tensor", dict(out=zn, in0=zn, in1=hbv, op=ALU.mult)), reads=[zsrc, C["hb"]], writes=[zsrc])
        mk.op("vector", ("tensor_tensor", dict(out=zn, in0=zn, in1=tv, op=ALU.add)), reads=[zsrc, tmp], writes=[zsrc])
        mk.op("vector", ("tensor_tensor", dict(out=zn, in0=zn, in1=uc[o][:, 0:M, :], op=ALU.mult)), reads=[zsrc, uc[o]], writes=[zsrc])
    hyo = d["hyo" + tag]
    for b in range(2):
        mk.dma("sync", hyo.t[b].rearrange("(n m) c -> n m c", m=M), uc[2][b * 64:(b + 1) * 64, 0:M, :], reads=[uc[2]], writes=[hyo])


def emit_fnet(mk, B, M, tag, d, C):
    L = 64 * M
    cr, ci, bb, tmp = B["cr"], B["ci"], B["bb"], B["tmp"]
    fnv = d["fnv" + tag]
    V = bb
    Vv = bb[:, :, :].rearrange("p a b -> p (a b)")[:, 0:M * 64].rearrange("p (m c) -> p m c", c=64)
    mk.dma("gpsimd", Vv[0:64], fnv.t[:, 0:64].rearrange("(n m) c -> n m c", m=M), reads=[fnv], writes=[bb])
    mk.dma("gpsimd", Vv[64:128], fnv.t[:, 64:128].rearrange("(n m) c -> n m c", m=M), reads=[fnv], writes=[bb])
    TW, WCm = C["twf" + tag], C["wc" + tag]

    per = 4
    for c0 in range(0, 64, per):
        pa = _nps(B)
        for j in range(per):
            mk.mm(pa[0:M, j * 128:(j + 1) * 128], Vv[:, :, c0 + j], C["ra64"][:, :], True, True, reads=[bb, C["ra64"]], writes=[pa])
        pv = pa[0:M, :].rearrange("p (j r k) -> p j r k", j=per, r=2)
        sv = lambda t: t[0:M, 0:per * 64].rearrange("p (j k) -> p j k", j=per)
        trb = TW[0:M, 0, :].unsqueeze(1).broadcast_to([M, per, 64])
        tib = TW[0:M, 1, :].unsqueeze(1).broadcast_to([M, per, 64])
        crv = cr[0:M, c0 * 64:(c0 + per) * 64].rearrange("p (j k) -> p j k", j=per)
        civ = ci[0:M, c0 * 64:(c0 + per) * 64].rearrange("p (j k) -> p j k", j=per)
        _cmul_psum(mk, B, pv[:, :, 0, :], pv[:, :, 1, :], pa, trb, tib, [TW], crv, civ, cr, ci, sv)
    scale = 1.0 / math.sqrt(L * 64.0)
    fo = tmp[:, 0:4096].rearrange("p (k c) -> p k c", c=64)
    for blk in range(8):
        cs = slice(blk * 512, (blk + 1) * 512)
        p = _nps(B)
        mk.mm(p[0:M, :], WCm[0:M, 0, :], cr[0:M, cs], True, False, reads=[WCm, cr], writes=[p])
        mk.mm(p[0:M, :], WCm[0:M, 2, :], ci[0:M, cs], False, True, reads=[WCm, ci], writes=[p])
        ov = fo[0:M, :, blk * 8:(blk + 1) * 8].rearrange("p k c -> p c k")
        mk.op("scalar", ("activation", dict(out=ov, in_=p[0:M, :].rearrange("p (c k) -> p c k", k=64), func=ACT.Copy, scale=scale)),
              reads=[p], writes=[tmp])
    fno = d["fno" + tag]
    mk.dma("sync", fno.t.rearrange("(a k) c -> a (k c)", k=64), tmp[0:M, 0:4096], reads=[tmp], writes=[fno])


def build_p2(seqs=(("m", 128), ("c", 4))):
    nc = bass.Bass("TRN2", target_bir_lowering=False)
    mk = MK(nc)
    d = {}
    for tag, M in seqs:
        L = 64 * M
        d["hyu" + tag] = mk.dram_in("hyu" + tag, [2, L + 2, 96])
        d["fnv" + tag] = mk.dram_in("fnv" + tag, [L, 128])
        d["zfeat" + tag] = mk.dram_in("zfeat" + tag, [33, 128 * M])
        d["hyo" + tag] = mk.dram_out("hyo" + tag, [2, L, 32])
        d["fno" + tag] = mk.dram_out("fno" + tag, [L, 64])
    C = {}

    def cload(name, shape, dtype, q=None):
        dd = mk.dram_in(name, shape)
        t = mk.sb(shape, dtype)
        mk.dma(q or ("gpsimd" if dtype == BF16 else "sync"), t[:], dd[:], reads=[dd], writes=[t])
        C[name] = t
        return t
    cload("ra", [128, 256], BF16)
    cload("rf", [128, 256], BF16)
    cload("ra64", [128, 128], BF16)
    cload("l1", [128, 128], BF16)
    cload("l2", [128, 128], BF16)
    cload("ones", [128, 128], F32)
    cload("w1", [33, 64], F32)
    cload("w2", [64, 64], F32)
    cload("fa", [64, 2], F32)
    cload("fc", [64, 2], F32)
    cload("w3", [64, 2, 64], BF16)
    cload("hb", [128, 2, 32], F32)
    cload("cw", [128, 3, 96], F32)
    cload("cb", [128, 96], F32)
    for tag, M in seqs:
        d["decay" + tag] = mk.dram_in("decay" + tag, [128, M, 32])
        cload("tw" + tag, [M, 2, 128], F32)
        cload("twt" + tag, [128, 2, M], F32)
        cload("twf" + tag, [M, 2, 64], F32)
        cload("wc" + tag, [M, 3, M], BF16)
        cload("ic" + tag, [M, 2, 2 * M], BF16)
    mk.op("vector", ("tensor_tensor", dict(out=C["fc"][:], in0=C["fc"][:], in1=C["fa"][:], op=ALU.mult)), reads=[C["fa"], C["fc"]], writes=[C["fc"]])
    mk.op("vector", ("tensor_scalar_mul", dict(out=C["fc"][:], in0=C["fc"][:], scalar1=1.0 / TWO_PI)), reads=[C["fc"]], writes=[C["fc"]])
    mk.op("vector", ("tensor_scalar_mul", dict(out=C["fa"][:], in0=C["fa"][:], scalar1=1.0 / TWO_PI)), reads=[C["fa"]], writes=[C["fa"]])
    B = _p2_bufs(mk)
    for tag, M in seqs:
        emit_hyena(mk, B, M, tag, d, C)
        emit_fnet(mk, B, M, tag, d, C)
    mk.finish()
    mk.emit()
    return nc

import os as _os
MS_ENG = _os.environ.get('MS_ENG', 'vector')
NKT = 20


def build_p3a(NT=NT, NLAT=NLAT, stop=99):
    ROWS = NT * 128
    NKT_ = NLAT + 4
    nc = bass.Bass("TRN2", target_bir_lowering=False)
    mk = MK(nc)
    xs = mk.dram_in("xs", [ROWS, D])
    condT = mk.dram_in("condT", [128, 16])
    w_ada = mk.dram_in("w_ada", [D, 6 * D])
    b_ada = mk.dram_in("b_ada", [6 * D])
    n1g = mk.dram_in("n1g", [D])
    w_in = mk.dram_in("w_in", [D, 4864])
    q_d = mk.dram_in("q", [ROWS, 512])
    kvx = mk.dram_in("kvx", [NKT_ * 128, 256])
    hy_d = mk.dram_in("hy", [ROWS, 256])
    fn_d = mk.dram_in("fn", [ROWS, 256])
    w_pa = mk.dram_in("w_pa", [512, D])
    w_ph = mk.dram_in("w_ph", [256, D])
    w_pf = mk.dram_in("w_pf", [256, D])
    w_o = mk.dram_in("w_o", [D, D])
    sink = mk.dram_in("sink", [8])
    maskb = mk.dram_in("maskb", [4, 128, 512])
    ident = mk.dram_in("ident", [128, 128])
    x1_o = mk.dram_out("x1", [ROWS, D])

    psS = [mk.ps([128, 512], F32) for _ in range(3)]
    psO = mk.ps([128, 4, 65], F32)
    PT = mk.ps([128, 8, 128], BF16)
    psG = [mk.ps([128, 512], F32) for _ in range(3)]

    identb = mk.sb([128, 128], BF16)
    mk.dma("gpsimd", identb[:], ident[:], reads=[ident], writes=[identb])
    mb = mk.sb([128, 4, 512], BF16)
    mk.dma("gpsimd", mb[:], maskb.t.rearrange("i p n -> p i n"), reads=[maskb], writes=[mb])
    esink = mk.sb([128, 8], F32)
    mk.dma("sync", esink[:], sink[:].partition_broadcast(128), reads=[sink], writes=[esink])
    mk.op("scalar", ("activation", dict(out=esink[:], in_=esink[:], func=ACT.Exp)), reads=[esink], writes=[esink])
    epsb = mk.sb([128, 1], F32)
    mk.op("vector", ("memset", dict(ap=epsb[:], constant=EPS)), writes=[epsb])
    ss = mk.sb([128, NT], F32)
    rs = mk.sb([128, NT], F32)
    mk.op("vector", ("memset", dict(ap=ss[:], constant=0.0)), writes=[ss])

    WG = mk.sb([128, 8, 3072], BF16)
    mods = emit_adaln(mk, condT, w_ada, b_ada, 0, 3072, psG[0], wa_views=[(WG, WG[:, :, 0:512]), (WG, WG[:, :, 512:1024])])
    emit_modulate_setup(mk, mods, n1g, 0, 1024)

    if stop == 1:
        mk.finish(); mk.emit(); return nc
    wv = w_in.t.rearrange("(c p) n -> p c n", p=128)
    for c in range(8):
        mk.dma("gpsimd", WG[:, c, :], wv[:, c, 1792:4864], reads=[w_in], writes=[WG])
    WP = mk.sb([128, 8, D], BF16)
    mk.dma("gpsimd", WP[:, 0:4, :], w_pa.t.rearrange("(c p) n -> p c n", p=128), reads=[w_pa], writes=[WP])
    mk.dma("gpsimd", WP[:, 4:6, :], w_ph.t.rearrange("(c p) n -> p c n", p=128), reads=[w_ph], writes=[WP])
    mk.dma("gpsimd", WP[:, 6:8, :], w_pf.t.rearrange("(c p) n -> p c n", p=128), reads=[w_pf], writes=[WP])
    WO = mk.sb([128, 8, D], BF16)
    wov = w_o.t.rearrange("(c p) n -> p c n", p=128)
    for c in range(0, 8, 2):
        mk.dma("gpsimd", WO[:, c:c + 2, :], wov[:, c:c + 2, :], reads=[w_o], writes=[WO])

    if stop == 2:
        mk.finish(); mk.emit(); return nc
    KZ = [[mk.sb([128, NKT_, 128], BF16) for _ in range(2)] for _ in range(2)]
    for kv in range(2):
        for hf_ in range(2):
            mk.op(MS_ENG, ("memset", dict(ap=KZ[kv][hf_][:], constant=0.0)), writes=[KZ[kv][hf_]])
    VA = mk.sb([128, NKT_, 2, 65], BF16)
    mk.op(MS_ENG, ("memset", dict(ap=VA[:], constant=1.0)), writes=[VA])
    kvt = [mk.sb([128, 256], F32), mk.sb([128, 256], F32)]
    kb = mk.sb([128, 2, 128], BF16)
    PTk = PT
    for kt in range(NKT_):
        kf = kvt[kt % 2]
        mk.dma("sync", kf[:], kvx[kt * 128:(kt + 1) * 128, :], reads=[kvx], writes=[kf])
        mk.op("vector", ("tensor_copy", dict(out=kb[:, 0, :], in_=kf[:, 0:128])), reads=[kf], writes=[kb])
        mk.op("vector", ("tensor_copy", dict(out=kb[:, 1, 0:64], in_=kf[:, 64:128])), reads=[kf], writes=[kb])
        mk.op("vector", ("tensor_copy", dict(out=kb[:, 1, 64:128], in_=kf[:, 0:64])), reads=[kf], writes=[kb])
        mk.op("vector", ("tensor_copy", dict(out=VA[:, kt, :, 0:64], in_=kf[:, 128:256].rearrange("p (h d) -> p h d", h=2))), reads=[kf], writes=[VA])
        if stop == 31:
            mk.finish(); mk.emit(); return nc
        mk.tr(PTk[:, 0, :], kb[:, 0, :], identb[:], reads=[kb, identb], writes=[PTk])
        mk.tr(PTk[:, 1, :], kb[:, 1, :], identb[:], reads=[kb, identb], writes=[PTk])
        if stop == 32:
            mk.finish(); mk.emit(); return nc
        mk.op("scalar", ("copy", dict(out=KZ[0][0][0:64, kt, :], in_=PTk[0:64, 0, :])), reads=[PTk], writes=[KZ[0][0]])
        mk.op("scalar", ("copy", dict(out=KZ[1][1][64:128, kt, :], in_=PTk[64:128, 0, :])), reads=[PTk], writes=[KZ[1][1]])
        if stop == 33:
            mk.finish(); mk.emit(); return nc
        mk.op("scalar", ("copy", dict(out=KZ[1][0][0:64, kt, :], in_=PTk[0:64, 1, :])), reads=[PTk], writes=[KZ[1][0]])
        mk.op("scalar", ("copy", dict(out=KZ[0][1][64:128, kt, :], in_=PTk[64:128, 1, :])), reads=[PTk], writes=[KZ[0][1]])
        if stop == 34 + kt:
            mk.finish(); mk.emit(); return nc

    if stop == 3:
        mk.finish(); mk.emit(); return nc
    xts = [mk.sb([128, D], F32), mk.sb([128, D], F32)]
    hf = mk.sb([128, D], F32)
    hb = mk.sb([128, D], BF16)
    hT = mk.sb([128, 8, 128], BF16)
    qf1 = mk.sb([128, 512], F32)
    qf = [qf1, qf1]
    qb = mk.sb([128, 512], BF16)
    QT = mk.sb([128, 4, 128], BF16)
    Es = [mk.sb([128, 512], BF16) for _ in range(5)]
    den = mk.sb([128, 4], F32)
    attn = mk.sb([128, 512], BF16)
    hyf1 = mk.sb([128, 512], F32)
    hyf = [hyf1, hyf1]
    hfb = mk.sb([128, 512], BF16)
    BT = mk.sb([128, 8, 128], BF16)
    Gt = mk.sb([128, 3072], BF16)
    t1 = mk.sb([128, 512], F32)
    t2 = mk.sb([128, 512], F32)
    mbf = mk.sb([128, D], BF16)
    mT = mk.sb([128, 8, 128], BF16)
    xo1 = mk.sb([128, D], F32)
    xo = [xo1, xo1]
    si = 0
    for t in range(NT):
        r = 0 if t < NLAT else 1
        rows = slice(t * 128, (t + 1) * 128)
        if t < NLAT:
            keys = [(t, 0 if t == 0 else 1), (t + 1, None), (t + 2, 3 if t == NLAT - 1 else 2), (NLAT + 2, None), (NLAT + 3, None)]
        else:
            keys = [(NLAT + 2, None), (NLAT + 3, None)]
        q_ = qf[t % 2]
        mk.dma("sync", q_[:], q_d[rows, :], reads=[q_d], writes=[q_])
        mk.op("vector", ("tensor_copy", dict(out=qb[:], in_=q_[:])), reads=[q_], writes=[qb])
        for p_ in range(4):
            mk.tr(PT[:, p_, :], qb[:, p_ * 128:(p_ + 1) * 128], identb[:], reads=[qb, identb], writes=[PT])
        mk.op("scalar", ("copy", dict(out=QT[:], in_=PT[:, 0:4, :])), reads=[PT], writes=[QT])
        for kv in range(2):
            for ki, (kt, mi) in enumerate(keys):
                S = psS[si % 3]
                si += 1
                if mi is not None:
                    mk.mm(S[:], identb[:], mb[:, mi, :], True, False, reads=[identb, mb], writes=[S])
                for hh in range(4):
                    h = 4 * kv + hh
                    mk.mm(S[:, hh * 128:(hh + 1) * 128], KZ[kv][h % 2][:, kt, :], QT[:, h // 2, :],
                          (mi is None and hh == 0), hh == 3, reads=[KZ[kv][h % 2], QT], writes=[S])
                mk.op("scalar", ("activation", dict(out=Es[ki][:], in_=S[:], func=ACT.Exp, scale=0.125)), reads=[S], writes=[Es[ki]])
            for hh in range(4):
                for ki, (kt, mi) in enumerate(keys):
                    mk.mm(psO[:, hh, :], Es[ki][:, hh * 128:(hh + 1) * 128], VA[:, kt, kv, :], ki == 0, ki == len(keys) - 1,
                          reads=[Es[ki], VA], writes=[psO])
            mk.op("vector", ("tensor_tensor", dict(out=den[:], in0=psO[:, :, 64], in1=esink[:, 4 * kv:4 * kv + 4], op=ALU.add)), reads=[psO, esink], writes=[den])
            mk.op("vector", ("reciprocal", dict(out=den[:], in_=den[:])), reads=[den], writes=[den])
            mk.op("vector", ("tensor_tensor", dict(out=attn[:, kv * 256:(kv + 1) * 256].rearrange("p (h d) -> p h d", h=4), in0=psO[:, :, 0:64],
                                                    in1=den[:, :].unsqueeze(2).broadcast_to([128, 4, 64]), op=ALU.mult)), reads=[psO, den], writes=[attn])
        if stop == 4:
            mk.finish(); mk.emit(); return nc
        hy_ = hyf[t % 2]
        mk.dma("sync", hy_[:, 0:256], hy_d[rows, :], reads=[hy_d], writes=[hy_])
        mk.dma("sync", hy_[:, 256:512], fn_d[rows, :], reads=[fn_d], writes=[hy_])
        mk.op("vector", ("tensor_copy", dict(out=hfb[:], in_=hy_[:])), reads=[hy_], writes=[hfb])
        for c in range(4):
            mk.tr(PT[:, c, :], attn[:, c * 128:(c + 1) * 128], identb[:], reads=[attn, identb], writes=[PT])
        for c in range(4):
            mk.tr(PT[:, 4 + c, :], hfb[:, c * 128:(c + 1) * 128], identb[:], reads=[hfb, identb], writes=[PT])
        mk.op("scalar", ("copy", dict(out=BT[:], in_=PT[:])), reads=[PT], writes=[BT])
        if stop == 5:
            mk.finish(); mk.emit(); return nc
        xt = xts[t % 2]
        mk.dma("sync", xt[:], xs[rows, :], reads=[xs], writes=[xt])
        emit_norm_mod_T(mk, xt, mods[r], 0, 1024, ss, rs, t, epsb, hf, hb, PT, hT, identb)
        for nb in range(6):
            pg = psG[nb % 3]
            for c in range(8):
                mk.mm(pg[:], hT[:, c, :], WG[:, c, nb * 512:(nb + 1) * 512], c == 0, c == 7, reads=[hT, WG], writes=[pg])
            mk.op("scalar", ("activation", dict(out=Gt[:, nb * 512:(nb + 1) * 512], in_=pg[:], func=ACT.Sigmoid)), reads=[pg], writes=[Gt])
        if stop == 6:
            mk.finish(); mk.emit(); return nc
        for half in range(2):
            cs = slice(half * 512, (half + 1) * 512)
            for bi, (c0, c1) in enumerate(((0, 4), (4, 6), (6, 8))):
                for c in range(c0, c1):
                    mk.mm(psG[bi][:], BT[:, c, :], WP[:, c, cs], c == c0, c == c1 - 1, reads=[BT, WP], writes=[psG[bi]])
            mk.op("vector", ("tensor_tensor", dict(out=t1[:], in0=psG[0][:], in1=Gt[:, half * 512:(half + 1) * 512], op=ALU.mult)), reads=[psG[0], Gt], writes=[t1])
            mk.op("vector", ("tensor_tensor", dict(out=t2[:], in0=psG[1][:], in1=Gt[:, 1024 + half * 512:1024 + (half + 1) * 512], op=ALU.mult)), reads=[psG[1], Gt], writes=[t2])
            mk.op("gpsimd", ("tensor_tensor", dict(out=t1[:], in0=t1[:], in1=t2[:], op=ALU.add)), reads=[t1, t2], writes=[t1])
            mk.op("vector", ("tensor_tensor", dict(out=t2[:], in0=psG[2][:], in1=Gt[:, 2048 + half * 512:2048 + (half + 1) * 512], op=ALU.mult)), reads=[psG[2], Gt], writes=[t2])
            mk.op("gpsimd", ("tensor_tensor", dict(out=mbf[:, cs], in0=t1[:], in1=t2[:], op=ALU.add)), reads=[t1, t2], writes=[mbf])
        for c in range(8):
            mk.tr(PT[:, c, :], mbf[:, c * 128:(c + 1) * 128], identb[:], reads=[mbf, identb], writes=[PT])
        mk.op("scalar", ("copy", dict(out=mT[:], in_=PT[:])), reads=[PT], writes=[mT])
        xo_ = xo[t % 2]
        for half in range(2):
            cs = slice(half * 512, (half + 1) * 512)
            pg = psG[half]
            for c in range(8):
                mk.mm(pg[:], mT[:, c, :], WO[:, c, cs], c == 0, c == 7, reads=[mT, WO], writes=[pg])
            mk.op("vector", ("tensor_tensor", dict(out=t1[:], in0=pg[:], in1=mods[r][:, 2048 + half * 512:2048 + (half + 1) * 512], op=ALU.mult)), reads=[pg, mods[r]], writes=[t1])
            mk.op("gpsimd", ("tensor_tensor", dict(out=xo_[:, cs], in0=t1[:], in1=xt[:, cs], op=ALU.add)), reads=[t1, xt], writes=[xo_])
        mk.dma("sync", x1_o[rows, :], xo_[:], reads=[xo_], writes=[x1_o])
    mk.finish()
    mk.emit()
    return nc


def build_p3b(NE, NT=NT, NLAT=NLAT):
    ROWS = NT * 128
    FF = 2816
    NFC = FF // 128
    GF = 4
    nc = bass.Bass("TRN2", target_bir_lowering=False)
    mk = MK(nc)
    xs = mk.dram_in("xs", [ROWS, D])
    condT = mk.dram_in("condT", [128, 16])
    w_ada = mk.dram_in("w_ada", [D, 6 * D])
    b_ada = mk.dram_in("b_ada", [6 * D])
    n2g = mk.dram_in("n2g", [D])
    wg_d = mk.dram_in("wg", [NE, D, FF])
    wu_d = mk.dram_in("wu", [NE, D, FF])
    wd_d = mk.dram_in("wd", [NE, FF, D])
    ident = mk.dram_in("ident", [128, 128])
    if NE > 1:
        wr_d = mk.dram_in("wr", [D, 8])
    x2_o = mk.dram_out("x2", [ROWS, D])

    psG = [mk.ps([128, 512], F32) for _ in range(2)]
    psU = [mk.ps([128, 512], F32) for _ in range(2)]
    psY = [mk.ps([128, 512], F32) for _ in range(3)]
    PT = mk.ps([128, 8, 128], BF16)
    psR = psY[2]

    identb = mk.sb([128, 128], BF16)
    mk.dma("gpsimd", identb[:], ident[:], reads=[ident], writes=[identb])
    epsb = mk.sb([128, 1], F32)
    mk.op("vector", ("memset", dict(ap=epsb[:], constant=EPS)), writes=[epsb])
    ss = mk.sb([128, NT], F32)
    rs = mk.sb([128, NT], F32)
    mk.op("vector", ("memset", dict(ap=ss[:], constant=0.0)), writes=[ss])
    h2T = mk.sb([128, 8, ROWS], BF16)
    if ROWS >= 1024:
        wav = [(h2T, h2T[:, :, 0:512]), (h2T, h2T[:, :, 512:1024])]
    else:
        wav = None
    yacc = mk.sb([128, NT, D], F32)
    mk.op("gpsimd", ("memset", dict(ap=yacc[:], constant=0.0)), writes=[yacc])
    comb = mk.sb([128, NT, 8], F32)
    gmods = emit_adaln(mk, condT, w_ada, b_ada, 5120, 6144, psR, wa_views=wav)
    mk.phase_begin()
    mods = emit_adaln(mk, condT, w_ada, b_ada, 3072, 5120, psR, wa_views=wav)
    emit_modulate_setup(mk, mods, n2g, 0, 1024)
    if NE > 1:
        wrb = mk.sb([128, 8, 8], BF16)
        mk.dma("gpsimd", wrb[:], wr_d.t.rearrange("(c p) n -> p c n", p=128), reads=[wr_d], writes=[wrb])
    xts = [mk.sb([128, D], F32), mk.sb([128, D], F32)]
    hf = mk.sb([128, D], F32)
    hb = mk.sb([128, D], BF16)
    hT = mk.sb([128, 8, 128], BF16)
    sm = mk.sb([128, 64], F32)
    for t in range(NT):
        r = 0 if t < NLAT else 1
        xt = xts[t % 2]
        mk.dma("sync", xt[:], xs[t * 128:(t + 1) * 128, :], reads=[xs], writes=[xt])
        emit_norm_mod_T(mk, xt, mods[r], 0, 1024, ss, rs, t, epsb, hf, hb, PT, h2T, identb, hT_ap=h2T[:, :, t * 128:(t + 1) * 128])
        if NE > 1:
            for c in range(8):
                mk.mm(psR[:, 0:8], h2T[:, c, t * 128:(t + 1) * 128], wrb[:, c, :], c == 0, c == 7, reads=[h2T, wrb], writes=[psR])
            lg, m1, eq1, lg2, m2, eq2, dl, w1, w2 = (sm[:, 0:8], sm[:, 8:9], sm[:, 16:24], sm[:, 24:32], sm[:, 9:10], sm[:, 32:40],
                                                    sm[:, 10:11], sm[:, 11:12], sm[:, 12:13])
            R, W = [sm], [sm]
            mk.op("vector", ("tensor_copy", dict(out=lg, in_=psR[:, 0:8])), reads=[psR], writes=W)
            mk.op("vector", ("tensor_reduce", dict(out=m1, in_=lg, axis=AX.X, op=ALU.max)), reads=R, writes=W)
            mk.op("vector", ("tensor_scalar", dict(out=eq1, in0=lg, scalar1=m1, scalar2=None, op0=ALU.is_equal)), reads=R, writes=W)
            mk.op("vector", ("scalar_tensor_tensor", dict(out=lg2, in0=eq1, scalar=-1e30, in1=lg, op0=ALU.mult, op1=ALU.add)), reads=R, writes=W)
            mk.op("vector", ("tensor_reduce", dict(out=m2, in_=lg2, axis=AX.X, op=ALU.max)), reads=R, writes=W)
            mk.op("vector", ("tensor_scalar", dict(out=eq2, in0=lg2, scalar1=m2, scalar2=None, op0=ALU.is_equal)), reads=R, writes=W)
            mk.op("vector", ("tensor_tensor", dict(out=dl, in0=m1, in1=m2, op=ALU.subtract)), reads=R, writes=W)
            mk.op("scalar", ("activation", dict(out=w1, in_=dl, func=ACT.Sigmoid)), reads=R, writes=W)
            mk.op("scalar", ("activation", dict(out=w2, in_=dl, func=ACT.Sigmoid, scale=-1.0)), reads=R, writes=W)
            mk.op("vector", ("tensor_scalar", dict(out=eq1, in0=eq1, scalar1=w1, scalar2=None, op0=ALU.mult)), reads=R, writes=W)
            mk.op("vector", ("scalar_tensor_tensor", dict(out=comb[:, t, :], in0=eq2, scalar=w2, in1=eq1, op0=ALU.mult, op1=ALU.add)), reads=R, writes=[comb])
    mk.phase_end()
    mk.phase_begin()
    wgs = [mk.sb([128, 8, GF * 128], BF16) for _ in range(2)]
    wus = [mk.sb([128, 8, GF * 128], BF16) for _ in range(2)]
    wds = [mk.sb([128, GF, D], BF16) for _ in range(2)]
    aT = mk.sb([128, GF, ROWS], BF16)
    sg = [mk.sb([128, 512], F32), mk.sb([128, 512], F32)]
    tblocks = [(a, min(512, ROWS - a)) for a in range(0, ROWS, 512)]
    pys = [psY[0], psY[1], psY[2]]
    gi = 0
    pi = 0
    for e in range(NE):
        wgv = wg_d.t[e].rearrange("(c p) n -> p c n", p=128)
        wuv = wu_d.t[e].rearrange("(c p) n -> p c n", p=128)
        wdv = wd_d.t[e].rearrange("(c p) n -> p c n", p=128)
        for g0 in range(0, NFC, GF):
            gf = min(GF, NFC - g0)
            wg_, wu_, wd_ = wgs[gi % 2], wus[gi % 2], wds[gi % 2]
            gi += 1
            mk.dma("gpsimd", wg_[:, :, 0:gf * 128], wgv[:, :, g0 * 128:(g0 + gf) * 128], reads=[wg_d], writes=[wg_])
            mk.dma("gpsimd", wu_[:, :, 0:gf * 128], wuv[:, :, g0 * 128:(g0 + gf) * 128], reads=[wu_d], writes=[wu_])
            mk.dma("gpsimd", wd_[:, 0:gf, :], wdv[:, g0:g0 + gf, :], reads=[wd_d], writes=[wd_])
            for j in range(gf):
                for (a, w) in tblocks:
                    pg, pu = psG[pi % 2], psU[pi % 2]
                    s_ = sg[pi % 2]
                    pi += 1
                    for c in range(8):
                        mk.mm(pg[:, 0:w], wg_[:, c, j * 128:(j + 1) * 128], h2T[:, c, a:a + w], c == 0, c == 7, reads=[wg_, h2T], writes=[pg])
                    for c in range(8):
                        mk.mm(pu[:, 0:w], wu_[:, c, j * 128:(j + 1) * 128], h2T[:, c, a:a + w], c == 0, c == 7, reads=[wu_, h2T], writes=[pu])
                    mk.op("scalar", ("activation", dict(out=s_[:, 0:w], in_=pg[:, 0:w], func=ACT.Silu)), reads=[pg], writes=[s_])
                    mk.op("vector", ("tensor_tensor", dict(out=aT[:, j, a:a + w], in0=s_[:, 0:w], in1=pu[:, 0:w], op=ALU.mult)), reads=[s_, pu], writes=[aT])
            for t in range(NT):
                for half in range(2):
                    py = pys[(2 * t + half) % len(pys)]
                    cs = slice(half * 512, (half + 1) * 512)
                    for j in range(gf):
                        mk.mm(py[:], aT[:, j, t * 128:(t + 1) * 128], wd_[:, j, cs], j == 0, j == gf - 1, reads=[aT, wd_], writes=[py])
                    sc = comb[:, t, e:e + 1] if NE > 1 else 1.0
                    rd = [py, yacc] + ([comb] if NE > 1 else [])
                    eng = "vector" if half == 0 else "gpsimd"
                    if eng == "gpsimd":
                        s_ = sg[(2 * t + half) % 2]
                        mk.op("scalar", ("activation", dict(out=s_[:], in_=py[:], func=ACT.Copy, scale=sc)), reads=[py] + ([comb] if NE > 1 else []), writes=[s_])
                        mk.op("gpsimd", ("tensor_tensor", dict(out=yacc[:, t, cs], in0=s_[:], in1=yacc[:, t, cs], op=ALU.add)),
                              reads=[s_, yacc], writes=[yacc])
                    else:
                        mk.op("vector", ("scalar_tensor_tensor", dict(out=yacc[:, t, cs], in0=py[:], scalar=sc, in1=yacc[:, t, cs], op0=ALU.mult, op1=ALU.add)),
                              reads=rd, writes=[yacc])
    mk.phase_end()
    mk.phase_begin()
    xts = [mk.sb([128, D], F32), mk.sb([128, D], F32)]
    xo = [mk.sb([128, D], F32), mk.sb([128, D], F32)]
    for t in range(NT):
        r = 0 if t < NLAT else 1
        xt = xts[t % 2]
        mk.dma("sync", xt[:], xs[t * 128:(t + 1) * 128, :], reads=[xs], writes=[xt])
        xo_ = xo[t % 2]
        mk.op("vector", ("tensor_tensor", dict(out=xo_[:], in0=yacc[:, t, :], in1=gmods[r][:, 0:1024], op=ALU.mult)), reads=[yacc, gmods[r]], writes=[xo_])
        mk.op("gpsimd", ("tensor_tensor", dict(out=xo_[:], in0=xo_[:], in1=xt[:], op=ALU.add)), reads=[xo_, xt], writes=[xo_])
        mk.dma("sync", x2_o[t * 128:(t + 1) * 128, :], xo_[:], reads=[xo_], writes=[x2_o])
    mk.phase_end()
    mk.finish()
    mk.emit()
    return nc

def _rope_tables_core(qtr):
    t = np.arange(2048, dtype=np.int64) + qtr * 2048
    pos = np.stack([t // 64, t % 64], axis=-1).astype(np.float32)
    inv = (np.float32(10000.0) ** (-np.arange(16, dtype=np.float32) / np.float32(16))).astype(np.float32)
    ang = (pos[:, :, None] * inv).astype(np.float32)
    c = np.cos(ang).astype(np.float32).reshape(2048, 32)
    s = np.sin(ang).astype(np.float32).reshape(2048, 32)
    c = np.concatenate([c, np.ones((256, 32), np.float32)], 0)
    s = np.concatenate([s, np.zeros((256, 32), np.float32)], 0)
    return np.ascontiguousarray(c), np.ascontiguousarray(s)


def _dft(n):
    k = np.arange(n)
    a = 2.0 * np.pi * np.outer(k, k) / n
    return np.cos(a), np.sin(a)


def _condT(cb, c_ctx):
    cond = np.stack([cb, c_ctx], 0).astype(np.float32)
    return np.ascontiguousarray(cond.reshape(2, 8, 128).transpose(2, 0, 1).reshape(128, 16))


_CACHE = {}


def _get(name, fn):
    if name not in _CACHE:
        _CACHE[name] = fn()
    return _CACHE[name]


def p1_inmaps(inp, l, xcur, ctxcur):
    c64, s64 = _dft(64)
    f64 = np.concatenate([c64, -s64], 1).astype(np.float32)
    bd = np.zeros((128, 256), np.float32)
    bd[:64, :128] = f64
    bd[64:, 128:] = f64
    f64 = bd
    ident = np.eye(128, dtype=np.float32)
    maps = []
    for i in range(8):
        b, qtr = i // 4, i % 4
        rc, rs = _rope_tables_core(qtr)
        maps.append(dict(
            xs=np.ascontiguousarray(np.concatenate([xcur[b, qtr * 2048:(qtr + 1) * 2048], ctxcur[b]], 0)),
            condT=_condT(inp["c"][b], inp["c_ctx"]),
            w_ada=inp["w_ada"][l], b_ada=inp["b_ada"][l], n1g=inp["norm1_g"][l], w_in=inp["w_in"][l],
            qg=inp["q_norm_g"][l], kg=inp["k_norm_g"][l], ropec=rc, ropes=rs, f64=f64, ident=ident))
    return maps


def _zfeat_circ(L):
    f32 = np.float32
    t = np.linspace(0.0, 1.0, L, dtype=f32)[:, None]
    w = (f32(2.0 * math.pi) * np.arange(L, dtype=f32)[:, None] / f32(L)).astype(f32)
    fb = np.linspace(1e-4, 15, 16, dtype=f32)
    z = np.concatenate([t, np.cos(fb * w), -np.sin(fb * w)], axis=-1).astype(f32)
    zc = np.zeros((2 * L, 33), f32)
    zc[:L] = z
    zc[L + 1:] = z[1:][::-1]
    return np.ascontiguousarray(zc.T), t[:, 0]


def _decay_circ(L, M, chs):
    f32 = np.float32
    t = np.linspace(0.0, 1.0, L, dtype=f32)
    deltas = np.abs(np.linspace(math.log(1e-2) / 1.5, math.log(1e-2) / 0.3, 256, dtype=f32)).astype(f32)
    dec = np.exp(-(t[:, None] * deltas[None, chs])).astype(f32)
    dc = np.zeros((2 * L, len(chs)), f32)
    dc[:L] = dec
    dc[L + 1:] = dec[1:][::-1]
    return np.ascontiguousarray(dc.reshape(128, M, len(chs)))


def p2_consts(M):
    N = 128 * M
    L = 64 * M
    wr, ws = _dft(128)
    wi = -ws
    out = {}
    out["ra"] = np.concatenate([np.concatenate([wr[:64], wi[:64]], 1), np.concatenate([-wi[:64], wr[:64]], 1)], 0)
    out["rf"] = np.concatenate([wr, wi], 1)
    c64, s64 = _dft(64)
    w64r, w64i = c64, -s64
    out["ra64"] = np.concatenate([np.concatenate([w64r, w64i], 1), np.concatenate([-w64i, w64r], 1)], 0)
    out["l1"] = np.concatenate([wr[:, :64], -wi[:, :64]], 1)
    out["l2"] = np.concatenate([wi[:, :64], wr[:, :64]], 1)
    n2 = np.arange(M)[:, None]
    k1 = np.arange(128)[None, :]
    a = 2.0 * np.pi * n2 * k1 / N
    tw = np.stack([np.cos(a), -np.sin(a)], 1)
    out["tw"] = tw
    out["twt"] = np.ascontiguousarray(tw.transpose(2, 1, 0))
    a = 2.0 * np.pi * n2 * np.arange(64)[None, :] / L
    out["twf"] = np.stack([np.cos(a), -np.sin(a)], 1)
    mr, ms = _dft(M)
    mi = -ms
    out["wc"] = np.stack([mr, mi, -mi], 1)
    out["ic"] = np.stack([np.concatenate([mr, -mi], 1), np.concatenate([mi, mr], 1)], 1)
    return {k: np.ascontiguousarray(v.astype(np.float32)) for k, v in out.items()}


P2_SEQS = (("m", 128), ("c", 4))


def p2_inmaps(inp, l, hy_full, fnv_full, hy_ctx, fnv_ctx):
    maps = []
    cm = {tag: _get("p2c%d" % M, lambda M=M: p2_consts(M)) for tag, M in P2_SEQS}
    zf = {tag: _get("zf%d" % M, lambda M=M: _zfeat_circ(64 * M)[0]) for tag, M in P2_SEQS}
    for i in range(8):
        chs = np.arange(i * 32, (i + 1) * 32)
        b, g = i // 4, i % 4
        m = {}
        for (tag, M), hy, fv in ((P2_SEQS[0], hy_full, fnv_full), (P2_SEQS[1], hy_ctx, fnv_ctx)):
            L = 64 * M
            cols = np.concatenate([chs, 256 + chs, 512 + chs])
            hp = np.zeros((2, L + 2, 96), np.float32)
            hp[:, 1:L + 1] = hy[:, :, cols]
            m["hyu" + tag] = hp
            m["fnv" + tag] = np.ascontiguousarray(fv[b, :, g * 128:(g + 1) * 128])
            m["zfeat" + tag] = zf[tag]
            m["decay" + tag] = _get("dec%d_%d" % (M, i), lambda M=M, chs=chs: _decay_circ(64 * M, M, chs))
            for k in ("tw", "twt", "twf", "wc", "ic"):
                m[k + tag] = cm[tag][k]
        for k in ("ra", "rf", "ra64", "l1", "l2"):
            m[k] = cm["m"][k]
        m["ones"] = np.ones((128, 128), np.float32)
        m["w1"] = inp["hy_filt_w1"][l]
        m["w2"] = inp["hy_filt_w2"][l]
        m["fa"] = np.ascontiguousarray(np.stack([inp["hy_filt_freq1"][l], inp["hy_filt_freq2"][l]], 1))
        m["fc"] = np.ascontiguousarray(np.stack([inp["hy_filt_b1"][l], inp["hy_filt_b2"][l]], 1))
        w3 = inp["hy_filt_w3"][l].reshape(64, 2, 2, 256)[:, :, :, chs]
        m["w3"] = np.ascontiguousarray(w3.transpose(0, 2, 1, 3).reshape(64, 2, 64))
        m["hb"] = np.ascontiguousarray(np.broadcast_to(inp["hy_bias"][l][:, chs][None], (128, 2, 32)))
        cw = inp["hy_conv_w"][l][:, 0, :]
        cols = np.concatenate([chs, 256 + chs, 512 + chs])
        m["cw"] = np.ascontiguousarray(np.broadcast_to(cw[:, cols][None], (128, 3, 96)))
        m["cb"] = np.ascontiguousarray(np.broadcast_to(inp["hy_conv_b"][l][cols][None], (128, 96)))
        maps.append(m)
    return maps


def _masks(qtr):
    j = np.arange(128)[:, None]
    q = np.arange(128)[None, :]
    NEG = np.float32(-30000.0)
    prev = np.where(j >= q, np.float32(0), NEG).astype(np.float32)
    nxt = np.where(j <= q, np.float32(0), NEG).astype(np.float32)
    allm = np.full((128, 128), NEG, np.float32)
    ms = [allm if qtr == 0 else prev, prev, nxt, allm if qtr == 3 else nxt]
    return np.ascontiguousarray(np.stack([np.tile(m, (1, 4)) for m in ms], 0))


def p3a_inmaps(inp, l, xcur, ctxcur, q_all, kv_all, kv_ctx, hyo, fno, hyo_c, fno_c, q_ctx):
    ident = np.eye(128, dtype=np.float32)
    maps = []
    z = np.zeros((128, 256), np.float32)
    for i in range(8):
        b, qtr = i // 4, i % 4
        sl = slice(qtr * 2048, (qtr + 1) * 2048)
        prev = kv_all[b, qtr * 2048 - 128:qtr * 2048] if qtr > 0 else z
        nxt = kv_all[b, (qtr + 1) * 2048:(qtr + 1) * 2048 + 128] if qtr < 3 else z
        maps.append(dict(
            xs=np.ascontiguousarray(np.concatenate([xcur[b, sl], ctxcur[b]], 0)),
            condT=_condT(inp["c"][b], inp["c_ctx"]),
            w_ada=inp["w_ada"][l], b_ada=inp["b_ada"][l], n1g=inp["norm1_g"][l], w_in=inp["w_in"][l],
            q=np.ascontiguousarray(np.concatenate([q_all[b, sl], q_ctx[b]], 0)),
            kvx=np.ascontiguousarray(np.concatenate([prev, kv_all[b, sl], nxt, kv_ctx[b]], 0)),
            hy=np.ascontiguousarray(np.concatenate([hyo[b, sl], hyo_c[b]], 0)),
            fn=np.ascontiguousarray(np.concatenate([fno[b, sl], fno_c[b]], 0)),
            w_pa=inp["w_proj_attn"][l], w_ph=inp["w_proj_hyena"][l], w_pf=inp["w_proj_fnet"][l], w_o=inp["w_out"][l],
            sink=inp["attn_sink"][l], maskb=_masks(qtr), ident=ident))
    return maps


def p3b_inmaps(inp, l, x1, ctx1):
    ident = np.eye(128, dtype=np.float32)
    i_ = l // 2
    maps = []
    for i in range(8):
        b, qtr = i // 4, i % 4
        sl = slice(qtr * 2048, (qtr + 1) * 2048)
        m = dict(xs=np.ascontiguousarray(np.concatenate([x1[b, sl], ctx1[b]], 0)),
                 condT=_condT(inp["c"][b], inp["c_ctx"]), w_ada=inp["w_ada"][l], b_ada=inp["b_ada"][l],
                 n2g=inp["norm2_g"][l], ident=ident)
        if l % 2 == 0:
            m.update(wg=inp["ffn_w_gate"][i_][None], wu=inp["ffn_w_up"][i_][None], wd=inp["ffn_w_down"][i_][None])
        else:
            m.update(wg=inp["moe_w_gate"][i_], wu=inp["moe_w_up"][i_], wd=inp["moe_w_down"][i_], wr=inp["moe_router"][i_])
        maps.append(m)
    return maps


_NC = {}


def _prog(name, fn):
    if name not in _NC:
        _NC[name] = fn()
    return _NC[name]


def _run(nc, maps):
    res = run_bass_kernel_spmd(nc, maps, core_ids=list(range(8)))
    return res.results


def _gather_tok(results, key, width):
    lat = np.empty((2, 8192, width), np.float32)
    cx = np.empty((2, 256, width), np.float32)
    for i in range(8):
        b, qtr = i // 4, i % 4
        lat[b, qtr * 2048:(qtr + 1) * 2048] = results[i][key][:2048]
        if qtr == 0:
            cx[b] = results[i][key][2048:]
    return lat, cx


def kernel(**inputs):
    inp = {k: np.ascontiguousarray(np.asarray(v, dtype=np.float32)) for k, v in inputs.items()}
    x = inp["x"]
    ctx = inp["ctx"]
    for l in range(4):
        r1 = _run(_prog("p1", build_p1), p1_inmaps(inp, l, x, ctx))
        q_all, q_ctx = _gather_tok(r1, "q_o", 512)
        kv_all, kv_ctx = _gather_tok(r1, "kv_o", 256)
        hy_all, hy_ctx = _gather_tok(r1, "hy_o", 768)
        fv_all, fv_ctx = _gather_tok(r1, "fnv_o", 512)
        del r1
        r2 = _run(_prog("p2", build_p2), p2_inmaps(inp, l, hy_all, fv_all, hy_ctx, fv_ctx))
        hyo = np.concatenate([r2[i]["hyom"] for i in range(8)], -1)
        hyo_c = np.concatenate([r2[i]["hyoc"] for i in range(8)], -1)
        fno = np.stack([np.concatenate([r2[b * 4 + g]["fnom"] for g in range(4)], -1) for b in range(2)], 0)
        fno_c = np.stack([np.concatenate([r2[b * 4 + g]["fnoc"] for g in range(4)], -1) for b in range(2)], 0)
        del r2
        r3 = _run(_prog("p3a", build_p3a), p3a_inmaps(inp, l, x, ctx, q_all, kv_all, kv_ctx, hyo, fno, hyo_c, fno_c, q_ctx))
        x1, ctx1 = _gather_tok(r3, "x1", 1024)
        del r3
        ne = 1 if l % 2 == 0 else 8
        r4 = _run(_prog("p3b%d" % ne, lambda: build_p3b(ne)), p3b_inmaps(inp, l, x1, ctx1))
        x, ctx = _gather_tok(r4, "x2", 1024)
        del r4
    return x
```

```python
import contextlib
import numpy as np
import concourse.bass as bass
import concourse.mybir as mybir
from concourse.bass_utils import run_bass_kernel_spmd

F32 = mybir.dt.float32
BF16 = mybir.dt.bfloat16
ALU = mybir.AluOpType
ACT = mybir.ActivationFunctionType
AX = mybir.AxisListType
NPOOL = 12


class Buf:
    def __init__(self, t):
        self.t = t
        self.w = None
        self.r = {}

    def __getitem__(self, k):
        return self.t[k]


class MK:
    def __init__(self, nc):
        self.nc = nc
        self.es = contextlib.ExitStack()
        self.sems = {}
        self.engs = {}
        for name in ["tensor", "vector", "scalar", "gpsimd", "sync"]:
            sem = self.es.enter_context(nc.semaphore("s_" + name))
            self.sems[name] = sem
            self.engs[name] = dict(key=name, cnt=0, seen={}, ops=[])
        self.pool = {}
        self.rr = {}
        for q in ["sync", "gpsimd", "scalar"]:
            self.pool[q] = []
            for i in range(NPOOL):
                key = "d_%s%d" % (q, i)
                self.sems[key] = self.es.enter_context(nc.semaphore(key))
                self.pool[q].append(dict(key=key, val=0))
            self.rr[q] = 0
        self.nbuf = 0

    def phase_begin(self):
        self.pes = contextlib.ExitStack()

    def phase_end(self):
        self.barrier()
        self.pes.close()
        self.pes = None

    def sb(self, shape, dtype, name=None):
        self.nbuf += 1
        st = self.pes if getattr(self, "pes", None) is not None else self.es
        t = st.enter_context(self.nc.sbuf_tensor(name or ("sb%d" % self.nbuf), list(shape), dtype))
        return Buf(t)

    def ps(self, shape, dtype, name=None):
        self.nbuf += 1
        st = self.pes if getattr(self, "pes", None) is not None else self.es
        t = st.enter_context(self.nc.psum_tensor(name or ("ps%d" % self.nbuf), list(shape), dtype))
        return Buf(t)

    def dram_in(self, name, shape, dtype=F32):
        return Buf(self.nc.dram_tensor(name, list(shape), dtype, kind="ExternalInput").ap())

    def dram_out(self, name, shape, dtype=F32):
        return Buf(self.nc.dram_tensor(name, list(shape), dtype, kind="ExternalOutput").ap())

    def dram_tmp(self, name, shape, dtype=F32):
        return Buf(self.nc.dram_tensor(name, list(shape), dtype, kind="Internal").ap())

    def _deps(self, E, reads, writes, skip_self):
        deps = {}

        def need(ev):
            if ev is None:
                return
            k, v = ev
            if deps.get(k, 0) < v:
                deps[k] = v

        for b in reads:
            need(b.w)
        for b in writes:
            need(b.w)
            for k, v in b.r.items():
                need((k, v))
        waits = []
        for k, v in deps.items():
            if skip_self and k == E["key"]:
                continue
            if E["seen"].get(k, 0) >= v:
                continue
            E["seen"][k] = v
            waits.append((k, v))
        return waits

    def _mark(self, ev, reads, writes):
        k, v = ev
        for b in reads:
            if b.r.get(k, 0) < v:
                b.r[k] = v
        for b in writes:
            b.w = ev
            b.r = {}

    def op(self, en, fn, reads=(), writes=()):
        E = self.engs[en]
        waits = self._deps(E, reads, writes, skip_self=(en == "tensor"))
        E["cnt"] += 1
        ev = (E["key"], E["cnt"])
        if isinstance(fn, tuple):
            nm, kw = fn
            fn = (lambda e, nm=nm, kw=kw: getattr(e, nm)(**kw))
        E["ops"].append((waits, fn, (E["key"], 1)))
        self._mark(ev, reads, writes)

    def dma(self, q, out, in_, reads=(), writes=(), fn=None, **kw):
        E = self.engs[q]
        p = self.pool[q][self.rr[q] % NPOOL]
        self.rr[q] += 1
        waits = self._deps(E, reads, writes, skip_self=False)
        if p["val"] > 0 and E["seen"].get(p["key"], 0) < p["val"]:
            E["seen"][p["key"]] = p["val"]
            waits.append((p["key"], p["val"]))
        p["val"] += 16
        ev = (p["key"], p["val"])
        if fn is None:
            fn = (lambda e, out=out, in_=in_, kw=kw: e.dma_start(out=out, in_=in_, **kw))
        E["ops"].append((waits, fn, (p["key"], 16)))
        self._mark(ev, reads, writes)

    def barrier(self):
        allw = []
        for q in self.pool:
            for p in self.pool[q]:
                if p["val"] > 0:
                    allw.append((p["key"], p["val"]))
        for name in self.engs:
            c = self.engs[name]["cnt"]
            if c > 0:
                allw.append((name, c))
        for name, E in self.engs.items():
            waits = []
            for k, v in allw:
                if k == name:
                    continue
                if E["seen"].get(k, 0) >= v:
                    continue
                E["seen"][k] = v
                waits.append((k, v))
            E["ops"].append((waits, None, None))

    def finish(self):
        E = self.engs["sync"]
        waits = []
        for q in self.pool:
            for p in self.pool[q]:
                if p["val"] > 0:
                    waits.append((p["key"], p["val"]))
        for name in ["tensor", "vector", "scalar", "gpsimd"]:
            c = self.engs[name]["cnt"]
            if c > 0:
                waits.append((name, c))
        E["ops"].append((waits, None, None))

    def emit(self):
        nc = self.nc
        sems = self.sems
        engs = self.engs

        def run(e, name):
            for waits, fn, inc in engs[name]["ops"]:
                for k, v in waits:
                    e.wait_ge(sems[k], v)
                if fn is None:
                    continue
                ins = fn(e)
                ins.then_inc(sems[inc[0]], inc[1])

        with nc.Block() as block:
            @block.sync
            def _(e):
                run(e, "sync")

            @block.tensor
            def _(e):
                run(e, "tensor")

            @block.vector
            def _(e):
                run(e, "vector")

            @block.scalar
            def _(e):
                run(e, "scalar")

            @block.gpsimd
            def _(e):
                run(e, "gpsimd")
        self.es.close()

    def mm(self, out, lhsT, rhs, start, stop, reads, writes):
        self.op("tensor", lambda e, out=out, lhsT=lhsT, rhs=rhs, start=start, stop=stop: e.matmul(out, lhsT, rhs, start=start, stop=stop),
                reads=reads, writes=writes)

    def tr(self, out, in_, ident, reads, writes):
        self.op("tensor", lambda e, out=out, in_=in_, ident=ident: e.transpose(out, in_, ident), reads=reads, writes=writes)
import math

D = 1024
NLAT = 16
NT = 18
ROWS = NT * 128
EPS = 1e-6


def emit_adaln(mk, condT_d, w_ada_d, b_ada_d, c0, c1, psb, wa_views=None):
    n = c1 - c0
    ct = mk.sb([128, 16], F32)
    mk.dma("sync", ct[:], condT_d[:], reads=[condT_d], writes=[ct])
    sc = mk.sb([128, 16], F32)
    mk.op("scalar", ("activation", dict(out=sc[:], in_=ct[:], func=ACT.Silu)), reads=[ct], writes=[sc])
    LB = mk.sb([128, 16, 128], BF16)
    mk.op("vector", ("tensor_copy", dict(out=LB[:], in_=sc[:, :].unsqueeze(2).broadcast_to([128, 16, 128]))),
          reads=[sc], writes=[LB])
    bbs = [mk.sb([128, 512], F32), mk.sb([128, 512], F32)]
    mods = [mk.sb([128, n], F32), mk.sb([128, n], F32)]
    if wa_views is None:
        w0, w1 = mk.sb([128, 8, 512], BF16), mk.sb([128, 8, 512], BF16)
        wa_views = [(w0, w0[:, :, :]), (w1, w1[:, :, :])]
    wv = w_ada_d.t.rearrange("(c p) n -> p c n", p=128)
    for nb in range(n // 512):
        wa, wap = wa_views[nb % 2]
        bb = bbs[nb % 2]
        mk.dma("sync", bb[:], b_ada_d[c0 + nb * 512:c0 + (nb + 1) * 512].partition_broadcast(128), reads=[b_ada_d], writes=[bb])
        mk.dma("gpsimd", wap, wv[:, :, c0 + nb * 512:c0 + (nb + 1) * 512], reads=[w_ada_d], writes=[wa])
        for r in range(2):
            for c in range(8):
                mk.mm(psb[:], LB[:, r * 8 + c, :], wap[:, c, :], c == 0, c == 7, reads=[LB, wa], writes=[psb])
            mk.op("vector", ("tensor_tensor", dict(out=mods[r][:, nb * 512:(nb + 1) * 512], in0=psb[:], in1=bb[:], op=ALU.add)),
                  reads=[psb, bb], writes=[mods[r]])
    return mods


def emit_modulate_setup(mk, mods, g_d, sh_off, sc_off):
    gb = mk.sb([128, D], F32)
    mk.dma("sync", gb[:], g_d[:].partition_broadcast(128), reads=[g_d], writes=[gb])
    for r in range(2):
        m = mods[r]
        mk.op("vector", ("scalar_tensor_tensor", dict(out=m[:, sc_off:sc_off + D], in0=m[:, sc_off:sc_off + D], scalar=1.0,
                                                               in1=gb[:], op0=ALU.add, op1=ALU.mult)),
              reads=[m, gb], writes=[m])


def emit_norm_mod_T(mk, xt, m, sh_off, sc_off, ss, rs, col, epsb, hf, hb, PT, hT, identb, hT_ap=None):
    mk.op("scalar", ("activation", dict(out=hf[:], in_=xt[:], func=ACT.Square, accum_out=ss[:, col:col + 1])),
          reads=[xt], writes=[hf, ss])
    mk.op("scalar", ("activation", dict(out=rs[:, col:col + 1], in_=ss[:, col:col + 1], func=ACT.Sqrt, scale=1.0 / D, bias=epsb[:, 0:1])),
          reads=[ss, epsb], writes=[rs])
    mk.op("vector", ("reciprocal", dict(out=rs[:, col:col + 1], in_=rs[:, col:col + 1])), reads=[rs], writes=[rs])
    mk.op("vector", ("scalar_tensor_tensor", dict(out=hf[:], in0=xt[:], scalar=rs[:, col:col + 1], in1=m[:, sc_off:sc_off + D],
                                                     op0=ALU.mult, op1=ALU.mult)), reads=[xt, rs, m], writes=[hf])
    mk.op("vector", ("tensor_tensor", dict(out=hb[:], in0=hf[:], in1=m[:, sh_off:sh_off + D], op=ALU.add)),
          reads=[hf, m], writes=[hb])
    for c in range(8):
        mk.tr(PT[:, c, :], hb[:, c * 128:(c + 1) * 128], identb[:], reads=[hb, identb], writes=[PT])
    mk.op("scalar", ("copy", dict(out=(hT[:] if hT_ap is None else hT_ap), in_=PT[:])), reads=[PT], writes=[hT])


def emit_headnorm_rope(mk, src_ap, src_buf, nh, gain_b, cs_c, cs_s, tabs, sqb, qs, qn, qo, epsb):
    w = nh * 64
    mk.op("scalar", ("activation", dict(out=sqb[:, 0:w], in_=src_ap, func=ACT.Square)), reads=[src_buf], writes=[sqb])
    mk.op("vector", ("tensor_reduce", dict(out=qs[:, 0:nh], in_=sqb[:, 0:w].rearrange("p (h d) -> p h d", h=nh), axis=AX.X, op=ALU.add)),
          reads=[sqb], writes=[qs])
    mk.op("scalar", ("activation", dict(out=qs[:, 0:nh], in_=qs[:, 0:nh], func=ACT.Sqrt, scale=1.0 / 64, bias=epsb[:, 0:1])),
          reads=[qs, epsb], writes=[qs])
    mk.op("vector", ("reciprocal", dict(out=qs[:, 0:nh], in_=qs[:, 0:nh])), reads=[qs], writes=[qs])
    qn3 = qn[:, 0:w].rearrange("p (h d) -> p h d", h=nh)
    mk.op("vector", ("tensor_tensor", dict(out=qn3, in0=src_ap.rearrange("p (h d) -> p h d", h=nh),
                                              in1=qs[:, 0:nh].unsqueeze(2).broadcast_to([128, nh, 64]), op=ALU.mult)),
          reads=[src_buf, qs], writes=[qn])
    mk.op("vector", ("tensor_tensor", dict(out=qn3, in0=qn3, in1=gain_b[:, :].unsqueeze(1).broadcast_to([128, nh, 64]), op=ALU.mult)),
          reads=[qn, gain_b], writes=[qn])
    v5 = qn[:, 0:w].rearrange("p (h a j f) -> p h a j f", h=nh, a=2, j=2)
    o5 = qo[:, 0:w].rearrange("p (h a j f) -> p h a j f", h=nh, a=2, j=2)
    t5 = sqb[:, 0:w].rearrange("p (h a j f) -> p h a j f", h=nh, a=2, j=2)
    x1, x2 = v5[:, :, :, 0, :], v5[:, :, :, 1, :]
    cb = cs_c.unsqueeze(1).broadcast_to([128, nh, 2, 16])
    sb_ = cs_s.unsqueeze(1).broadcast_to([128, nh, 2, 16])
    rd = [qn]
    mk.op("vector", ("tensor_tensor", dict(out=t5[:, :, :, 0, :], in0=x1, in1=cb, op=ALU.mult)), reads=rd + tabs, writes=[sqb])
    mk.op("vector", ("tensor_tensor", dict(out=t5[:, :, :, 1, :], in0=x2, in1=sb_, op=ALU.mult)), reads=rd + tabs, writes=[sqb])
    mk.op("vector", ("tensor_tensor", dict(out=o5[:, :, :, 0, :], in0=t5[:, :, :, 0, :], in1=t5[:, :, :, 1, :], op=ALU.subtract)),
          reads=[sqb], writes=[qo])
    mk.op("vector", ("tensor_tensor", dict(out=t5[:, :, :, 0, :], in0=x1, in1=sb_, op=ALU.mult)), reads=rd + tabs, writes=[sqb])
    mk.op("vector", ("tensor_tensor", dict(out=t5[:, :, :, 1, :], in0=x2, in1=cb, op=ALU.mult)), reads=rd + tabs, writes=[sqb])
    mk.op("vector", ("tensor_tensor", dict(out=o5[:, :, :, 1, :], in0=t5[:, :, :, 0, :], in1=t5[:, :, :, 1, :], op=ALU.add)),
          reads=[sqb], writes=[qo])


def build_p1(NT=NT, NLAT=NLAT):
    ROWS = NT * 128
    nc = bass.Bass("TRN2", target_bir_lowering=False)
    mk = MK(nc)
    xs = mk.dram_in("xs", [ROWS, D])
    condT = mk.dram_in("condT", [128, 16])
    w_ada = mk.dram_in("w_ada", [D, 6 * D])
    b_ada = mk.dram_in("b_ada", [6 * D])
    n1g = mk.dram_in("n1g", [D])
    w_in = mk.dram_in("w_in", [D, 4864])
    qg = mk.dram_in("qg", [64])
    kg = mk.dram_in("kg", [64])
    ropec = mk.dram_in("ropec", [ROWS, 32])
    ropes = mk.dram_in("ropes", [ROWS, 32])
    f64 = mk.dram_in("f64", [128, 256])
    ident = mk.dram_in("ident", [128, 128])
    q_o = mk.dram_out("q_o", [ROWS, 512])
    kv_o = mk.dram_out("kv_o", [ROWS, 256])
    hy_o = mk.dram_out("hy_o", [ROWS, 768])
    fnv_o = mk.dram_out("fnv_o", [ROWS, 512])

    psAs = [mk.ps([128, 512], F32), mk.ps([128, 512], F32)]
    psBs = [mk.ps([128, 512], F32), mk.ps([128, 512], F32)]
    psA = psAs[0]
    psC = mk.ps([128, 512], F32)
    psFV = mk.ps([128, 512], F32)
    PTs = [mk.ps([128, 8, 128], BF16), mk.ps([128, 8, 128], BF16)]

    identb = mk.sb([128, 128], BF16)
    mk.dma("gpsimd", identb[:], ident[:], reads=[ident], writes=[identb])
    f64b = mk.sb([128, 256], BF16)
    mk.dma("gpsimd", f64b[:], f64[:], reads=[f64], writes=[f64b])
    epsb = mk.sb([128, 1], F32)
    mk.op("vector", ("memset", dict(ap=epsb[:], constant=EPS)), writes=[epsb])
    ss = mk.sb([128, NT], F32)
    rs = mk.sb([128, NT], F32)
    mk.op("vector", ("memset", dict(ap=ss[:], constant=0.0)), writes=[ss])
    qgb = mk.sb([128, 64], F32)
    kgb = mk.sb([128, 64], F32)
    mk.dma("sync", qgb[:], qg[:].partition_broadcast(128), reads=[qg], writes=[qgb])
    mk.dma("sync", kgb[:], kg[:].partition_broadcast(128), reads=[kg], writes=[kgb])
    rc = mk.sb([128, NT, 32], F32)
    rsn = mk.sb([128, NT, 32], F32)
    mk.dma("sync", rc[:], ropec.t.rearrange("(t p) f -> p t f", p=128), reads=[ropec], writes=[rc])
    mk.dma("sync", rsn[:], ropes.t.rearrange("(t p) f -> p t f", p=128), reads=[ropes], writes=[rsn])

    mods = emit_adaln(mk, condT, w_ada, b_ada, 0, 2048, psA)
    emit_modulate_setup(mk, mods, n1g, 0, 1024)

    WB = mk.sb([128, 8, 1792], BF16)
    wv = w_in.t.rearrange("(c p) n -> p c n", p=128)
    for c in range(8):
        mk.dma("gpsimd", WB[:, c, :], wv[:, c, 0:1792], reads=[w_in], writes=[WB])

    xts = [mk.sb([128, D], F32), mk.sb([128, D], F32)]
    hfs = [mk.sb([128, D], F32), mk.sb([128, D], F32)]
    hbs = [mk.sb([128, D], BF16), mk.sb([128, D], BF16)]
    hTs = [mk.sb([128, 8, 128], BF16), mk.sb([128, 8, 128], BF16)]
    sqbs = [mk.sb([128, 512], F32) for _ in range(2)]
    qss = [mk.sb([128, 8], F32) for _ in range(2)]
    qns = [mk.sb([128, 512], F32) for _ in range(2)]
    sqbk = [mk.sb([128, 128], F32) for _ in range(2)]
    qsk = [mk.sb([128, 8], F32) for _ in range(2)]
    qnk = [mk.sb([128, 128], F32) for _ in range(2)]
    qos = [mk.sb([128, 512], F32), mk.sb([128, 512], F32)]
    kvos = [mk.sb([128, 256], F32), mk.sb([128, 256], F32)]
    hyos = [mk.sb([128, 768], F32), mk.sb([128, 768], F32)]
    fnTs = [mk.sb([128, 2, 128], BF16), mk.sb([128, 2, 128], BF16)]
    fvs = [mk.sb([128, 512], F32), mk.sb([128, 512], F32)]

    for t in range(NT):
        r = 0 if t < NLAT else 1
        xt = xts[t % 2]
        hT = hTs[t % 2]
        qo, kvo, hyo, fv = qos[t % 2], kvos[t % 2], hyos[t % 2], fvs[t % 2]
        rows = slice(t * 128, (t + 1) * 128)
        psA, psB = psAs[t % 2], psBs[t % 2]
        psF = psV = psFV
        hf, hb, sqb, qs, qn, fnT = hfs[t % 2], hbs[t % 2], sqbs[t % 2], qss[t % 2], qns[t % 2], fnTs[t % 2]
        PT = PTs[t % 2]
        mk.dma("sync", xt[:], xs[rows, :], reads=[xs], writes=[xt])
        emit_norm_mod_T(mk, xt, mods[r], 0, 1024, ss, rs, t, epsb, hf, hb, PT, hT, identb)
        for (pb, c0) in ((psA, 0), (psB, 512), (psC, 1024)):
            for c in range(8):
                mk.mm(pb[:], hT[:, c, :], WB[:, c, c0:c0 + 512], c == 0, c == 7, reads=[hT, WB], writes=[pb])
        for blk in range(2):
            for c in range(8):
                mk.mm(psF[:, blk * 128:(blk + 1) * 128], WB[:, c, 1536 + blk * 128:1536 + (blk + 1) * 128], hT[:, c, :], c == 0, c == 7,
                      reads=[hT, WB], writes=[psF])
        mk.op("scalar", ("copy", dict(out=fnT[:], in_=psF[:, 0:256].rearrange("p (b n) -> p b n", b=2))), reads=[psF], writes=[fnT])
        for blk in range(2):
            mk.mm(psV[:, blk * 256:(blk + 1) * 256], fnT[:, blk, :], f64b[:, :], True, True, reads=[fnT, f64b], writes=[psV])
        mk.op("scalar", ("copy", dict(out=fv[:], in_=psV[:])), reads=[psV], writes=[fv])
        mk.dma("gpsimd", fnv_o[rows, :], fv[:], reads=[fv], writes=[fnv_o])
        emit_headnorm_rope(mk, psA[:, :], psA, 8, qgb, rc[:, t, :].rearrange("p (a f) -> p a f", a=2),
                           rsn[:, t, :].rearrange("p (a f) -> p a f", a=2), [rc, rsn], sqb, qs, qn, qo, epsb)
        mk.dma("gpsimd", q_o[rows, :], qo[:], reads=[qo], writes=[q_o])
        emit_headnorm_rope(mk, psB[:, 0:128], psB, 2, kgb, rc[:, t, :].rearrange("p (a f) -> p a f", a=2),
                           rsn[:, t, :].rearrange("p (a f) -> p a f", a=2), [rc, rsn], sqbk[t % 2], qsk[t % 2], qnk[t % 2], kvo, epsb)
        mk.op("scalar", ("copy", dict(out=kvo[:, 128:256], in_=psB[:, 128:256])), reads=[psB], writes=[kvo])
        mk.dma("gpsimd", kv_o[rows, :], kvo[:], reads=[kvo], writes=[kv_o])
        mk.op("scalar", ("copy", dict(out=hyo[:, 0:256], in_=psB[:, 256:512])), reads=[psB], writes=[hyo])
        mk.op("scalar", ("copy", dict(out=hyo[:, 256:768], in_=psC[:, :])), reads=[psC], writes=[hyo])
        mk.dma("gpsimd", hy_o[rows, :], hyo[:], reads=[hyo], writes=[hy_o])
    mk.finish()
    mk.emit()
    return nc

TWO_PI = 2.0 * math.pi


def _p2_bufs(mk):
    B = {}
    B["uh"] = [mk.sb([128, 130, 32], F32) for _ in range(2)]
    B["uc"] = [mk.sb([128, 128, 32], F32) for _ in range(3)]
    B["tmp"] = mk.sb([128, 128 * 32], F32)
    B["zb"] = mk.sb([128, 128, 32], BF16)
    B["cr"] = mk.sb([128, 4096], BF16)
    B["ci"] = mk.sb([128, 4096], BF16)
    B["bb"] = mk.sb([128, 2, 4096], BF16)
    B["h2t"] = mk.sb([64, 16384], BF16)
    B["ta"] = mk.sb([128, 512], F32)
    B["tb"] = mk.sb([128, 512], F32)
    B["tc"] = mk.sb([128, 512], F32)
    B["td"] = mk.sb([128, 512], F32)
    B["zt"] = [mk.sb([33, 512], F32), mk.sb([33, 512], F32)]
    B["tiq"] = mk.sb([64, 512], mybir.dt.int32)
    B["h1"] = mk.sb([64, 512], F32)
    B["dec"] = [mk.sb([128, 8, 32], F32), mk.sb([128, 8, 32], F32)]
    B["deci"] = 0
    B["asum"] = mk.sb([128, 32], F32)
    B["rn"] = mk.sb([128, 32], F32)
    B["ps"] = [mk.ps([128, 512], F32) for _ in range(6)]
    B["psi"] = 0
    return B


def _nps(B):
    p = B["ps"][B["psi"] % len(B["ps"])]
    B["psi"] += 1
    return p


def _cmul_psum(mk, B, pr_ap, pi_ap, pbuf, tr_ap, ti_ap, tbufs, outr_ap, outi_ap, outr_buf, outi_buf, shape_view, conj=False):
    ta, tb, tc, td = B["ta"], B["tb"], B["tc"], B["td"]
    va, vb, vc, vd = shape_view(ta), shape_view(tb), shape_view(tc), shape_view(td)
    mk.op("vector", ("tensor_tensor", dict(out=va, in0=pr_ap, in1=tr_ap, op=ALU.mult)), reads=[pbuf] + tbufs, writes=[ta])
    mk.op("vector", ("tensor_tensor", dict(out=vb, in0=pi_ap, in1=ti_ap, op=ALU.mult)), reads=[pbuf] + tbufs, writes=[tb])
    mk.op("gpsimd", ("tensor_tensor", dict(out=outr_ap, in0=va, in1=vb, op=(ALU.add if conj else ALU.subtract))),
          reads=[ta, tb], writes=[outr_buf])
    mk.op("vector", ("tensor_tensor", dict(out=vc, in0=pr_ap, in1=ti_ap, op=ALU.mult)), reads=[pbuf] + tbufs, writes=[tc])
    mk.op("vector", ("tensor_tensor", dict(out=vd, in0=pi_ap, in1=tr_ap, op=ALU.mult)), reads=[pbuf] + tbufs, writes=[td])
    if conj:
        mk.op("gpsimd", ("tensor_tensor", dict(out=outi_ap, in0=vd, in1=vc, op=ALU.subtract)), reads=[tc, td], writes=[outi_buf])
    else:
        mk.op("gpsimd", ("tensor_tensor", dict(out=outi_ap, in0=vc, in1=vd, op=ALU.add)), reads=[tc, td], writes=[outi_buf])


def emit_fft_fwd(mk, B, M, src, nch, RAb, TW, WC, w):
    cr, ci = B["cr"], B["ci"]
    per = 512 // (2 * w)
    for c0 in range(0, nch, per):
        pa = _nps(B)
        for j in range(per):
            mk.mm(pa[0:M, j * 2 * w:(j + 1) * 2 * w], src[:, 0:M, c0 + j], RAb[:, :], True, True, reads=[src, RAb], writes=[pa])
        pv = pa[0:M, :].rearrange("p (j r k) -> p j r k", j=per, r=2)
        sv = lambda t: t[0:M, 0:per * w].rearrange("p (j k) -> p j k", j=per)
        trb = TW[0:M, 0, :].unsqueeze(1).broadcast_to([M, per, w])
        tib = TW[0:M, 1, :].unsqueeze(1).broadcast_to([M, per, w])
        crv = cr[0:M, c0 * w:(c0 + per) * w].rearrange("p (j k) -> p j k", j=per)
        civ = ci[0:M, c0 * w:(c0 + per) * w].rearrange("p (j k) -> p j k", j=per)
        _cmul_psum(mk, B, pv[:, :, 0, :], pv[:, :, 1, :], pa, trb, tib, [TW], crv, civ, cr, ci, sv)


def emit_hyena(mk, B, M, tag, d, C):
    L = 64 * M
    NCIRC = 128 * M
    uh, uc, tmp, zb, cr, ci, bb, h2t = B["uh"], B["uc"], B["tmp"], B["zb"], B["cr"], B["ci"], B["bb"], B["h2t"]
    hyu = d["hyu" + tag]
    cw, cb = C["cw"], C["cb"]
    tv = tmp[:, 0:M * 32].rearrange("p (m c) -> p m c", c=32)
    for comp in range(3):
        cs = slice(comp * 32, (comp + 1) * 32)
        uhc = uh[comp % 2]
        for b in range(2):
            ps_ = slice(b * 64, (b + 1) * 64)
            mk.dma("sync", uhc[ps_, 1:M + 1, :], hyu.t[b, 1:L + 1, cs].rearrange("(n m) c -> n m c", m=M), reads=[hyu], writes=[uhc])
            mk.dma("sync", uhc[ps_, 0, :], hyu.t[b, 0:L, cs].rearrange("(n m) c -> n m c", m=M)[:, 0, :], reads=[hyu], writes=[uhc])
            mk.dma("sync", uhc[ps_, M + 1, :], hyu.t[b, 2:L + 2, cs].rearrange("(n m) c -> n m c", m=M)[:, M - 1, :], reads=[hyu], writes=[uhc])
        o = uc[comp][:, 0:M, :]
        wb = lambda k, cs=cs: cw[:, k, cs].unsqueeze(1).broadcast_to([128, M, 32])
        mk.op("vector", ("tensor_tensor", dict(out=o, in0=uhc[:, 0:M, :], in1=wb(0), op=ALU.mult)), reads=[uhc, cw], writes=[uc[comp]])
        mk.op("gpsimd", ("tensor_tensor", dict(out=tv, in0=uhc[:, 1:M + 1, :], in1=wb(1), op=ALU.mult)), reads=[uhc, cw], writes=[tmp])
        mk.op("vector", ("tensor_tensor", dict(out=o, in0=o, in1=tv, op=ALU.add)), reads=[uc[comp], tmp], writes=[uc[comp]])
        mk.op("gpsimd", ("tensor_tensor", dict(out=tv, in0=uhc[:, 2:M + 2, :], in1=wb(2), op=ALU.mult)), reads=[uhc, cw], writes=[tmp])
        mk.op("vector", ("tensor_tensor", dict(out=o, in0=o, in1=tv, op=ALU.add)), reads=[uc[comp], tmp], writes=[uc[comp]])
        mk.op("vector", ("tensor_tensor", dict(out=o, in0=o, in1=cb[:, cs].unsqueeze(1).broadcast_to([128, M, 32]), op=ALU.add)),
              reads=[uc[comp], cb], writes=[uc[comp]])
    zf = d["zfeat" + tag]
    zt_s = B["zt"]
    tq, tiq, tfq = B["ta"], B["tiq"], B["tb"]
    h1 = B["h1"]
    nblk = max(1, NCIRC // 512)
    bw = min(512, NCIRC)
    for blk in range(nblk):
        zt = zt_s[blk % 2]
        mk.dma("sync", zt[:, 0:bw], zf[:, blk * bw:(blk + 1) * bw], reads=[zf], writes=[zt])
        src, srcb = zt, None
        for layer in range(2):
            wl = C["w1"] if layer == 0 else C["w2"]
            kk = 33 if layer == 0 else 64
            p = _nps(B)
            rhs = zt[0:33, 0:bw] if layer == 0 else h1[:, 0:bw]
            rb = zt if layer == 0 else h1
            mk.mm(p[0:64, 0:bw], wl[0:kk, :], rhs, True, True, reads=[wl, rb], writes=[p])
            a_, c_ = C["fa"][:, layer:layer + 1], C["fc"][:, layer:layer + 1]
            mk.op("vector", ("tensor_scalar", dict(out=tq[0:64, 0:bw], in0=p[0:64, 0:bw], scalar1=a_, scalar2=c_, op0=ALU.mult, op1=ALU.add)),
                  reads=[p, C["fa"], C["fc"]], writes=[tq])
            mk.op("vector", ("tensor_copy", dict(out=tiq[:, 0:bw], in_=tq[0:64, 0:bw])), reads=[tq], writes=[tiq])
            mk.op("vector", ("tensor_copy", dict(out=tfq[0:64, 0:bw], in_=tiq[:, 0:bw])), reads=[tiq], writes=[tfq])
            mk.op("vector", ("tensor_tensor", dict(out=tq[0:64, 0:bw], in0=tq[0:64, 0:bw], in1=tfq[0:64, 0:bw], op=ALU.subtract)), reads=[tq, tfq], writes=[tq])
            if layer == 0:
                mk.op("scalar", ("activation", dict(out=h1[:, 0:bw], in_=tq[0:64, 0:bw], func=ACT.Sin, scale=TWO_PI)), reads=[tq], writes=[h1])
            else:
                mk.op("scalar", ("activation", dict(out=h2t[:, blk * bw:(blk + 1) * bw], in_=tq[0:64, 0:bw], func=ACT.Sin, scale=TWO_PI)),
                      reads=[tq], writes=[h2t])
    hr, hi = uh[0], uh[1]
    taps = mk_view_taps = tmp
    hrv = lambda: hr[:, :, :].rearrange("p a b -> p (a b)")[0:M, 0:4096]
    hiv = lambda: hi[:, :, :].rearrange("p a b -> p (a b)")[0:M, 0:4096]
    taps3 = tmp[:, 0:M * 32].rearrange("p (m c) -> p m c", c=32)
    tapv = taps3
    tapsb = zb
    dec_d = d["decay" + tag]
    TW, TWT = C["tw" + tag], C["twt" + tag]
    WCm, ICm = C["wc" + tag], C["ic" + tag]
    h2v = h2t[:, 0:NCIRC].rearrange("p (n m) -> p n m", m=M)
    zsrc = uc[2]
    for o in range(2):
        G = min(8, M)
        for g0 in range(0, M, G):
            p = _nps(B)
            for j in range(G):
                mk.mm(p[:, j * 64:(j + 1) * 64], h2v[:, :, g0 + j], C["w3"][:, o, :], True, True, reads=[h2t, C["w3"]], writes=[p])
            pv = p[:, 0:G * 64].rearrange("p (j r c) -> p j r c", j=G, r=2)
            dec = B["dec"][B["deci"] % 2]
            B["deci"] += 1
            mk.dma("sync", dec[:, 0:G, :], dec_d[:, g0:g0 + G, :], reads=[dec_d], writes=[dec])
            mk.op("vector", ("tensor_tensor", dict(out=taps3[0:64, g0:g0 + G, :], in0=pv[0:64, :, 0, :], in1=dec[0:64, 0:G, :], op=ALU.mult)),
                  reads=[p, dec], writes=[taps])
            mk.op("vector", ("tensor_tensor", dict(out=taps3[64:128, g0:g0 + G, :], in0=pv[64:128, :, 1, :], in1=dec[64:128, 0:G, :], op=ALU.mult)),
                  reads=[p, dec], writes=[taps])
        asum = B["asum"]
        mk.op("vector", ("tensor_reduce", dict(out=asum[:], in_=tapv.rearrange("p m c -> p c m"), axis=AX.X, op=ALU.add, apply_absolute_value=True)),
              reads=[taps], writes=[asum])
        p = _nps(B)
        mk.mm(p[:, 0:32], C["ones"][:, :], asum[:, :], True, True, reads=[C["ones"], asum], writes=[p])
        rn = B["rn"]
        mk.op("vector", ("reciprocal", dict(out=rn[:], in_=p[:, 0:32])), reads=[p], writes=[rn])
        mk.op("vector", ("tensor_copy", dict(out=tapsb[:, 0:M, :], in_=tapv)), reads=[taps], writes=[tapsb])
        emit_fft_fwd(mk, B, M, tapsb, 32, C["rf"], TW, WCm, 128)
        for blk in range(8):
            cs = slice(blk * 512, (blk + 1) * 512)
            pr_, pi_ = _nps(B), _nps(B)
            mk.mm(pr_[0:M, :], WCm[0:M, 0, :], cr[0:M, cs], True, False, reads=[WCm, cr], writes=[pr_])
            mk.mm(pr_[0:M, :], WCm[0:M, 2, :], ci[0:M, cs], False, True, reads=[WCm, ci], writes=[pr_])
            mk.mm(pi_[0:M, :], WCm[0:M, 1, :], cr[0:M, cs], True, False, reads=[WCm, cr], writes=[pi_])
            mk.mm(pi_[0:M, :], WCm[0:M, 0, :], ci[0:M, cs], False, True, reads=[WCm, ci], writes=[pi_])
            rnb = rn[0:M, blk * 4:(blk + 1) * 4].unsqueeze(2).broadcast_to([M, 4, 128])
            mk.op("vector", ("tensor_tensor", dict(out=hrv()[:, cs].rearrange("p (j k) -> p j k", j=4),
                                                                              in0=pr_[0:M, :].rearrange("p (j k) -> p j k", j=4), in1=rnb, op=ALU.mult)),
                  reads=[pr_, rn], writes=[hr])
            mk.op("vector", ("tensor_tensor", dict(out=hiv()[:, cs].rearrange("p (j k) -> p j k", j=4),
                                                                              in0=pi_[0:M, :].rearrange("p (j k) -> p j k", j=4), in1=rnb, op=ALU.mult)),
                  reads=[pi_, rn], writes=[hi])
        zsv = zsrc[:, 0:M, :]
        mk.op("vector", ("tensor_copy", dict(out=zb[:, 0:M, :], in_=zsv)), reads=[zsrc], writes=[zb])
        emit_fft_fwd(mk, B, M, zb, 32, C["ra"], TW, WCm, 128)
        for blk in range(8):
            cs = slice(blk * 512, (blk + 1) * 512)
            pr_, pi_ = _nps(B), _nps(B)
            mk.mm(pr_[0:M, :], WCm[0:M, 0, :], cr[0:M, cs], True, False, reads=[WCm, cr], writes=[pr_])
            mk.mm(pr_[0:M, :], WCm[0:M, 2, :], ci[0:M, cs], False, True, reads=[WCm, ci], writes=[pr_])
            mk.mm(pi_[0:M, :], WCm[0:M, 1, :], cr[0:M, cs], True, False, reads=[WCm, cr], writes=[pi_])
            mk.mm(pi_[0:M, :], WCm[0:M, 0, :], ci[0:M, cs], False, True, reads=[WCm, ci], writes=[pi_])
            ta, tb, tc, td = B["ta"], B["tb"], B["tc"], B["td"]
            mk.op("vector", ("tensor_tensor", dict(out=ta[0:M, :], in0=pr_[0:M, :], in1=hrv()[:, cs], op=ALU.mult)), reads=[pr_, hr], writes=[ta])
            mk.op("vector", ("tensor_tensor", dict(out=tb[0:M, :], in0=pi_[0:M, :], in1=hiv()[:, cs], op=ALU.mult)), reads=[pi_, hi], writes=[tb])
            mk.op("gpsimd", ("tensor_tensor", dict(out=cr[0:M, cs], in0=ta[0:M, :], in1=tb[0:M, :], op=ALU.subtract)), reads=[ta, tb], writes=[cr])
            mk.op("vector", ("tensor_tensor", dict(out=tc[0:M, :], in0=pr_[0:M, :], in1=hiv()[:, cs], op=ALU.mult)), reads=[pr_, hi], writes=[tc])
            mk.op("vector", ("tensor_tensor", dict(out=td[0:M, :], in0=pi_[0:M, :], in1=hrv()[:, cs], op=ALU.mult)), reads=[pi_, hr], writes=[td])
            mk.op("gpsimd", ("tensor_tensor", dict(out=ci[0:M, cs], in0=tc[0:M, :], in1=td[0:M, :], op=ALU.add)), reads=[tc, td], writes=[ci])
        per = min(32, 512 // (2 * M))
        for c0 in range(0, 32, per):
            pb_ = _nps(B)
            for j in range(per):
                ch = c0 + j
                osl = pb_[:, j * 2 * M:(j + 1) * 2 * M]
                mk.mm(osl, cr[0:M, ch * 128:(ch + 1) * 128], ICm[0:M, 0, :], True, False, reads=[cr, ICm], writes=[pb_])
                mk.mm(osl, ci[0:M, ch * 128:(ch + 1) * 128], ICm[0:M, 1, :], False, True, reads=[ci, ICm], writes=[pb_])
            pv = pb_[:, 0:per * 2 * M].rearrange("p (j r k) -> p j r k", j=per, r=2)
            sv = lambda t: t[:, 0:per * M].rearrange("p (j k) -> p j k", j=per)
            trb = TWT[:, 0, :].unsqueeze(1).broadcast_to([128, per, M])
            tib = TWT[:, 1, :].unsqueeze(1).broadcast_to([128, per, M])
            brv = bb[:, 0, c0 * M:(c0 + per) * M].rearrange("p (j k) -> p j k", j=per)
            biv = bb[:, 1, c0 * M:(c0 + per) * M].rearrange("p (j k) -> p j k", j=per)
            _cmul_psum(mk, B, pv[:, :, 0, :], pv[:, :, 1, :], pb_, trb, tib, [TWT], brv, biv, bb, bb, sv, conj=True)
        tot = 32 * M
        bwid = min(512, tot)
        cpb = bwid // M
        for blk in range(tot // bwid):
            po = _nps(B)
            cs = slice(blk * bwid, (blk + 1) * bwid)
            mk.mm(po[:, 0:bwid], C["l1"][:, :], bb[:, 0, cs], True, False, reads=[C["l1"], bb], writes=[po])
            mk.mm(po[:, 0:bwid], C["l2"][:, :], bb[:, 1, cs], False, True, reads=[C["l2"], bb], writes=[po])
            ov = tmp[:, 0:M * 32].rearrange("p (m c) -> p m c", c=32)[:, :, blk * cpb:(blk + 1) * cpb].rearrange("p m c -> p c m")
            mk.op("scalar", ("activation", dict(out=ov, in_=po[:, 0:bwid].rearrange("p (c m) -> p c m", m=M), func=ACT.Copy, scale=1.0 / NCIRC)),
                  reads=[po], writes=[tmp])
        zn = zsv
        hbv = C["hb"][:, o, :].unsqueeze(1).broadcast_to([128, M, 32])
        mk.op("vector", ("tensor_tensor", dict(out=zn, in0=zn, in1=hbv, op=ALU.mult)), reads=[zsrc, C["hb"]], writes=[zsrc])
        mk.op("vector", ("tensor_tensor", dict(out=zn, in0=zn, in1=tv, op=ALU.add)), reads=[zsrc, tmp], writes=[zsrc])
        mk.op("vector", ("tensor_tensor", dict(out=zn, in0=zn, in1=uc[o][:, 0:M, :], op=ALU.mult)), reads=[zsrc, uc[o]], writes=[zsrc])
    hyo = d["hyo" + tag]
    for b in range(2):
        mk.dma("sync", hyo.t[b].rearrange("(n m) c -> n m c", m=M), uc[2][b * 64:(b + 1) * 64, 0:M, :], reads=[uc[2]], writes=[hyo])


def emit_fnet(mk, B, M, tag, d, C):
    L = 64 * M
    cr, ci, bb, tmp = B["cr"], B["ci"], B["bb"], B["tmp"]
    fnv = d["fnv" + tag]
    V = bb
    Vv = bb[:, :, :].rearrange("p a b -> p (a b)")[:, 0:M * 64].rearrange("p (m c) -> p m c", c=64)
    mk.dma("gpsimd", Vv[0:64], fnv.t[:, 0:64].rearrange("(n m) c -> n m c", m=M), reads=[fnv], writes=[bb])
    mk.dma("gpsimd", Vv[64:128], fnv.t[:, 64:128].rearrange("(n m) c -> n m c", m=M), reads=[fnv], writes=[bb])
    TW, WCm = C["twf" + tag], C["wc" + tag]

    per = 4
    for c0 in range(0, 64, per):
        pa = _nps(B)
        for j in range(per):
            mk.mm(pa[0:M, j * 128:(j + 1) * 128], Vv[:, :, c0 + j], C["ra64"][:, :], True, True, reads=[bb, C["ra64"]], writes=[pa])
        pv = pa[0:M, :].rearrange("p (j r k) -> p j r k", j=per, r=2)
        sv = lambda t: t[0:M, 0:per * 64].rearrange("p (j k) -> p j k", j=per)
        trb = TW[0:M, 0, :].unsqueeze(1).broadcast_to([M, per, 64])
        tib = TW[0:M, 1, :].unsqueeze(1).broadcast_to([M, per, 64])
        crv = cr[0:M, c0 * 64:(c0 + per) * 64].rearrange("p (j k) -> p j k", j=per)
        civ = ci[0:M, c0 * 64:(c0 + per) * 64].rearrange("p (j k) -> p j k", j=per)
        _cmul_psum(mk, B, pv[:, :, 0, :], pv[:, :, 1, :], pa, trb, tib, [TW], crv, civ, cr, ci, sv)
    scale = 1.0 / math.sqrt(L * 64.0)
    fo = tmp[:, 0:4096].rearrange("p (k c) -> p k c", c=64)
    for blk in range(8):
        cs = slice(blk * 512, (blk + 1) * 512)
        p = _nps(B)
        mk.mm(p[0:M, :], WCm[0:M, 0, :], cr[0:M, cs], True, False, reads=[WCm, cr], writes=[p])
        mk.mm(p[0:M, :], WCm[0:M, 2, :], ci[0:M, cs], False, True, reads=[WCm, ci], writes=[p])
        ov = fo[0:M, :, blk * 8:(blk + 1) * 8].rearrange("p k c -> p c k")
        mk.op("scalar", ("activation", dict(out=ov, in_=p[0:M, :].rearrange("p (c k) -> p c k", k=64), func=ACT.Copy, scale=scale)),
              reads=[p], writes=[tmp])
    fno = d["fno" + tag]
    mk.dma("sync", fno.t.rearrange("(a k) c -> a (k c)", k=64), tmp[0:M, 0:4096], reads=[tmp], writes=[fno])


def build_p2(seqs=(("m", 128), ("c", 4))):
    nc = bass.Bass("TRN2", target_bir_lowering=False)
    mk = MK(nc)
    d = {}
    for tag, M in seqs:
        L = 64 * M
        d["hyu" + tag] = mk.dram_in("hyu" + tag, [2, L + 2, 96])
        d["fnv" + tag] = mk.dram_in("fnv" + tag, [L, 128])
        d["zfeat" + tag] = mk.dram_in("zfeat" + tag, [33, 128 * M])
        d["hyo" + tag] = mk.dram_out("hyo" + tag, [2, L, 32])
        d["fno" + tag] = mk.dram_out("fno" + tag, [L, 64])
    C = {}

    def cload(name, shape, dtype, q=None):
        dd = mk.dram_in(name, shape)
        t = mk.sb(shape, dtype)
        mk.dma(q or ("gpsimd" if dtype == BF16 else "sync"), t[:], dd[:], reads=[dd], writes=[t])
        C[name] = t
        return t
    cload("ra", [128, 256], BF16)
    cload("rf", [128, 256], BF16)
    cload("ra64", [128, 128], BF16)
    cload("l1", [128, 128], BF16)
    cload("l2", [128, 128], BF16)
    cload("ones", [128, 128], F32)
    cload("w1", [33, 64], F32)
    cload("w2", [64, 64], F32)
    cload("fa", [64, 2], F32)
    cload("fc", [64, 2], F32)
    cload("w3", [64, 2, 64], BF16)
    cload("hb", [128, 2, 32], F32)
    cload("cw", [128, 3, 96], F32)
    cload("cb", [128, 96], F32)
    for tag, M in seqs:
        d["decay" + tag] = mk.dram_in("decay" + tag, [128, M, 32])
        cload("tw" + tag, [M, 2, 128], F32)
        cload("twt" + tag, [128, 2, M], F32)
        cload("twf" + tag, [M, 2, 64], F32)
        cload("wc" + tag, [M, 3, M], BF16)
        cload("ic" + tag, [M, 2, 2 * M], BF16)
    mk.op("vector", ("tensor_tensor", dict(out=C["fc"][:], in0=C["fc"][:], in1=C["fa"][:], op=ALU.mult)), reads=[C["fa"], C["fc"]], writes=[C["fc"]])
    mk.op("vector", ("tensor_scalar_mul", dict(out=C["fc"][:], in0=C["fc"][:], scalar1=1.0 / TWO_PI)), reads=[C["fc"]], writes=[C["fc"]])
    mk.op("vector", ("tensor_scalar_mul", dict(out=C["fa"][:], in0=C["fa"][:], scalar1=1.0 / TWO_PI)), reads=[C["fa"]], writes=[C["fa"]])
    B = _p2_bufs(mk)
    for tag, M in seqs:
        emit_hyena(mk, B, M, tag, d, C)
        emit_fnet(mk, B, M, tag, d, C)
    mk.finish()
    mk.emit()
    return nc

import os as _os
MS_ENG = _os.environ.get('MS_ENG', 'vector')
NKT = 20


def build_p3a(NT=NT, NLAT=NLAT, stop=99):
    ROWS = NT * 128
    NKT_ = NLAT + 4
    nc = bass.Bass("TRN2", target_bir_lowering=False)
    mk = MK(nc)
    xs = mk.dram_in("xs", [ROWS, D])
    condT = mk.dram_in("condT", [128, 16])
    w_ada = mk.dram_in("w_ada", [D, 6 * D])
    b_ada = mk.dram_in("b_ada", [6 * D])
    n1g = mk.dram_in("n1g", [D])
    w_in = mk.dram_in("w_in", [D, 4864])
    q_d = mk.dram_in("q", [ROWS, 512])
    kvx = mk.dram_in("kvx", [NKT_ * 128, 256])
    hy_d = mk.dram_in("hy", [ROWS, 256])
    fn_d = mk.dram_in("fn", [ROWS, 256])
    w_pa = mk.dram_in("w_pa", [512, D])
    w_ph = mk.dram_in("w_ph", [256, D])
    w_pf = mk.dram_in("w_pf", [256, D])
    w_o = mk.dram_in("w_o", [D, D])
    sink = mk.dram_in("sink", [8])
    maskb = mk.dram_in("maskb", [4, 128, 512])
    ident = mk.dram_in("ident", [128, 128])
    x1_o = mk.dram_out("x1", [ROWS, D])

    psS = [mk.ps([128, 512], F32) for _ in range(3)]
    psO = mk.ps([128, 4, 65], F32)
    PT = mk.ps([128, 8, 128], BF16)
    psG = [mk.ps([128, 512], F32) for _ in range(3)]

    identb = mk.sb([128, 128], BF16)
    mk.dma("gpsimd", identb[:], ident[:], reads=[ident], writes=[identb])
    mb = mk.sb([128, 4, 512], BF16)
    mk.dma("gpsimd", mb[:], maskb.t.rearrange("i p n -> p i n"), reads=[maskb], writes=[mb])
    esink = mk.sb([128, 8], F32)
    mk.dma("sync", esink[:], sink[:].partition_broadcast(128), reads=[sink], writes=[esink])
    mk.op("scalar", ("activation", dict(out=esink[:], in_=esink[:], func=ACT.Exp)), reads=[esink], writes=[esink])
    epsb = mk.sb([128, 1], F32)
    mk.op("vector", ("memset", dict(ap=epsb[:], constant=EPS)), writes=[epsb])
    ss = mk.sb([128, NT], F32)
    rs = mk.sb([128, NT], F32)
    mk.op("vector", ("memset", dict(ap=ss[:], constant=0.0)), writes=[ss])

    WG = mk.sb([128, 8, 3072], BF16)
    mods = emit_adaln(mk, condT, w_ada, b_ada, 0, 3072, psG[0], wa_views=[(WG, WG[:, :, 0:512]), (WG, WG[:, :, 512:1024])])
    emit_modulate_setup(mk, mods, n1g, 0, 1024)

    if stop == 1:
        mk.finish(); mk.emit(); return nc
    wv = w_in.t.rearrange("(c p) n -> p c n", p=128)
    for c in range(8):
        mk.dma("gpsimd", WG[:, c, :], wv[:, c, 1792:4864], reads=[w_in], writes=[WG])
    WP = mk.sb([128, 8, D], BF16)
    mk.dma("gpsimd", WP[:, 0:4, :], w_pa.t.rearrange("(c p) n -> p c n", p=128), reads=[w_pa], writes=[WP])
    mk.dma("gpsimd", WP[:, 4:6, :], w_ph.t.rearrange("(c p) n -> p c n", p=128), reads=[w_ph], writes=[WP])
    mk.dma("gpsimd", WP[:, 6:8, :], w_pf.t.rearrange("(c p) n -> p c n", p=128), reads=[w_pf], writes=[WP])
    WO = mk.sb([128, 8, D], BF16)
    wov = w_o.t.rearrange("(c p) n -> p c n", p=128)
    for c in range(0, 8, 2):
        mk.dma("gpsimd", WO[:, c:c + 2, :], wov[:, c:c + 2, :], reads=[w_o], writes=[WO])

    if stop == 2:
        mk.finish(); mk.emit(); return nc
    KZ = [[mk.sb([128, NKT_, 128], BF16) for _ in range(2)] for _ in range(2)]
    for kv in range(2):
        for hf_ in range(2):
            mk.op(MS_ENG, ("memset", dict(ap=KZ[kv][hf_][:], constant=0.0)), writes=[KZ[kv][hf_]])
    VA = mk.sb([128, NKT_, 2, 65], BF16)
    mk.op(MS_ENG, ("memset", dict(ap=VA[:], constant=1.0)), writes=[VA])
    kvt = [mk.sb([128, 256], F32), mk.sb([128, 256], F32)]
    kb = mk.sb([128, 2, 128], BF16)
    PTk = PT
    for kt in range(NKT_):
        kf = kvt[kt % 2]
        mk.dma("sync", kf[:], kvx[kt * 128:(kt + 1) * 128, :], reads=[kvx], writes=[kf])
        mk.op("vector", ("tensor_copy", dict(out=kb[:, 0, :], in_=kf[:, 0:128])), reads=[kf], writes=[kb])
        mk.op("vector", ("tensor_copy", dict(out=kb[:, 1, 0:64], in_=kf[:, 64:128])), reads=[kf], writes=[kb])
        mk.op("vector", ("tensor_copy", dict(out=kb[:, 1, 64:128], in_=kf[:, 0:64])), reads=[kf], writes=[kb])
        mk.op("vector", ("tensor_copy", dict(out=VA[:, kt, :, 0:64], in_=kf[:, 128:256].rearrange("p (h d) -> p h d", h=2))), reads=[kf], writes=[VA])
        if stop == 31:
            mk.finish(); mk.emit(); return nc
        mk.tr(PTk[:, 0, :], kb[:, 0, :], identb[:], reads=[kb, identb], writes=[PTk])
        mk.tr(PTk[:, 1, :], kb[:, 1, :], identb[:], reads=[kb, identb], writes=[PTk])
        if stop == 32:
            mk.finish(); mk.emit(); return nc
        mk.op("scalar", ("copy", dict(out=KZ[0][0][0:64, kt, :], in_=PTk[0:64, 0, :])), reads=[PTk], writes=[KZ[0][0]])
        mk.op("scalar", ("copy", dict(out=KZ[1][1][64:128, kt, :], in_=PTk[64:128, 0, :])), reads=[PTk], writes=[KZ[1][1]])
        if stop == 33:
            mk.finish(); mk.emit(); return nc
        mk.op("scalar", ("copy", dict(out=KZ[1][0][0:64, kt, :], in_=PTk[0:64, 1, :])), reads=[PTk], writes=[KZ[1][0]])
        mk.op("scalar", ("copy", dict(out=KZ[0][1][64:128, kt, :], in_=PTk[64:128, 1, :])), reads=[PTk], writes=[KZ[0][1]])
        if stop == 34 + kt:
            mk.finish(); mk.emit(); return nc

    if stop == 3:
        mk.finish(); mk.emit(); return nc
    xts = [mk.sb([128, D], F32), mk.sb([128, D], F32)]
    hf = mk.sb([128, D], F32)
    hb = mk.sb([128, D], BF16)
    hT = mk.sb([128, 8, 128], BF16)
    qf1 = mk.sb([128, 512], F32)
    qf = [qf1, qf1]
    qb = mk.sb([128, 512], BF16)
    QT = mk.sb([128, 4, 128], BF16)
    Es = [mk.sb([128, 512], BF16) for _ in range(5)]
    den = mk.sb([128, 4], F32)
    attn = mk.sb([128, 512], BF16)
    hyf1 = mk.sb([128, 512], F32)
    hyf = [hyf1, hyf1]
    hfb = mk.sb([128, 512], BF16)
    BT = mk.sb([128, 8, 128], BF16)
    Gt = mk.sb([128, 3072], BF16)
    t1 = mk.sb([128, 512], F32)
    t2 = mk.sb([128, 512], F32)
    mbf = mk.sb([128, D], BF16)
    mT = mk.sb([128, 8, 128], BF16)
    xo1 = mk.sb([128, D], F32)
    xo = [xo1, xo1]
    si = 0
    for t in range(NT):
        r = 0 if t < NLAT else 1
        rows = slice(t * 128, (t + 1) * 128)
        if t < NLAT:
            keys = [(t, 0 if t == 0 else 1), (t + 1, None), (t + 2, 3 if t == NLAT - 1 else 2), (NLAT + 2, None), (NLAT + 3, None)]
        else:
            keys = [(NLAT + 2, None), (NLAT + 3, None)]
        xt = xts[t % 2]
        mk.dma("sync", xt[:], xs[rows, :], reads=[xs], writes=[xt])
        emit_norm_mod_T(mk, xt, mods[r], 0, 1024, ss, rs, t, epsb, hf, hb, PT, hT, identb)
        for nb in range(6):
            pg = psG[nb % 3]
            for c in range(8):
                mk.mm(pg[:], hT[:, c, :], WG[:, c, nb * 512:(nb + 1) * 512], c == 0, c == 7, reads=[hT, WG], writes=[pg])
            mk.op("scalar", ("activation", dict(out=Gt[:, nb * 512:(nb + 1) * 512], in_=pg[:], func=ACT.Sigmoid)), reads=[pg], writes=[Gt])
        if stop == 6:
            mk.finish(); mk.emit(); return nc
        q_ = qf[t % 2]
        mk.dma("sync", q_[:], q_d[rows, :], reads=[q_d], writes=[q_])
        mk.op("vector", ("tensor_copy", dict(out=qb[:], in_=q_[:])), reads=[q_], writes=[qb])
        for p_ in range(4):
            mk.tr(PT[:, p_, :], qb[:, p_ * 128:(p_ + 1) * 128], identb[:], reads=[qb, identb], writes=[PT])
        mk.op("scalar", ("copy", dict(out=QT[:], in_=PT[:, 0:4, :])), reads=[PT], writes=[QT])
        for kv in range(2):
            for ki, (kt, mi) in enumerate(keys):
                S = psS[si % 3]
                si += 1
                if mi is not None:
                    mk.mm(S[:], identb[:], mb[:, mi, :], True, False, reads=[identb, mb], writes=[S])
                for hh in range(4):
                    h = 4 * kv + hh
                    mk.mm(S[:, hh * 128:(hh + 1) * 128], KZ[kv][h % 2][:, kt, :], QT[:, h // 2, :],
                          (mi is None and hh == 0), hh == 3, reads=[KZ[kv][h % 2], QT], writes=[S])
                mk.op("scalar", ("activation", dict(out=Es[ki][:], in_=S[:], func=ACT.Exp, scale=0.125)), reads=[S], writes=[Es[ki]])
            for hh in range(4):
                for ki, (kt, mi) in enumerate(keys):
                    mk.mm(psO[:, hh, :], Es[ki][:, hh * 128:(hh + 1) * 128], VA[:, kt, kv, :], ki == 0, ki == len(keys) - 1,
                          reads=[Es[ki], VA], writes=[psO])
            mk.op("vector", ("tensor_tensor", dict(out=den[:], in0=psO[:, :, 64], in1=esink[:, 4 * kv:4 * kv + 4], op=ALU.add)), reads=[psO, esink], writes=[den])
            mk.op("vector", ("reciprocal", dict(out=den[:], in_=den[:])), reads=[den], writes=[den])
            mk.op("vector", ("tensor_tensor", dict(out=attn[:, kv * 256:(kv + 1) * 256].rearrange("p (h d) -> p h d", h=4), in0=psO[:, :, 0:64],
                                                    in1=den[:, :].unsqueeze(2).broadcast_to([128, 4, 64]), op=ALU.mult)), reads=[psO, den], writes=[attn])
        if stop == 4:
            mk.finish(); mk.emit(); return nc
        hy_ = hyf[t % 2]
        mk.dma("sync", hy_[:, 0:256], hy_d[rows, :], reads=[hy_d], writes=[hy_])
        mk.dma("sync", hy_[:, 256:512], fn_d[rows, :], reads=[fn_d], writes=[hy_])
        mk.op("vector", ("tensor_copy", dict(out=hfb[:], in_=hy_[:])), reads=[hy_], writes=[hfb])
        for c in range(4):
            mk.tr(PT[:, c, :], attn[:, c * 128:(c + 1) * 128], identb[:], reads=[attn, identb], writes=[PT])
        for c in range(4):
            mk.tr(PT[:, 4 + c, :], hfb[:, c * 128:(c + 1) * 128], identb[:], reads=[hfb, identb], writes=[PT])
        mk.op("scalar", ("copy", dict(out=BT[:], in_=PT[:])), reads=[PT], writes=[BT])
        if stop == 5:
            mk.finish(); mk.emit(); return nc
        for half in range(2):
            cs = slice(half * 512, (half + 1) * 512)
            for bi, (c0, c1) in enumerate(((0, 4), (4, 6), (6, 8))):
                for c in range(c0, c1):
                    mk.mm(psG[bi][:], BT[:, c, :], WP[:, c, cs], c == c0, c == c1 - 1, reads=[BT, WP], writes=[psG[bi]])
            mk.op("vector", ("tensor_tensor", dict(out=t1[:], in0=psG[0][:], in1=Gt[:, half * 512:(half + 1) * 512], op=ALU.mult)), reads=[psG[0], Gt], writes=[t1])
            mk.op("vector", ("tensor_tensor", dict(out=t2[:], in0=psG[1][:], in1=Gt[:, 1024 + half * 512:1024 + (half + 1) * 512], op=ALU.mult)), reads=[psG[1], Gt], writes=[t2])
            mk.op("gpsimd", ("tensor_tensor", dict(out=t1[:], in0=t1[:], in1=t2[:], op=ALU.add)), reads=[t1, t2], writes=[t1])
            mk.op("vector", ("tensor_tensor", dict(out=t2[:], in0=psG[2][:], in1=Gt[:, 2048 + half * 512:2048 + (half + 1) * 512], op=ALU.mult)), reads=[psG[2], Gt], writes=[t2])
            mk.op("gpsimd", ("tensor_tensor", dict(out=mbf[:, cs], in0=t1[:], in1=t2[:], op=ALU.add)), reads=[t1, t2], writes=[mbf])
        for c in range(8):
            mk.tr(PT[:, c, :], mbf[:, c * 128:(c + 1) * 128], identb[:], reads=[mbf, identb], writes=[PT])
        mk.op("scalar", ("copy", dict(out=mT[:], in_=PT[:])), reads=[PT], writes=[mT])
        xo_ = xo[t % 2]
        for half in range(2):
            cs = slice(half * 512, (half + 1) * 512)
            pg = psG[half]
            for c in range(8):
                mk.mm(pg[:], mT[:, c, :], WO[:, c, cs], c == 0, c == 7, reads=[mT, WO], writes=[pg])
            mk.op("vector", ("tensor_tensor", dict(out=t1[:], in0=pg[:], in1=mods[r][:, 2048 + half * 512:2048 + (half + 1) * 512], op=ALU.mult)), reads=[pg, mods[r]], writes=[t1])
            mk.op("gpsimd", ("tensor_tensor", dict(out=xo_[:, cs], in0=t1[:], in1=xt[:, cs], op=ALU.add)), reads=[t1, xt], writes=[xo_])
        mk.dma("gpsimd", x1_o[rows, :], xo_[:], reads=[xo_], writes=[x1_o])
    mk.finish()
    mk.emit()
    return nc


def build_p3b(NE, NT=NT, NLAT=NLAT):
    ROWS = NT * 128
    FF = 2816
    NFC = FF // 128
    GF = 4
    nc = bass.Bass("TRN2", target_bir_lowering=False)
    mk = MK(nc)
    xs = mk.dram_in("xs", [ROWS, D])
    condT = mk.dram_in("condT", [128, 16])
    w_ada = mk.dram_in("w_ada", [D, 6 * D])
    b_ada = mk.dram_in("b_ada", [6 * D])
    n2g = mk.dram_in("n2g", [D])
    wg_d = mk.dram_in("wg", [NE, D, FF])
    wu_d = mk.dram_in("wu", [NE, D, FF])
    wd_d = mk.dram_in("wd", [NE, FF, D])
    ident = mk.dram_in("ident", [128, 128])
    if NE > 1:
        wr_d = mk.dram_in("wr", [D, 8])
    x2_o = mk.dram_out("x2", [ROWS, D])

    psG = [mk.ps([128, 512], F32) for _ in range(2)]
    psU = [mk.ps([128, 512], F32) for _ in range(2)]
    psY = [mk.ps([128, 512], F32) for _ in range(3)]
    PT = mk.ps([128, 8, 128], BF16)
    psR = psY[2]

    identb = mk.sb([128, 128], BF16)
    mk.dma("gpsimd", identb[:], ident[:], reads=[ident], writes=[identb])
    epsb = mk.sb([128, 1], F32)
    mk.op("vector", ("memset", dict(ap=epsb[:], constant=EPS)), writes=[epsb])
    ss = mk.sb([128, NT], F32)
    rs = mk.sb([128, NT], F32)
    mk.op("vector", ("memset", dict(ap=ss[:], constant=0.0)), writes=[ss])
    h2T = mk.sb([128, 8, ROWS], BF16)
    if ROWS >= 1024:
        wav = [(h2T, h2T[:, :, 0:512]), (h2T, h2T[:, :, 512:1024])]
    else:
        wav = None
    yacc = mk.sb([128, NT, D], F32)
    mk.op("gpsimd", ("memset", dict(ap=yacc[:], constant=0.0)), writes=[yacc])
    comb = mk.sb([128, NT, 8], F32)
    gmods = emit_adaln(mk, condT, w_ada, b_ada, 5120, 6144, psR, wa_views=wav)
    mk.phase_begin()
    mods = emit_adaln(mk, condT, w_ada, b_ada, 3072, 5120, psR, wa_views=wav)
    emit_modulate_setup(mk, mods, n2g, 0, 1024)
    if NE > 1:
        wrb = mk.sb([128, 8, 8], BF16)
        mk.dma("gpsimd", wrb[:], wr_d.t.rearrange("(c p) n -> p c n", p=128), reads=[wr_d], writes=[wrb])
    xts = [mk.sb([128, D], F32), mk.sb([128, D], F32)]
    hf = mk.sb([128, D], F32)
    hb = mk.sb([128, D], BF16)
    hT = mk.sb([128, 8, 128], BF16)
    sm = mk.sb([128, 64], F32)
    for t in range(NT):
        r = 0 if t < NLAT else 1
        xt = xts[t % 2]
        mk.dma("sync", xt[:], xs[t * 128:(t + 1) * 128, :], reads=[xs], writes=[xt])
        emit_norm_mod_T(mk, xt, mods[r], 0, 1024, ss, rs, t, epsb, hf, hb, PT, h2T, identb, hT_ap=h2T[:, :, t * 128:(t + 1) * 128])
        if NE > 1:
            for c in range(8):
                mk.mm(psR[:, 0:8], h2T[:, c, t * 128:(t + 1) * 128], wrb[:, c, :], c == 0, c == 7, reads=[h2T, wrb], writes=[psR])
            lg, m1, eq1, lg2, m2, eq2, dl, w1, w2 = (sm[:, 0:8], sm[:, 8:9], sm[:, 16:24], sm[:, 24:32], sm[:, 9:10], sm[:, 32:40],
                                                    sm[:, 10:11], sm[:, 11:12], sm[:, 12:13])
            R, W = [sm], [sm]
            mk.op("vector", ("tensor_copy", dict(out=lg, in_=psR[:, 0:8])), reads=[psR], writes=W)
            mk.op("vector", ("tensor_reduce", dict(out=m1, in_=lg, axis=AX.X, op=ALU.max)), reads=R, writes=W)
            mk.op("vector", ("tensor_scalar", dict(out=eq1, in0=lg, scalar1=m1, scalar2=None, op0=ALU.is_equal)), reads=R, writes=W)
            mk.op("vector", ("scalar_tensor_tensor", dict(out=lg2, in0=eq1, scalar=-1e30, in1=lg, op0=ALU.mult, op1=ALU.add)), reads=R, writes=W)
            mk.op("vector", ("tensor_reduce", dict(out=m2, in_=lg2, axis=AX.X, op=ALU.max)), reads=R, writes=W)
            mk.op("vector", ("tensor_scalar", dict(out=eq2, in0=lg2, scalar1=m2, scalar2=None, op0=ALU.is_equal)), reads=R, writes=W)
            mk.op("vector", ("tensor_tensor", dict(out=dl, in0=m1, in1=m2, op=ALU.subtract)), reads=R, writes=W)
            mk.op("scalar", ("activation", dict(out=w1, in_=dl, func=ACT.Sigmoid)), reads=R, writes=W)
            mk.op("scalar", ("activation", dict(out=w2, in_=dl, func=ACT.Sigmoid, scale=-1.0)), reads=R, writes=W)
            mk.op("vector", ("tensor_scalar", dict(out=eq1, in0=eq1, scalar1=w1, scalar2=None, op0=ALU.mult)), reads=R, writes=W)
            mk.op("vector", ("scalar_tensor_tensor", dict(out=comb[:, t, :], in0=eq2, scalar=w2, in1=eq1, op0=ALU.mult, op1=ALU.add)), reads=R, writes=[comb])
    mk.phase_end()
    mk.phase_begin()
    wgs = [mk.sb([128, 8, GF * 128], BF16) for _ in range(2)]
    wus = [mk.sb([128, 8, GF * 128], BF16) for _ in range(2)]
    wds = [mk.sb([128, GF, D], BF16) for _ in range(2)]
    aT = mk.sb([128, GF, ROWS], BF16)
    sg = [mk.sb([128, 512], F32), mk.sb([128, 512], F32)]
    tblocks = [(a, min(512, ROWS - a)) for a in range(0, ROWS, 512)]
    pys = [psY[0], psY[1], psY[2]]
    gi = 0
    pi = 0
    groups = [(e, g0) for e in range(NE) for g0 in range(0, NFC, GF)]

    def load_group(k):
        e, g0 = groups[k]
        gf = min(GF, NFC - g0)
        wgv = wg_d.t[e].rearrange("(c p) n -> p c n", p=128)
        wuv = wu_d.t[e].rearrange("(c p) n -> p c n", p=128)
        wdv = wd_d.t[e].rearrange("(c p) n -> p c n", p=128)
        wg_, wu_, wd_ = wgs[k % 2], wus[k % 2], wds[k % 2]
        mk.dma("gpsimd", wg_[:, :, 0:gf * 128], wgv[:, :, g0 * 128:(g0 + gf) * 128], reads=[wg_d], writes=[wg_])
        mk.dma("gpsimd", wu_[:, :, 0:gf * 128], wuv[:, :, g0 * 128:(g0 + gf) * 128], reads=[wu_d], writes=[wu_])
        mk.dma("gpsimd", wd_[:, 0:gf, :], wdv[:, g0:g0 + gf, :], reads=[wd_d], writes=[wd_])

    load_group(0)
    for k, (e, g0) in enumerate(groups):
        if True:
            gf = min(GF, NFC - g0)
            wg_, wu_, wd_ = wgs[k % 2], wus[k % 2], wds[k % 2]
            if k + 1 < len(groups):
                load_group(k + 1)
            for j in range(gf):
                for (a, w) in tblocks:
                    pg, pu = psG[pi % 2], psU[pi % 2]
                    s_ = sg[pi % 2]
                    pi += 1
                    for c in range(8):
                        mk.mm(pg[:, 0:w], wg_[:, c, j * 128:(j + 1) * 128], h2T[:, c, a:a + w], c == 0, c == 7, reads=[wg_, h2T], writes=[pg])
                    for c in range(8):
                        mk.mm(pu[:, 0:w], wu_[:, c, j * 128:(j + 1) * 128], h2T[:, c, a:a + w], c == 0, c == 7, reads=[wu_, h2T], writes=[pu])
                    mk.op("scalar", ("activation", dict(out=s_[:, 0:w], in_=pg[:, 0:w], func=ACT.Silu)), reads=[pg], writes=[s_])
                    mk.op("vector", ("tensor_tensor", dict(out=aT[:, j, a:a + w], in0=s_[:, 0:w], in1=pu[:, 0:w], op=ALU.mult)), reads=[s_, pu], writes=[aT])
            for t in range(NT):
                for half in range(2):
                    py = pys[(2 * t + half) % len(pys)]
                    cs = slice(half * 512, (half + 1) * 512)
                    for j in range(gf):
                        mk.mm(py[:], aT[:, j, t * 128:(t + 1) * 128], wd_[:, j, cs], j == 0, j == gf - 1, reads=[aT, wd_], writes=[py])
                    sc = comb[:, t, e:e + 1] if NE > 1 else 1.0
                    rd = [py, yacc] + ([comb] if NE > 1 else [])
                    eng = "vector" if half == 0 else "gpsimd"
                    if eng == "gpsimd":
                        s_ = sg[(2 * t + half) % 2]
                        mk.op("scalar", ("activation", dict(out=s_[:], in_=py[:], func=ACT.Copy, scale=sc)), reads=[py] + ([comb] if NE > 1 else []), writes=[s_])
                        mk.op("gpsimd", ("tensor_tensor", dict(out=yacc[:, t, cs], in0=s_[:], in1=yacc[:, t, cs], op=ALU.add)),
                              reads=[s_, yacc], writes=[yacc])
                    else:
                        mk.op("vector", ("scalar_tensor_tensor", dict(out=yacc[:, t, cs], in0=py[:], scalar=sc, in1=yacc[:, t, cs], op0=ALU.mult, op1=ALU.add)),
                              reads=rd, writes=[yacc])
    mk.phase_end()
    mk.phase_begin()
    xts = [mk.sb([128, D], F32), mk.sb([128, D], F32)]
    xo = [mk.sb([128, D], F32), mk.sb([128, D], F32)]
    for t in range(NT):
        r = 0 if t < NLAT else 1
        xt = xts[t % 2]
        mk.dma("sync", xt[:], xs[t * 128:(t + 1) * 128, :], reads=[xs], writes=[xt])
        xo_ = xo[t % 2]
        mk.op("vector", ("tensor_tensor", dict(out=xo_[:], in0=yacc[:, t, :], in1=gmods[r][:, 0:1024], op=ALU.mult)), reads=[yacc, gmods[r]], writes=[xo_])
        mk.op("gpsimd", ("tensor_tensor", dict(out=xo_[:], in0=xo_[:], in1=xt[:], op=ALU.add)), reads=[xo_, xt], writes=[xo_])
        mk.dma("gpsimd", x2_o[t * 128:(t + 1) * 128, :], xo_[:], reads=[xo_], writes=[x2_o])
    mk.phase_end()
    mk.finish()
    mk.emit()
    return nc

def _rope_tables_core(qtr):
    t = np.arange(2048, dtype=np.int64) + qtr * 2048
    pos = np.stack([t // 64, t % 64], axis=-1).astype(np.float32)
    inv = (np.float32(10000.0) ** (-np.arange(16, dtype=np.float32) / np.float32(16))).astype(np.float32)
    ang = (pos[:, :, None] * inv).astype(np.float32)
    c = np.cos(ang).astype(np.float32).reshape(2048, 32)
    s = np.sin(ang).astype(np.float32).reshape(2048, 32)
    c = np.concatenate([c, np.ones((256, 32), np.float32)], 0)
    s = np.concatenate([s, np.zeros((256, 32), np.float32)], 0)
    return np.ascontiguousarray(c), np.ascontiguousarray(s)


def _dft(n):
    k = np.arange(n)
    a = 2.0 * np.pi * np.outer(k, k) / n
    return np.cos(a), np.sin(a)


def _condT(cb, c_ctx):
    cond = np.stack([cb, c_ctx], 0).astype(np.float32)
    return np.ascontiguousarray(cond.reshape(2, 8, 128).transpose(2, 0, 1).reshape(128, 16))


_CACHE = {}


def _get(name, fn):
    if name not in _CACHE:
        _CACHE[name] = fn()
    return _CACHE[name]


def p1_inmaps(inp, l, xcur, ctxcur):
    c64, s64 = _dft(64)
    f64 = np.concatenate([c64, -s64], 1).astype(np.float32)
    bd = np.zeros((128, 256), np.float32)
    bd[:64, :128] = f64
    bd[64:, 128:] = f64
    f64 = bd
    ident = np.eye(128, dtype=np.float32)
    maps = []
    for i in range(8):
        b, qtr = i // 4, i % 4
        rc, rs = _rope_tables_core(qtr)
        maps.append(dict(
            xs=np.ascontiguousarray(np.concatenate([xcur[b, qtr * 2048:(qtr + 1) * 2048], ctxcur[b]], 0)),
            condT=_condT(inp["c"][b], inp["c_ctx"]),
            w_ada=inp["w_ada"][l], b_ada=inp["b_ada"][l], n1g=inp["norm1_g"][l], w_in=inp["w_in"][l],
            qg=inp["q_norm_g"][l], kg=inp["k_norm_g"][l], ropec=rc, ropes=rs, f64=f64, ident=ident))
    return maps


def _zfeat_circ(L):
    f32 = np.float32
    t = np.linspace(0.0, 1.0, L, dtype=f32)[:, None]
    w = (f32(2.0 * math.pi) * np.arange(L, dtype=f32)[:, None] / f32(L)).astype(f32)
    fb = np.linspace(1e-4, 15, 16, dtype=f32)
    z = np.concatenate([t, np.cos(fb * w), -np.sin(fb * w)], axis=-1).astype(f32)
    zc = np.zeros((2 * L, 33), f32)
    zc[:L] = z
    zc[L + 1:] = z[1:][::-1]
    return np.ascontiguousarray(zc.T), t[:, 0]


def _decay_circ(L, M, chs):
    f32 = np.float32
    t = np.linspace(0.0, 1.0, L, dtype=f32)
    deltas = np.abs(np.linspace(math.log(1e-2) / 1.5, math.log(1e-2) / 0.3, 256, dtype=f32)).astype(f32)
    dec = np.exp(-(t[:, None] * deltas[None, chs])).astype(f32)
    dc = np.zeros((2 * L, len(chs)), f32)
    dc[:L] = dec
    dc[L + 1:] = dec[1:][::-1]
    return np.ascontiguousarray(dc.reshape(128, M, len(chs)))


def p2_consts(M):
    N = 128 * M
    L = 64 * M
    wr, ws = _dft(128)
    wi = -ws
    out = {}
    out["ra"] = np.concatenate([np.concatenate([wr[:64], wi[:64]], 1), np.concatenate([-wi[:64], wr[:64]], 1)], 0)
    out["rf"] = np.concatenate([wr, wi], 1)
    c64, s64 = _dft(64)
    w64r, w64i = c64, -s64
    out["ra64"] = np.concatenate([np.concatenate([w64r, w64i], 1), np.concatenate([-w64i, w64r], 1)], 0)
    out["l1"] = np.concatenate([wr[:, :64], -wi[:, :64]], 1)
    out["l2"] = np.concatenate([wi[:, :64], wr[:, :64]], 1)
    n2 = np.arange(M)[:, None]
    k1 = np.arange(128)[None, :]
    a = 2.0 * np.pi * n2 * k1 / N
    tw = np.stack([np.cos(a), -np.sin(a)], 1)
    out["tw"] = tw
    out["twt"] = np.ascontiguousarray(tw.transpose(2, 1, 0))
    a = 2.0 * np.pi * n2 * np.arange(64)[None, :] / L
    out["twf"] = np.stack([np.cos(a), -np.sin(a)], 1)
    mr, ms = _dft(M)
    mi = -ms
    out["wc"] = np.stack([mr, mi, -mi], 1)
    out["ic"] = np.stack([np.concatenate([mr, -mi], 1), np.concatenate([mi, mr], 1)], 1)
    return {k: np.ascontiguousarray(v.astype(np.float32)) for k, v in out.items()}


P2_SEQS = (("m", 128), ("c", 4))


def p2_inmaps(inp, l, hy_full, fnv_full, hy_ctx, fnv_ctx):
    maps = []
    cm = {tag: _get("p2c%d" % M, lambda M=M: p2_consts(M)) for tag, M in P2_SEQS}
    zf = {tag: _get("zf%d" % M, lambda M=M: _zfeat_circ(64 * M)[0]) for tag, M in P2_SEQS}
    for i in range(8):
        chs = np.arange(i * 32, (i + 1) * 32)
        b, g = i // 4, i % 4
        m = {}
        for (tag, M), hy, fv in ((P2_SEQS[0], hy_full, fnv_full), (P2_SEQS[1], hy_ctx, fnv_ctx)):
            L = 64 * M
            cols = np.concatenate([chs, 256 + chs, 512 + chs])
            hp = np.zeros((2, L + 2, 96), np.float32)
            hp[:, 1:L + 1] = hy[:, :, cols]
            m["hyu" + tag] = hp
            m["fnv" + tag] = np.ascontiguousarray(fv[b, :, g * 128:(g + 1) * 128])
            m["zfeat" + tag] = zf[tag]
            m["decay" + tag] = _get("dec%d_%d" % (M, i), lambda M=M, chs=chs: _decay_circ(64 * M, M, chs))
            for k in ("tw", "twt", "twf", "wc", "ic"):
                m[k + tag] = cm[tag][k]
        for k in ("ra", "rf", "ra64", "l1", "l2"):
            m[k] = cm["m"][k]
        m["ones"] = np.ones((128, 128), np.float32)
        m["w1"] = inp["hy_filt_w1"][l]
        m["w2"] = inp["hy_filt_w2"][l]
        m["fa"] = np.ascontiguousarray(np.stack([inp["hy_filt_freq1"][l], inp["hy_filt_freq2"][l]], 1))
        m["fc"] = np.ascontiguousarray(np.stack([inp["hy_filt_b1"][l], inp["hy_filt_b2"][l]], 1))
        w3 = inp["hy_filt_w3"][l].reshape(64, 2, 2, 256)[:, :, :, chs]
        m["w3"] = np.ascontiguousarray(w3.transpose(0, 2, 1, 3).reshape(64, 2, 64))
        m["hb"] = np.ascontiguousarray(np.broadcast_to(inp["hy_bias"][l][:, chs][None], (128, 2, 32)))
        cw = inp["hy_conv_w"][l][:, 0, :]
        cols = np.concatenate([chs, 256 + chs, 512 + chs])
        m["cw"] = np.ascontiguousarray(np.broadcast_to(cw[:, cols][None], (128, 3, 96)))
        m["cb"] = np.ascontiguousarray(np.broadcast_to(inp["hy_conv_b"][l][cols][None], (128, 96)))
        maps.append(m)
    return maps


def _masks(qtr):
    j = np.arange(128)[:, None]
    q = np.arange(128)[None, :]
    NEG = np.float32(-30000.0)
    prev = np.where(j >= q, np.float32(0), NEG).astype(np.float32)
    nxt = np.where(j <= q, np.float32(0), NEG).astype(np.float32)
    allm = np.full((128, 128), NEG, np.float32)
    ms = [allm if qtr == 0 else prev, prev, nxt, allm if qtr == 3 else nxt]
    return np.ascontiguousarray(np.stack([np.tile(m, (1, 4)) for m in ms], 0))


def p3a_inmaps(inp, l, xcur, ctxcur, q_all, kv_all, kv_ctx, hyo, fno, hyo_c, fno_c, q_ctx):
    ident = np.eye(128, dtype=np.float32)
    maps = []
    z = np.zeros((128, 256), np.float32)
    for i in range(8):
        b, qtr = i // 4, i % 4
        sl = slice(qtr * 2048, (qtr + 1) * 2048)
        prev = kv_all[b, qtr * 2048 - 128:qtr * 2048] if qtr > 0 else z
        nxt = kv_all[b, (qtr + 1) * 2048:(qtr + 1) * 2048 + 128] if qtr < 3 else z
        maps.append(dict(
            xs=np.ascontiguousarray(np.concatenate([xcur[b, sl], ctxcur[b]], 0)),
            condT=_condT(inp["c"][b], inp["c_ctx"]),
            w_ada=inp["w_ada"][l], b_ada=inp["b_ada"][l], n1g=inp["norm1_g"][l], w_in=inp["w_in"][l],
            q=np.ascontiguousarray(np.concatenate([q_all[b, sl], q_ctx[b]], 0)),
            kvx=np.ascontiguousarray(np.concatenate([prev, kv_all[b, sl], nxt, kv_ctx[b]], 0)),
            hy=np.ascontiguousarray(np.concatenate([hyo[b, sl], hyo_c[b]], 0)),
            fn=np.ascontiguousarray(np.concatenate([fno[b, sl], fno_c[b]], 0)),
            w_pa=inp["w_proj_attn"][l], w_ph=inp["w_proj_hyena"][l], w_pf=inp["w_proj_fnet"][l], w_o=inp["w_out"][l],
            sink=inp["attn_sink"][l], maskb=_masks(qtr), ident=ident))
    return maps


def p3b_inmaps(inp, l, x1, ctx1):
    ident = np.eye(128, dtype=np.float32)
    i_ = l // 2
    maps = []
    for i in range(8):
        b, qtr = i // 4, i % 4
        sl = slice(qtr * 2048, (qtr + 1) * 2048)
        m = dict(xs=np.ascontiguousarray(np.concatenate([x1[b, sl], ctx1[b]], 0)),
                 condT=_condT(inp["c"][b], inp["c_ctx"]), w_ada=inp["w_ada"][l], b_ada=inp["b_ada"][l],
                 n2g=inp["norm2_g"][l], ident=ident)
        if l % 2 == 0:
            m.update(wg=inp["ffn_w_gate"][i_][None], wu=inp["ffn_w_up"][i_][None], wd=inp["ffn_w_down"][i_][None])
        else:
            m.update(wg=inp["moe_w_gate"][i_], wu=inp["moe_w_up"][i_], wd=inp["moe_w_down"][i_], wr=inp["moe_router"][i_])
        maps.append(m)
    return maps


_NC = {}


def _prog(name, fn):
    if name not in _NC:
        _NC[name] = fn()
    return _NC[name]


def _run(nc, maps):
    res = run_bass_kernel_spmd(nc, maps, core_ids=list(range(8)))
    return res.results


def _gather_tok(results, key, width):
    lat = np.empty((2, 8192, width), np.float32)
    cx = np.empty((2, 256, width), np.float32)
    for i in range(8):
        b, qtr = i // 4, i % 4
        lat[b, qtr * 2048:(qtr + 1) * 2048] = results[i][key][:2048]
        if qtr == 0:
            cx[b] = results[i][key][2048:]
    return lat, cx


def kernel(**inputs):
    inp = {k: np.ascontiguousarray(np.asarray(v, dtype=np.float32)) for k, v in inputs.items()}
    x = inp["x"]
    ctx = inp["ctx"]
    for l in range(4):
        r1 = _run(_prog("p1", build_p1), p1_inmaps(inp, l, x, ctx))
        q_all, q_ctx = _gather_tok(r1, "q_o", 512)
        kv_all, kv_ctx = _gather_tok(r1, "kv_o", 256)
        hy_all, hy_ctx = _gather_tok(r1, "hy_o", 768)
        fv_all, fv_ctx = _gather_tok(r1, "fnv_o", 512)
        del r1
        r2 = _run(_prog("p2", build_p2), p2_inmaps(inp, l, hy_all, fv_all, hy_ctx, fv_ctx))
        hyo = np.concatenate([r2[i]["hyom"] for i in range(8)], -1)
        hyo_c = np.concatenate([r2[i]["hyoc"] for i in range(8)], -1)
        fno = np.stack([np.concatenate([r2[b * 4 + g]["fnom"] for g in range(4)], -1) for b in range(2)], 0)
        fno_c = np.stack([np.concatenate([r2[b * 4 + g]["fnoc"] for g in range(4)], -1) for b in range(2)], 0)
        del r2
        r3 = _run(_prog("p3a", build_p3a), p3a_inmaps(inp, l, x, ctx, q_all, kv_all, kv_ctx, hyo, fno, hyo_c, fno_c, q_ctx))
        x1, ctx1 = _gather_tok(r3, "x1", 1024)
        del r3
        ne = 1 if l % 2 == 0 else 8
        r4 = _run(_prog("p3b%d" % ne, lambda: build_p3b(ne)), p3b_inmaps(inp, l, x1, ctx1))
        x, ctx = _gather_tok(r4, "x2", 1024)
        del r4
    return x
```
